# Optimizing a Trainium2 kernel written in Bass

```python
import math
import jax, jax.numpy as jnp
from jax import lax
import numpy as np

D_MODEL = 4096
BATCH = 2
SEQ = 8192
DEPTH = 1

HEAD_DIM = 128
A_HEADS = 16
A_KV_HEADS = 4
A_GROUP = A_HEADS // A_KV_HEADS
WINDOW = 128
A_WIDTH = A_HEADS * HEAD_DIM
B_HEADS = 16
B_WIDTH = B_HEADS * HEAD_DIM
Q_RANK = 1024
KV_RANK = 512
IDX_HEADS = 16
IDX_DIM = 128
TOPK_KEYS = 256
Q_BLOCK = 128
N_BUCKETS = 32
MAX_DISTANCE = 128
MEM_LEN = 256
MEM_HEADS = 4
MEM_WIDTH = MEM_HEADS * HEAD_DIM
N_GROUPS = 8
EXPERTS_PER_GROUP = 8
N_EXPERTS = N_GROUPS * EXPERTS_PER_GROUP
EXPERT_FF = 768
EXPERT_TOPK = 2
MOE_BLOCK = 128
EPS = 1e-6
IN_SIZES = (A_WIDTH, A_KV_HEADS * HEAD_DIM, A_KV_HEADS * HEAD_DIM, Q_RANK, KV_RANK, IDX_DIM, IDX_HEADS, D_MODEL, D_MODEL)
IN_WIDTH = A_WIDTH + 2 * A_KV_HEADS * HEAD_DIM + Q_RANK + KV_RANK + IDX_DIM + IDX_HEADS + 2 * D_MODEL

kernel_name = "hybrid_swa_dsa_hmoe_block"


def rmsnorm(x, g):
    xf = x.astype(jnp.float32)
    y = xf * lax.rsqrt(jnp.mean(xf * xf, axis=-1, keepdims=True) + EPS)
    return (y * g.astype(jnp.float32)).astype(x.dtype)


def rel_bucket(dist):
    max_exact = N_BUCKETS // 2
    d = jnp.maximum(dist, 0)
    df = jnp.maximum(d, 1).astype(jnp.float32)
    large = max_exact + (jnp.log(df / max_exact) / math.log(MAX_DISTANCE / max_exact) * (N_BUCKETS - max_exact)).astype(jnp.int32)
    large = jnp.minimum(large, N_BUCKETS - 1)
    return jnp.where(d < max_exact, d, large)


def swa_sink_attention(q, k, v, sink, bias_table):
    bsz, s = q.shape[0], q.shape[1]
    nb = s // WINDOW
    q = q.reshape(bsz, nb, WINDOW, A_KV_HEADS, A_GROUP, HEAD_DIM)
    k = k.reshape(bsz, nb, WINDOW, A_KV_HEADS, HEAD_DIM)
    v = v.reshape(bsz, nb, WINDOW, A_KV_HEADS, HEAD_DIM)

    def band(t):
        prev = jnp.pad(t[:, :-1], ((0, 0), (1, 0), (0, 0), (0, 0), (0, 0)))
        return jnp.concatenate([prev, t], axis=2)

    kb, vb = band(k), band(v)
    logits = jnp.einsum('bnqkgd,bnskd->bnkgqs', q, kb, preferred_element_type=jnp.float32) * (HEAD_DIM ** -0.5)
    qi = jnp.arange(WINDOW, dtype=jnp.int32)[:, None]
    sj = jnp.arange(2 * WINDOW, dtype=jnp.int32)[None, :]
    dist = qi + WINDOW - sj
    bias = bias_table[rel_bucket(dist)]
    bias = jnp.transpose(bias, (2, 0, 1)).reshape(A_KV_HEADS, A_GROUP, WINDOW, 2 * WINDOW).astype(jnp.float32)
    key_pos = (jnp.arange(nb, dtype=jnp.int32) * WINDOW - WINDOW)[:, None, None] + sj[None]
    valid = (dist >= 0) & (dist < WINDOW) & (key_pos >= 0)
    logits = jnp.where(valid[None, :, None, None], logits + bias, -jnp.inf)
    sink_l = jnp.broadcast_to(sink.reshape(A_KV_HEADS, A_GROUP, 1, 1).astype(jnp.float32), logits.shape[:-1] + (1,))
    p = jax.nn.softmax(jnp.concatenate([logits, sink_l], axis=-1), axis=-1)[..., :-1]
    out = jnp.einsum('bnkgqs,bnskd->bnqkgd', p.astype(vb.dtype), vb)
    return out.reshape(bsz, s, A_WIDTH)


def dsa_mla_attention(q_b, q_idx, w_idx, k_idx, c_kv, w_uk, w_uv, bias_table):
    bsz, s = q_b.shape[0], q_b.shape[1]
    nb = s // Q_BLOCK
    n_keep = min(TOPK_KEYS, s // 4)
    key_pos = jnp.arange(s, dtype=jnp.int32)

    def to_blocks(t):
        return jnp.moveaxis(t.reshape((bsz, nb, Q_BLOCK) + t.shape[2:]), 1, 0)

    def block(args):
        qb, qi, wi, start = args
        t = start + jnp.arange(Q_BLOCK, dtype=jnp.int32)
        rel = jax.nn.relu(jnp.einsum('bqhd,bsd->bqhs', qi, k_idx, preferred_element_type=jnp.float32) * (IDX_DIM ** -0.5))
        score = jnp.einsum('bqh,bqhs->bqs', wi.astype(jnp.float32), rel) * (IDX_HEADS ** -0.5)
        score = jnp.where((key_pos[None, :] <= t[:, None])[None], score, -jnp.inf)
        _, idx = lax.top_k(score, n_keep)
        c_sel = jax.vmap(lambda c, i: c[i])(c_kv, idx)
        q_lat = jnp.einsum('bqhd,rhd->bqhr', qb, w_uk)
        logits = jnp.einsum('bqhr,bqkr->bqhk', q_lat, c_sel, preferred_element_type=jnp.float32) * (HEAD_DIM ** -0.5)
        dist = t[None, :, None] - idx
        bias = jnp.moveaxis(bias_table[rel_bucket(dist)], -1, 2).astype(jnp.float32)
        logits = jnp.where((dist >= 0)[:, :, None, :], logits + bias, -jnp.inf)
        p = jax.nn.softmax(logits, axis=-1).astype(c_sel.dtype)
        o_lat = jnp.einsum('bqhk,bqkr->bqhr', p, c_sel)
        o = jnp.einsum('bqhr,rhd->bqhd', o_lat, w_uv)
        return o.reshape(bsz, Q_BLOCK, B_WIDTH)

    starts = jnp.arange(nb, dtype=jnp.int32) * Q_BLOCK
    out = lax.map(block, (to_blocks(q_b), to_blocks(q_idx), to_blocks(w_idx), starts))
    return jnp.moveaxis(out, 0, 1).reshape(bsz, s, B_WIDTH)


def memory_cross_attention(u, m, w_qm, w_km, w_vm, w_om):
    bsz, s = u.shape[0], u.shape[1]
    n_mem = m.shape[1]
    q = (u @ w_qm).reshape(bsz, s, MEM_HEADS, HEAD_DIM)
    k = (m @ w_km).reshape(bsz, n_mem, MEM_HEADS, HEAD_DIM)
    v = (m @ w_vm).reshape(bsz, n_mem, MEM_HEADS, HEAD_DIM)
    logits = jnp.einsum('bshd,bmhd->bhsm', q, k, preferred_element_type=jnp.float32) * (HEAD_DIM ** -0.5)
    p = jax.nn.softmax(logits, axis=-1).astype(v.dtype)
    o = jnp.einsum('bhsm,bmhd->bshd', p, v).reshape(bsz, s, MEM_WIDTH)
    return o @ w_om


def hierarchical_moe(u, w_grp, b_grp, w_exp, b_exp, w1, w3, w2):
    bsz, s, d = u.shape
    n = bsz * s
    ut = u.reshape(n, d)
    grp_logits = (ut @ w_grp).astype(jnp.float32) + b_grp.astype(jnp.float32)
    grp_p = jax.nn.softmax(grp_logits, axis=-1)
    g_top = jnp.argmax(grp_logits, axis=-1).astype(jnp.int32)
    g_gate = jnp.take_along_axis(grp_p, g_top[:, None], axis=1)
    exp_logits = ((ut @ w_exp).astype(jnp.float32) + b_exp.astype(jnp.float32)).reshape(n, N_GROUPS, EXPERTS_PER_GROUP)
    in_grp = jnp.take_along_axis(exp_logits, g_top[:, None, None], axis=1)[:, 0]
    top_v, top_j = lax.top_k(in_grp, EXPERT_TOPK)
    gate = g_gate * jax.nn.softmax(top_v, axis=-1)
    eid = (g_top[:, None] * EXPERTS_PER_GROUP + top_j).reshape(-1).astype(jnp.int32)
    w_f = gate.reshape(-1)
    n_assign = n * EXPERT_TOPK
    tok_f = jnp.repeat(jnp.arange(n, dtype=jnp.int32), EXPERT_TOPK)
    order = jnp.argsort(eid)
    se = eid[order]
    counts = jnp.bincount(eid, length=N_EXPERTS).astype(jnp.int32)
    padded = ((counts + MOE_BLOCK - 1) // MOE_BLOCK) * MOE_BLOCK
    pad_end = jnp.cumsum(padded)
    pad_start = pad_end - padded
    start = jnp.cumsum(counts) - counts
    rank = jnp.arange(n_assign, dtype=jnp.int32) - start[se]
    dest = pad_start[se] + rank
    n_slots = n_assign + N_EXPERTS * MOE_BLOCK
    n_blk = n_slots // MOE_BLOCK
    slot_tok = jnp.full((n_slots,), n, jnp.int32).at[dest].set(tok_f[order])
    slot_w = jnp.zeros((n_slots,), jnp.float32).at[dest].set(w_f[order])
    blk_e = jnp.minimum(jnp.searchsorted(pad_end, jnp.arange(n_blk, dtype=jnp.int32) * MOE_BLOCK, side='right'), N_EXPERTS - 1).astype(jnp.int32)
    u_ext = jnp.concatenate([ut, jnp.zeros((1, d), ut.dtype)], axis=0)

    def run(args):
        tok, e = args
        h = u_ext[tok]
        return (jax.nn.silu(h @ w1[e]) * (h @ w3[e])) @ w2[e]

    y = lax.map(run, (slot_tok.reshape(n_blk, MOE_BLOCK), blk_e)).reshape(n_slots, d)
    out = jnp.zeros((n + 1, d), y.dtype).at[slot_tok].add(y * slot_w[:, None].astype(y.dtype))[:n]
    return out.reshape(bsz, s, d).astype(u.dtype)


def hybrid_layer(x, mem, rel_bias, g_mix, w_in, g_cq, w_uq, w_qidx, g_ckv, w_uk, w_uv, g_kidx, sink_a,
                 w_pa, w_pb, w_out, g_xattn, g_mem, w_qm, w_km, w_vm, w_om, g_ffn,
                 w_grp, b_grp, w_exp, b_exp, w_e1, w_e3, w_e2):
    bsz, s, _ = x.shape
    u = rmsnorm(x, g_mix)
    proj = u @ w_in
    split_pts = [int(p) for p in np.cumsum(IN_SIZES)[:-1]]
    q_a, k_a, v_a, c_q, c_kv, k_i, w_i, gate_a, gate_b = jnp.split(proj, split_pts, axis=-1)
    o_a = swa_sink_attention(q_a.reshape(bsz, s, A_HEADS, HEAD_DIM),
                             k_a.reshape(bsz, s, A_KV_HEADS, HEAD_DIM),
                             v_a.reshape(bsz, s, A_KV_HEADS, HEAD_DIM),
                             sink_a, rel_bias[:, :A_HEADS])
    c_q = rmsnorm(c_q, g_cq)
    c_kv = rmsnorm(c_kv, g_ckv)
    k_i = rmsnorm(k_i, g_kidx)
    q_b = (c_q @ w_uq).reshape(bsz, s, B_HEADS, HEAD_DIM)
    q_i = (c_q @ w_qidx).reshape(bsz, s, IDX_HEADS, IDX_DIM)
    o_b = dsa_mla_attention(q_b, q_i, w_i, k_i, c_kv, w_uk, w_uv, rel_bias[:, A_HEADS:])
    y = jax.nn.sigmoid(gate_a) * (o_a @ w_pa) + jax.nn.sigmoid(gate_b) * (o_b @ w_pb)
    x = x + y @ w_out
    x = x + memory_cross_attention(rmsnorm(x, g_xattn), rmsnorm(mem, g_mem), w_qm, w_km, w_vm, w_om)
    x = x + hierarchical_moe(rmsnorm(x, g_ffn), w_grp, b_grp, w_exp, b_exp, w_e1, w_e3, w_e2)
    return x


def setup_inputs(seed: int = 0) -> dict:
    key = jax.random.key(seed)
    ks = jax.random.split(key, 32)
    f32 = jnp.float32
    L = DEPTH

    def nrm(k, shape, scale):
        return jax.random.normal(k, shape, f32) * scale

    def gain(k, shape):
        return 1.0 + 0.02 * jax.random.normal(k, shape, f32)

    return {
        "x": nrm(ks[0], (BATCH, SEQ, D_MODEL), 1.0),
        "mem": nrm(ks[1], (BATCH, MEM_LEN, D_MODEL), 1.0),
        "rel_bias": nrm(ks[2], (N_BUCKETS, A_HEADS + B_HEADS), 0.5),
        "g_mix": gain(ks[3], (L, D_MODEL)),
        "w_in": nrm(ks[4], (L, D_MODEL, IN_WIDTH), D_MODEL ** -0.5),
        "g_cq": gain(ks[5], (L, Q_RANK)),
        "w_uq": nrm(ks[6], (L, Q_RANK, B_WIDTH), Q_RANK ** -0.5),
        "w_qidx": nrm(ks[7], (L, Q_RANK, IDX_HEADS * IDX_DIM), Q_RANK ** -0.5),
        "g_ckv": gain(ks[8], (L, KV_RANK)),
        "w_uk": nrm(ks[9], (L, KV_RANK, B_HEADS, HEAD_DIM), KV_RANK ** -0.5),
        "w_uv": nrm(ks[10], (L, KV_RANK, B_HEADS, HEAD_DIM), KV_RANK ** -0.5),
        "g_kidx": gain(ks[11], (L, IDX_DIM)),
        "sink_a": nrm(ks[12], (L, A_HEADS), 0.5),
        "w_pa": nrm(ks[13], (L, A_WIDTH, D_MODEL), A_WIDTH ** -0.5),
        "w_pb": nrm(ks[14], (L, B_WIDTH, D_MODEL), B_WIDTH ** -0.5),
        "w_out": nrm(ks[15], (L, D_MODEL, D_MODEL), D_MODEL ** -0.5),
        "g_xattn": gain(ks[16], (L, D_MODEL)),
        "g_mem": gain(ks[17], (L, D_MODEL)),
        "w_qm": nrm(ks[18], (L, D_MODEL, MEM_WIDTH), D_MODEL ** -0.5),
        "w_km": nrm(ks[19], (L, D_MODEL, MEM_WIDTH), D_MODEL ** -0.5),
        "w_vm": nrm(ks[20], (L, D_MODEL, MEM_WIDTH), D_MODEL ** -0.5),
        "w_om": nrm(ks[21], (L, MEM_WIDTH, D_MODEL), MEM_WIDTH ** -0.5),
        "g_ffn": gain(ks[22], (L, D_MODEL)),
        "w_grp": nrm(ks[23], (L, D_MODEL, N_GROUPS), D_MODEL ** -0.5),
        "b_grp": nrm(ks[24], (L, N_GROUPS), 0.01),
        "w_exp": nrm(ks[25], (L, D_MODEL, N_EXPERTS), D_MODEL ** -0.5),
        "b_exp": nrm(ks[26], (L, N_EXPERTS), 0.01),
        "w_e1": nrm(ks[27], (L, N_EXPERTS, D_MODEL, EXPERT_FF), D_MODEL ** -0.5),
        "w_e3": nrm(ks[28], (L, N_EXPERTS, D_MODEL, EXPERT_FF), D_MODEL ** -0.5),
        "w_e2": nrm(ks[29], (L, N_EXPERTS, EXPERT_FF, D_MODEL), EXPERT_FF ** -0.5),
        "g_final": gain(ks[30], (D_MODEL,)),
    }


def reference(x, mem, rel_bias, g_mix, w_in, g_cq, w_uq, w_qidx, g_ckv, w_uk, w_uv, g_kidx, sink_a,
              w_pa, w_pb, w_out, g_xattn, g_mem, w_qm, w_km, w_vm, w_om, g_ffn,
              w_grp, b_grp, w_exp, b_exp, w_e1, w_e3, w_e2, g_final):
    for l in range(DEPTH):
        x = hybrid_layer(x, mem, rel_bias, g_mix[l], w_in[l], g_cq[l], w_uq[l], w_qidx[l], g_ckv[l],
                         w_uk[l], w_uv[l], g_kidx[l], sink_a[l], w_pa[l], w_pb[l], w_out[l],
                         g_xattn[l], g_mem[l], w_qm[l], w_km[l], w_vm[l], w_om[l], g_ffn[l],
                         w_grp[l], b_grp[l], w_exp[l], b_exp[l], w_e1[l], w_e3[l], w_e2[l])
    return rmsnorm(x, g_final)
```

```python
import math
from contextlib import ExitStack
import numpy as np
import concourse.bass as bass
import concourse.mybir as mybir
from concourse.bass_utils import run_bass_kernel_spmd

F32 = mybir.dt.float32
BF16 = mybir.dt.bfloat16
AF = mybir.ActivationFunctionType
ALU = mybir.AluOpType
AX = mybir.AxisListType

HD = 128
A_HEADS, A_KV = 16, 4
B_HEADS = 16
Q_RANK, KV_RANK = 1024, 512
IDX_HEADS, IDX_DIM = 16, 128
MEM_HEADS = 4
N_GROUPS = 8
EPS = 1e-6
NEG = -30000.0
EPOCH = 30000
DMA_EPOCH = 1800


class Buf:
    __slots__ = ("w", "r")

    def __init__(self):
        self.w = None
        self.r = {}


class T:
    def __init__(self, t):
        self.t = t
        self.b = Buf()

    def __getitem__(self, k):
        return self.t[k]


class Prog:
    def __init__(self, nc, es):
        self.nc = nc
        self.es = es
        self.E = dict(pe=nc.tensor, act=nc.scalar, dve=nc.vector, pool=nc.gpsimd, sp=nc.sync)
        self.cs = {e: [] for e in self.E}
        self.cnt = {e: 0 for e in self.E}
        self.seen = {}
        self.nds = 24
        self.ds = [None] * self.nds
        self.dc = [0] * self.nds
        self.did = [0] * self.nds
        self.dn = 0
        self.nsem = 0
        self.live_d = {}

    def _newsem(self):
        self.nsem += 1
        return self.es.enter_context(self.nc.semaphore(f"s{self.nsem}"))

    def _csem(self, e, ep):
        while len(self.cs[e]) <= ep:
            self.cs[e].append(self._newsem())
        return self.cs[e][ep]

    def _wait(self, e, key, val):
        kind = key[0]
        if kind == "c" and key[1] == e and e == "pe":
            return
        k = (e, key)
        if self.seen.get(k, 0) >= val:
            return
        self.seen[k] = val
        if kind == "c":
            sem = self.cs[key[1]][key[2]]
        else:
            sem = self.live_d[key[1]]
        self.E[e].wait_ge(sem, val)

    def _deps(self, e, reads, writes):
        for b in reads:
            b = b.b if isinstance(b, T) else b
            if b.w is not None:
                self._wait(e, *b.w)
        for b in writes:
            b = b.b if isinstance(b, T) else b
            if b.w is not None:
                self._wait(e, *b.w)
            for key, val in b.r.items():
                self._wait(e, key, val)

    def _mark(self, tok, reads, writes):
        key, val = tok
        for b in reads:
            b = b.b if isinstance(b, T) else b
            if b.r.get(key, 0) < val:
                b.r[key] = val
        for b in writes:
            b = b.b if isinstance(b, T) else b
            b.w = tok
            b.r = {}

    def op(self, e, fn, reads=(), writes=()):
        self._deps(e, reads, writes)
        ins = fn(self.E[e])
        i = self.cnt[e]
        self.cnt[e] += 1
        ep, v = i // EPOCH, i % EPOCH + 1
        ins.then_inc(self._csem(e, ep), 1)
        tok = (("c", e, ep), v)
        self._mark(tok, reads, writes)
        return tok

    def dma(self, q, out, in_, reads=(), writes=(), **kw):
        k = self.dn
        self.dn = (k + 1) % self.nds
        if self.ds[k] is not None and self.dc[k] > 0:
            self._wait(q, ("d", self.did[k]), 16 * self.dc[k])
        if self.ds[k] is None or self.dc[k] >= DMA_EPOCH:
            self.ds[k] = self._newsem()
            self.dc[k] = 0
            self.did[k] = self.nsem
            self.live_d[self.nsem] = self.ds[k]
        self._deps(q, reads, writes)
        ins = self.E[q].dma_start(out=out, in_=in_, **kw)
        self.dc[k] += 1
        ins.then_inc(self.ds[k], 16)
        tok = (("d", self.did[k]), 16 * self.dc[k])
        self._mark(tok, reads, writes)
        return tok

    def barrier(self):
        for e in self.E:
            for f in self.E:
                if f == e or self.cnt[f] == 0:
                    continue
                i = self.cnt[f] - 1
                self._wait(e, ("c", f, i // EPOCH), i % EPOCH + 1)
            for k in range(self.nds):
                if self.ds[k] is not None and self.dc[k] > 0:
                    self._wait(e, ("d", self.did[k]), 16 * self.dc[k])


class Stage:
    uid = 0

    def __init__(self, p):
        self.p = p
        self.es = ExitStack()
        self.n = 0

    def __enter__(self):
        self.es.__enter__()
        return self

    def __exit__(self, *a):
        self.p.barrier()
        return self.es.__exit__(*a)

    def sb(self, shape, dt, name="t"):
        Stage.uid += 1
        return T(self.es.enter_context(self.p.nc.sbuf_tensor(f"{name}_{Stage.uid}", list(shape), dt)))

    def ps(self, shape, dt=F32, name="p"):
        Stage.uid += 1
        return T(self.es.enter_context(self.p.nc.psum_tensor(f"{name}_{Stage.uid}", list(shape), dt)))


def kc_view(ap):
    return ap.rearrange("(kc p) n -> p kc n", p=128)


def gemm(p, dst, srcT, w, K, N, cols, tm, act=None, mulT=None, addT=None, add_tm=None,
         rowscale=None, dst_dt=BF16, cblk=512, wbufs=2, nblk=512, x_ap_fn=None):
    KC = K // 128
    with Stage(p) as s:
        wt = [s.sb([128, KC, cblk], BF16, "wt") for _ in range(wbufs)]
        xt = [s.sb([128, KC, nblk], BF16, "xt") for _ in range(2)]
        ps = [s.ps([128, 512]) for _ in range(2)]
        ot = [s.sb([128, 512], dst_dt, "ot") for _ in range(2)]
        t1 = [s.sb([128, 512], F32, "t1") for _ in range(2)]
        mt = [s.sb([128, 512], BF16, "mt") for _ in range(2)]
        at = [s.sb([128, 512], BF16 if not tm else F32, "at") for _ in range(2)]
        rs = [s.sb([128, 1], F32, "rs") for _ in range(2)]
        wv = kc_view(w)
        xv = kc_view(srcT) if x_ap_fn is None else None
        it = 0
        iw = 0
        ix = 0
        for cb in range(0, cols, cblk):
            cw = min(cblk, cols - cb)
            W = wt[iw % wbufs]
            iw += 1
            for c5 in range(0, cw, 512):
                c5w = min(512, cw - c5)
                p.dma("pool", W[:, :, c5:c5 + c5w], wv[:, :, cb + c5:cb + c5 + c5w], writes=[W])
            for nb in range(0, N, nblk):
                nw = min(nblk, N - nb)
                X = xt[ix % 2]
                ix += 1
                p.dma("sp", X[:, :, :nw], xv[:, :, nb:nb + nw] if x_ap_fn is None else x_ap_fn(nb, nw), writes=[X])
                if not tm:
                    for cc in range(cw // 128):
                        P_, O, T1, M, A = ps[it % 2], ot[it % 2], t1[it % 2], mt[it % 2], at[it % 2]
                        it += 1
                        r0 = cb + cc * 128
                        for kc in range(KC):
                            p.op("pe", lambda e, kc=kc: e.matmul(P_[:, :nw], lhsT=W[:, kc, cc * 128:(cc + 1) * 128],
                                                                  rhs=X[:, kc, :nw], start=(kc == 0), stop=(kc == KC - 1)),
                                 reads=[W, X], writes=[P_])
                        f = act if act is not None else AF.Copy
                        if mulT is None and addT is None:
                            p.op("act", lambda e: e.activation(out=O[:, :nw], in_=P_[:, :nw], func=f), reads=[P_], writes=[O])
                        else:
                            p.op("act", lambda e: e.activation(out=T1[:, :nw], in_=P_[:, :nw], func=f), reads=[P_], writes=[T1])
                            if mulT is not None:
                                p.dma("sp", M[:, :nw], mulT[r0:r0 + 128, nb:nb + nw], writes=[M])
                                tgt = O if addT is None else T1
                                p.op("dve", lambda e: e.tensor_tensor(out=tgt[:, :nw], in0=T1[:, :nw], in1=M[:, :nw], op=ALU.mult),
                                     reads=[T1, M], writes=[tgt])
                            if addT is not None:
                                p.dma("sp", A[:, :nw], addT[r0:r0 + 128, nb:nb + nw], writes=[A])
                                p.op("dve", lambda e: e.tensor_tensor(out=O[:, :nw], in0=T1[:, :nw], in1=A[:, :nw], op=ALU.add),
                                     reads=[T1, A], writes=[O])
                        p.dma("sp", dst[r0:r0 + 128, nb:nb + nw], O[:, :nw], reads=[O])
                else:
                    for ti in range(nw // 128):
                        t0 = nb + ti * 128
                        for c5 in range(0, cw, 512):
                            c5w = min(512, cw - c5)
                            P_, O, A, R = ps[it % 2], ot[it % 2], at[it % 2], rs[it % 2]
                            it += 1
                            for kc in range(KC):
                                p.op("pe", lambda e, kc=kc: e.matmul(P_[:, :c5w], lhsT=X[:, kc, ti * 128:(ti + 1) * 128],
                                                                      rhs=W[:, kc, c5:c5 + c5w], start=(kc == 0), stop=(kc == KC - 1)),
                                     reads=[W, X], writes=[P_])
                            if add_tm is not None:
                                p.dma("sp", A[:, :c5w], add_tm[t0:t0 + 128, cb + c5:cb + c5 + c5w], writes=[A])
                            if rowscale is not None:
                                p.dma("sp", R[:, :], rowscale[t0:t0 + 128, :], writes=[R], allow_slow_non_contiguous=True)
                                p.op("dve", lambda e: e.scalar_tensor_tensor(out=O[:, :c5w], in0=P_[:, :c5w], scalar=R[:, 0:1], in1=A[:, :c5w],
                                                                             op0=ALU.mult, op1=ALU.add), reads=[P_, R, A], writes=[O])
                            elif add_tm is not None:
                                p.op("dve", lambda e: e.tensor_tensor(out=O[:, :c5w], in0=P_[:, :c5w], in1=A[:, :c5w], op=ALU.add),
                                     reads=[P_, A], writes=[O])
                            else:
                                p.op("act", lambda e: e.activation(out=O[:, :c5w], in_=P_[:, :c5w], func=AF.Copy), reads=[P_], writes=[O])
                            p.dma("sp", dst[t0:t0 + 128, cb + c5:cb + c5 + c5w], O[:, :c5w], reads=[O])


def gemm_gated(p, dst_fn, x_ap_fn, w1, w3, gT_row, K, N, FF):
    KC = K // 128
    with Stage(p) as s:
        W1 = s.sb([128, KC, FF], BF16, "w1")
        W3 = s.sb([128, KC, FF], BF16, "w3")
        xt = [s.sb([128, KC, 512], BF16, "xt") for _ in range(2)]
        gb = [s.sb([128, 512], F32, "gb") for _ in range(2)]
        ps1 = [s.ps([128, 512]) for _ in range(2)]
        ps3 = [s.ps([128, 512]) for _ in range(2)]
        t1 = [s.sb([128, 512], F32, "t1") for _ in range(2)]
        ot = [s.sb([128, 512], BF16, "ot") for _ in range(2)]
        for c5 in range(0, FF, 512):
            c5w = min(512, FF - c5)
            p.dma("pool", W1[:, :, c5:c5 + c5w], kc_view(w1)[:, :, c5:c5 + c5w], writes=[W1])
            p.dma("pool", W3[:, :, c5:c5 + c5w], kc_view(w3)[:, :, c5:c5 + c5w], writes=[W3])
        it = 0
        for ib, nb in enumerate(range(0, N, 512)):
            nw = min(512, N - nb)
            X, GB = xt[ib % 2], gb[ib % 2]
            p.dma("sp", X[:, :, :nw], x_ap_fn(nb, nw), writes=[X])
            p.dma("sp", GB[:, :nw], gT_row[nb:nb + nw].partition_broadcast(128), writes=[GB])
            for cc in range(FF // 128):
                P1, P3, T1, O = ps1[it % 2], ps3[it % 2], t1[it % 2], ot[it % 2]
                it += 1
                for kc in range(KC):
                    p.op("pe", lambda e, kc=kc: e.matmul(P1[:, :nw], lhsT=W1[:, kc, cc * 128:(cc + 1) * 128], rhs=X[:, kc, :nw],
                                                          start=(kc == 0), stop=(kc == KC - 1)), reads=[W1, X], writes=[P1])
                for kc in range(KC):
                    p.op("pe", lambda e, kc=kc: e.matmul(P3[:, :nw], lhsT=W3[:, kc, cc * 128:(cc + 1) * 128], rhs=X[:, kc, :nw],
                                                          start=(kc == 0), stop=(kc == KC - 1)), reads=[W3, X], writes=[P3])
                p.op("act", lambda e: e.activation(out=T1[:, :nw], in_=P1[:, :nw], func=AF.Silu), reads=[P1], writes=[T1])
                p.op("dve", lambda e: e.tensor_tensor(out=T1[:, :nw], in0=T1[:, :nw], in1=P3[:, :nw], op=ALU.mult), reads=[T1, P3], writes=[T1])
                p.op("dve", lambda e: e.tensor_tensor(out=O[:, :nw], in0=T1[:, :nw], in1=GB[:, :nw], op=ALU.mult), reads=[T1, GB], writes=[O])
                p.dma("sp", dst_fn(cc, nb, nw), O[:, :nw].rearrange("p (t n) -> p t n", n=128), reads=[O])


def rmsnorm(p, src, g, n, W, dstT=None, dst_tm=None, dst_tm_dt=BF16, dstB=None):
    KC = W // 128
    with Stage(p) as s:
        ident = s.sb([128, 128], BF16, "id")
        idf = s.sb([128, 128], F32, "idf")
        p.op("pool", lambda e: e.memset(idf[:, :], 1.0), writes=[idf])
        p.op("pool", lambda e: e.affine_select(out=idf[:, :], in_=idf[:, :], pattern=[[-1, 128]], compare_op=ALU.is_equal,
                                               fill=0.0, base=0, channel_multiplier=1), reads=[idf], writes=[idf])
        p.op("dve", lambda e: e.tensor_copy(out=ident[:, :], in_=idf[:, :]), reads=[idf], writes=[ident])
        gt = s.sb([128, W], F32, "g")
        p.dma("sp", gt[:, :], g.partition_broadcast(128), writes=[gt])
        xt = [s.sb([128, W], F32, "x") for _ in range(2)]
        junk = s.sb([128, W], BF16, "junk")
        un = [s.sb([128, W], dst_tm_dt if dst_tm is not None else BF16, "un") for _ in range(2)]
        unb = [s.sb([128, W], BF16, "unb") for _ in range(2)] if (dst_tm is not None and dst_tm_dt != BF16 and (dstT is not None or dstB is not None)) else None
        ss = [s.sb([128, 1], F32, "ss") for _ in range(2)]
        rstd = [s.sb([128, 1], F32, "rstd") for _ in range(2)]
        uT = [s.sb([128, KC, 128], BF16, "uT") for _ in range(2)]
        pt = [s.ps([128, 512], BF16, "pt") for _ in range(2)]
        ip = 0
        for i in range(n // 128):
            X, U, SS, R, UT = xt[i % 2], un[i % 2], ss[i % 2], rstd[i % 2], uT[i % 2]
            p.dma("sp", X[:, :], src[i * 128:(i + 1) * 128, :], writes=[X])
            p.op("act", lambda e: e.activation(out=junk[:, :], in_=X[:, :], func=AF.Square, accum_out=SS[:, 0:1]),
                 reads=[X], writes=[junk, SS])
            p.op("dve", lambda e: e.tensor_scalar(out=R[:, :], in0=SS[:, :], scalar1=1.0 / W, scalar2=EPS, op0=ALU.mult, op1=ALU.add),
                 reads=[SS], writes=[R])
            p.op("act", lambda e: e.activation(out=R[:, :], in_=R[:, :], func=AF.Sqrt), reads=[R], writes=[R])
            p.op("dve", lambda e: e.reciprocal(out=R[:, :], in_=R[:, :]), reads=[R], writes=[R])
            p.op("dve", lambda e: e.scalar_tensor_tensor(out=U[:, :], in0=X[:, :], scalar=R[:, 0:1], in1=gt[:, :], op0=ALU.mult, op1=ALU.mult),
                 reads=[X, R, gt], writes=[U])
            if dst_tm is not None:
                p.dma("sp", dst_tm[i * 128:(i + 1) * 128, :], U[:, :], reads=[U])
            if dstT is None and dstB is None:
                continue
            UB = U
            if unb is not None:
                UB = unb[i % 2]
                p.op("act", lambda e: e.activation(out=UB[:, :], in_=U[:, :], func=AF.Copy), reads=[U], writes=[UB])
            for c0 in range(0, KC, 4):
                PT = pt[ip % 2]
                ip += 1
                nc_ = min(4, KC - c0)
                for j in range(nc_):
                    c = c0 + j
                    p.op("pe", lambda e, c=c, j=j: e.transpose(out=PT[:, j * 128:(j + 1) * 128], in_=UB[:, c * 128:(c + 1) * 128],
                                                               identity=ident[:, :]), reads=[UB, ident], writes=[PT])
                p.op("dve", lambda e: e.tensor_copy(out=UT[:, c0:c0 + nc_, :], in_=PT[:, :nc_ * 128].rearrange("p (c n) -> p c n", n=128)),
                     reads=[PT], writes=[UT])
            if dstB is not None:
                p.dma("sp", dstB[i // 4, :, :, (i % 4) * 128:(i % 4 + 1) * 128], UT[:, :, :], reads=[UT])
            else:
                p.dma("sp", kc_view(dstT)[:, :, i * 128:(i + 1) * 128], UT[:, :, :], reads=[UT])


def attention(p, s, S, outT, qT, kT, vtm, nheads, gqa, kbs_of, bias_of, mask_of, esink, scale, pre_qb=None, pe_bias=None):
    ones = s.sb([128, 128], BF16, "ones")
    p.op("pool", lambda e: e.memset(ones[:, :], 1.0), writes=[ones])
    qq = [s.sb([128, nheads, 128], BF16, "qq") for _ in range(2)]
    KCH = 8
    kk = [s.sb([128, 4 if gqa == 1 else 1, KCH * 128], BF16, "kk") for _ in range(2)]
    vv = [s.sb([128, KCH, 4 if gqa == 1 else 1, 128], BF16, "vv") for _ in range(2)]
    psl = [s.ps([128, 512], F32, "psl") for _ in range(2)]
    pso = [s.ps([128, 512], F32, "pso") for _ in range(4)]
    psd = s.ps([128, 512], F32, "psd")
    tmp = [s.sb([128, 512], F32, "tmp") for _ in range(2)]
    eT = [s.sb([128, 512], BF16, "eT") for _ in range(2)]
    rec = s.sb([128, 512], F32, "rec")
    oT = [s.sb([128, 4, 128], BF16, "oT") for _ in range(2)]
    qv = qT.rearrange("(h d) n -> d h n", d=128)
    kv = kT.rearrange("(h d) n -> d h n", d=128)
    vvw = vtm.rearrange("(kb s) (h d) -> s kb h d", s=128, d=128)
    ov = outT.rearrange("(h d) n -> d h n", d=128)
    it = 0
    ik = 0
    io = 0
    pending = [None]
    if pre_qb is not None:
        pre_qb(0, psl)
    for qb in range(S // 128):
        if pre_qb is not None and qb + 1 < S // 128:
            pre_qb(qb + 1, psl)
        Q = qq[qb % 2]
        p.dma("sp", Q[:, :, :], qv[:, :, qb * 128:(qb + 1) * 128], writes=[Q])
        kbs = kbs_of(qb)
        for hg in range(nheads // 4):
            h0 = hg * 4
            first = True
            for c0 in range(0, len(kbs), KCH):
                ch = kbs[c0:c0 + KCH]
                kb0, nkb = ch[0], len(ch)
                KK, VV = kk[ik % 2], vv[ik % 2]
                ik += 1
                if gqa == 1:
                    p.dma("sp", KK[:, :, :nkb * 128], kv[:, h0:h0 + 4, kb0 * 128:(kb0 + nkb) * 128], writes=[KK])
                    p.dma("sp", VV[:, :nkb, :, :], vvw[:, kb0:kb0 + nkb, h0:h0 + 4, :], writes=[VV])
                else:
                    p.dma("sp", KK[:, :, :nkb * 128], kv[:, hg:hg + 1, kb0 * 128:(kb0 + nkb) * 128], writes=[KK])
                    p.dma("sp", VV[:, :nkb, :, :], vvw[:, kb0:kb0 + nkb, hg:hg + 1, :], writes=[VV])
                for j, kb in enumerate(ch):
                    last = (c0 + j == len(kbs) - 1)
                    PL, TM, E = psl[it % 2], tmp[it % 2], eT[it % 2]
                    it += 1
                    bz = bias_of(qb, kb, h0) if bias_of is not None else None
                    mk = mask_of(qb, kb) if mask_of is not None else None
                    if pe_bias is not None:
                        kind, bap, bbuf = bz
                        map_, mbuf = mk
                        mrep = map_.unsqueeze(1).broadcast_to([128, 4, 128])
                        if kind == "tile":
                            p.op("pe", lambda e: e.matmul(PL[:, :], lhsT=pe_bias["ident"][:, :], rhs=bap, start=True, stop=False, skip_group_check=True),
                                 reads=[pe_bias["ident"], bbuf], writes=[PL])
                        p.op("pe", lambda e: e.matmul(PL[:, :].rearrange("p (h n) -> p h n", n=128), lhsT=pe_bias["ident"][:, :], rhs=mrep,
                                                      start=(kind != "tile"), stop=False, skip_group_check=True),
                             reads=[pe_bias["ident"], mbuf], writes=[PL])
                        for hh in range(4):
                            p.op("pe", lambda e, hh=hh: e.matmul(PL[:, hh * 128:(hh + 1) * 128], lhsT=KK[:, hh, j * 128:(j + 1) * 128],
                                                                 rhs=Q[:, h0 + hh, :], start=False, stop=(hh == 3), skip_group_check=True),
                                 reads=[KK, Q], writes=[PL])
                        if kind == "tile":
                            p.op("act", lambda e: e.activation(out=E[:, :], in_=PL[:, :], func=AF.Exp, scale=scale), reads=[PL], writes=[E])
                        else:
                            for hh in range(4):
                                p.op("act", lambda e, hh=hh: e.activation(out=E[:, hh * 128:(hh + 1) * 128], in_=PL[:, hh * 128:(hh + 1) * 128], func=AF.Exp,
                                                                          scale=scale, bias=bap[:, h0 + hh:h0 + hh + 1]), reads=[PL, bbuf], writes=[E])
                    else:
                        for hh in range(4):
                            kh = hh if gqa == 1 else 0
                            p.op("pe", lambda e, hh=hh, kh=kh: e.matmul(PL[:, hh * 128:(hh + 1) * 128], lhsT=KK[:, kh, j * 128:(j + 1) * 128],
                                                                        rhs=Q[:, h0 + hh, :], start=True, stop=True),
                                 reads=[KK, Q], writes=[PL])
                        if bz is None and mk is None:
                            p.op("act", lambda e: e.activation(out=E[:, :], in_=PL[:, :], func=AF.Exp, scale=scale), reads=[PL], writes=[E])
                        else:
                            bap, bbuf = bz
                            p.op("dve", lambda e: e.scalar_tensor_tensor(out=TM[:, :].rearrange("p (h n) -> p h n", n=128),
                                                                         in0=PL[:, :].rearrange("p (h n) -> p h n", n=128), scalar=scale, in1=bap,
                                                                         op0=ALU.mult, op1=ALU.add), reads=[PL, bbuf], writes=[TM])
                            p.op("act", lambda e: e.activation(out=E[:, :], in_=TM[:, :], func=AF.Exp), reads=[TM], writes=[E])

                    def pv(E=E, VV=VV, j=j, first=first, last=last):
                        for hh in range(4):
                            kh = hh if gqa == 1 else 0
                            PO = pso[hh]
                            p.op("pe", lambda e, hh=hh, kh=kh, PO=PO: e.matmul(PO[:, :128], lhsT=VV[:, j, kh, :], rhs=E[:, hh * 128:(hh + 1) * 128],
                                                                               start=first, stop=last), reads=[VV, E], writes=[PO])
                        p.op("pe", lambda e: e.matmul(psd[:, :], lhsT=ones[:, :], rhs=E[:, :], start=first, stop=last),
                             reads=[ones, E], writes=[psd])

                    if pending[0] is not None:
                        pending[0]()
                    pending[0] = pv
                    first = False
            if pending[0] is not None:
                pending[0]()
                pending[0] = None
            O = oT[io % 2]
            io += 1
            if esink is not None:
                for hh in range(4):
                    p.op("dve", lambda e, hh=hh: e.tensor_scalar(out=rec[:, hh * 128:(hh + 1) * 128], in0=psd[:, hh * 128:(hh + 1) * 128],
                                                                 scalar1=esink[:, h0 + hh:h0 + hh + 1], scalar2=None, op0=ALU.add),
                         reads=[psd, esink], writes=[rec])
                p.op("dve", lambda e: e.reciprocal(out=rec[:, :], in_=rec[:, :]), reads=[rec], writes=[rec])
            else:
                p.op("dve", lambda e: e.reciprocal(out=rec[:, :], in_=psd[:, :]), reads=[psd], writes=[rec])
            for hh in range(4):
                PO = pso[hh]
                p.op("dve", lambda e, hh=hh, PO=PO: e.tensor_tensor(out=O[:, hh, :], in0=PO[:, :128], in1=rec[:, hh * 128:(hh + 1) * 128], op=ALU.mult),
                     reads=[PO, rec], writes=[O])
            p.dma("sp", ov[:, h0:h0 + 4, qb * 128:(qb + 1) * 128], O[:, :, :], reads=[O])


IN_OFF = {}
_o = 0
for _n, _w in (("q_a", 2048), ("k_a", 512), ("v_a", 512), ("c_q", 1024), ("c_kv", 512), ("k_i", 128), ("w_i", 16),
               ("gate_a", None), ("gate_b", None)):
    IN_OFF[_n] = _o
    if _w is not None:
        _o += _w


def xb(B):
    return lambda nb, nw: B[nb // 512][:, :, :nw]


def build(cfg):
    D, S, ML, NE, EPG, FF, KEEP, NIT = cfg["D"], cfg["S"], cfg["ML"], cfg["NE"], cfg["EPG"], cfg["FF"], cfg["KEEP"], cfg["NIT"]
    NEX = N_GROUPS * EPG
    INW = 4752 + 2 * D
    OFF_GA, OFF_GB = 4752, 4752 + D
    NQB = S // 128
    nc = bass.Bass("TRN2", target_bir_lowering=False)

    def din(name, shape, dt=F32):
        return nc.dram_tensor(name, list(shape), dt, kind="ExternalInput").ap()

    def dscr(name, shape, dt):
        return nc.dram_tensor(name, list(shape), dt, kind="Internal").ap()

    x = din("x", [S, D]); mem = din("mem", [ML, D])
    rel31 = din("rel31", [16]); sbias = din("sbias", [128, 16, 2, 128]); dbias = din("dbias", [128, 16, 2, 128])
    g_mix = din("g_mix", [D]); w_in = din("w_in", [D, INW]); g_cq = din("g_cq", [Q_RANK]); w_uq = din("w_uq", [Q_RANK, 2048])
    w_qidx = din("w_qidx", [Q_RANK, 2048]); g_ckv = din("g_ckv", [KV_RANK]); w_uk = din("w_uk", [KV_RANK, 2048])
    w_uv = din("w_uv", [KV_RANK, 2048]); g_kidx = din("g_kidx", [IDX_DIM]); sink_a = din("sink_a", [16])
    w_pa = din("w_pa", [2048, D]); w_pb = din("w_pb", [2048, D]); w_out = din("w_out", [D, D])
    g_xattn = din("g_xattn", [D]); g_mem = din("g_mem", [D]); w_qm = din("w_qm", [D, 512]); w_km = din("w_km", [D, 512])
    w_vm = din("w_vm", [D, 512]); w_om = din("w_om", [512, D]); g_ffn = din("g_ffn", [D])
    w_grp = din("w_grp", [D, N_GROUPS]); b_grp = din("b_grp", [N_GROUPS]); w_exp = din("w_exp", [D, NEX]); b_exp = din("b_exp", [NEX])
    w_e1 = din("w_e1", [NE, D, FF]); w_e3 = din("w_e3", [NE, D, FF]); w_e2 = din("w_e2", [NE, FF, D]); g_final = din("g_final", [D])
    gsel = din("gsel", [N_GROUPS])
    esel = din("esel", [NE, NEX])
    out = nc.dram_tensor("out", [S, D], F32, kind="ExternalOutput").ap()
    own_o = nc.dram_tensor("own", [S, 1], F32, kind="ExternalOutput").ap()

    uT = dscr("uT", [S // 512, 128, D // 128, 512], BF16)
    qkT = dscr("qkT", [2560, S], BF16)
    sgT = dscr("sgT", [2 * D, S], BF16)
    vaTM = dscr("vaTM", [S, 512], BF16)
    tmA = dscr("tmA", [S, 1680], F32)
    cqT = dscr("cqT", [S // 512, 128, Q_RANK // 128, 512], BF16)
    ckvT = dscr("ckvT", [S // 512, 128, KV_RANK // 128, 512], BF16)
    kiT = dscr("kiT", [IDX_DIM, S], BF16)
    qbT = dscr("qbT", [2048, S], BF16)
    qiT = dscr("qiT", [2048, S], BF16)
    kbT = dscr("kbT", [2048, S], BF16)
    vbTM = dscr("vbTM", [S, 2048], BF16)
    oaT = dscr("oaT", [2048, S], BF16)
    obT = dscr("obT", [2048, S], BF16)
    y1T = dscr("y1T", [D, S], BF16)
    yT = dscr("yT", [D, S], BF16)
    x1 = dscr("x1", [S, D], F32)
    u2T = dscr("u2T", [S // 512, 128, D // 128, 512], BF16)
    memT = dscr("memT", [D, ML], BF16)
    qmT = dscr("qmT", [512, S], BF16)
    kmT = dscr("kmT", [512, ML], BF16)
    vmTM = dscr("vmTM", [ML, 512], BF16)
    omT = dscr("omT", [512, S], BF16)
    x2 = dscr("x2", [S, D], F32)
    u3T = dscr("u3T", [S // 512, 128, D // 128, 512], BF16)
    rl = dscr("rl", [S, N_GROUPS + NEX], F32)
    GTt = dscr("GTt", [NE, S], F32)
    HT4 = dscr("HT4", [S // 128, 128, NE * FF // 128, 128], BF16)

    with ExitStack() as es:
        p = Prog(nc, es)
        scale = HD ** -0.5

        rmsnorm(p, x, g_mix, S, D, dstB=uT)
        gemm(p, qkT, None, w_in[:, 0:2560], D, S, 2560, tm=False, cblk=1024, wbufs=1, x_ap_fn=xb(uT))
        gemm(p, sgT, None, w_in[:, OFF_GA:OFF_GA + 2 * D], D, S, 2 * D, tm=False, act=AF.Sigmoid, cblk=1024, wbufs=1, x_ap_fn=xb(uT))
        gemm(p, vaTM, None, w_in[:, 2560:3072], D, S, 512, tm=True, dst_dt=BF16, x_ap_fn=xb(uT))
        gemm(p, tmA, None, w_in[:, 3072:4752], D, S, 1680, tm=True, dst_dt=F32, cblk=1024, wbufs=1, x_ap_fn=xb(uT))
        rmsnorm(p, tmA[:, 0:1024], g_cq, S, 1024, dstB=cqT)
        rmsnorm(p, tmA[:, 1024:1536], g_ckv, S, 512, dstB=ckvT)
        rmsnorm(p, tmA[:, 1536:1664], g_kidx, S, 128, dstT=kiT)
        gemm(p, qbT, None, w_uq, Q_RANK, S, 2048, tm=False, x_ap_fn=xb(cqT))
        gemm(p, qiT, None, w_qidx, Q_RANK, S, 2048, tm=False, x_ap_fn=xb(cqT))
        gemm(p, kbT, None, w_uk, KV_RANK, S, 2048, tm=False, x_ap_fn=xb(ckvT))
        gemm(p, vbTM, None, w_uv, KV_RANK, S, 2048, tm=True, dst_dt=BF16, x_ap_fn=xb(ckvT))

        with Stage(p) as s:
            sb_ = s.sb([128, 16, 2, 128], F32, "sbias")
            p.dma("sp", sb_[:, :, :, :], sbias, writes=[sb_])
            es_ = s.sb([128, 16], F32, "esink")
            p.dma("sp", es_[:, :], sink_a.partition_broadcast(128), writes=[es_])
            p.op("act", lambda e: e.activation(out=es_[:, :], in_=es_[:, :], func=AF.Exp), reads=[es_], writes=[es_])

            def kbs_a(qb):
                return [qb] if qb == 0 else [qb - 1, qb]

            def bias_a(qb, kb, h0):
                j = 1 if kb == qb else 0
                return sb_[:, h0:h0 + 4, j, :], sb_

            attention(p, s, S, oaT, qkT[0:2048, :], qkT[2048:2560, :], vaTM, 16, 4, kbs_a, bias_a, None, es_, scale)

        with Stage(p) as s:
            inv = 1.0 / scale
            db_ = s.sb([128, 16, 2, 128], F32, "dbias")
            p.dma("sp", db_[:, :, :, :], dbias, writes=[db_])
            dbb = s.sb([128, 16, 2, 128], BF16, "dbb")
            p.op("dve", lambda e: e.tensor_scalar(out=dbb[:, :, :, :], in0=db_[:, :, :, :], scalar1=inv, scalar2=None, op0=ALU.mult),
                 reads=[db_], writes=[dbb])
            dbd = s.sb([128, 16, 128], BF16, "dbd")
            dba = s.sb([128, 16, 128], BF16, "dba")
            p.op("dve", lambda e: e.tensor_copy(out=dbd[:, :, :], in_=dbb[:, :, 0, :]), reads=[dbb], writes=[dbd])
            p.op("dve", lambda e: e.tensor_copy(out=dba[:, :, :], in_=dbb[:, :, 1, :]), reads=[dbb], writes=[dba])
            r31 = s.sb([128, 16], F32, "r31")
            p.dma("sp", r31[:, :], rel31.partition_broadcast(128), writes=[r31])
            ident = s.sb([128, 128], BF16, "id")
            idf = s.sb([128, 128], F32, "idf")
            p.op("pool", lambda e: e.memset(idf[:, :], 1.0), writes=[idf])
            p.op("pool", lambda e: e.affine_select(out=idf[:, :], in_=idf[:, :], pattern=[[-1, 128]], compare_op=ALU.is_equal,
                                                   fill=0.0, base=0, channel_multiplier=1), reads=[idf], writes=[idf])
            p.op("dve", lambda e: e.tensor_copy(out=ident[:, :], in_=idf[:, :]), reads=[idf], writes=[ident])
            cneg = s.sb([128, 128], F32, "cneg")
            p.op("pool", lambda e: e.memset(cneg[:, :], 0.0), writes=[cneg])
            p.op("pool", lambda e: e.affine_select(out=cneg[:, :], in_=cneg[:, :], pattern=[[-1, 128]], compare_op=ALU.is_ge,
                                                   fill=-1e30, base=0, channel_multiplier=1), reads=[cneg], writes=[cneg])
            kis = s.sb([128, S], BF16, "kis")
            p.dma("sp", kis[:, :], kiT, writes=[kis])
            acc = s.sb([128, S], F32, "acc")
            mk = s.sb([128, S], BF16, "mk")
            mkT = [s.sb([128, S], BF16, "mkT") for _ in range(2)]
            qi = [s.sb([128, 16, 128], BF16, "qi") for _ in range(2)]
            wi = [s.sb([128, 16], F32, "wi") for _ in range(2)]
            rl_ = [s.sb([128, 512], F32, "relu") for _ in range(2)]
            ptx = s.ps([128, 512], BF16, "ptx")
            sm = {n: s.sb([128, 1], F32, n) for n in ("lo", "hi", "mid", "cnt", "ge", "t1")}
            qiv = qiT.rearrange("(h d) n -> d h n", d=128)
            cnt_i = [0]

            def indexer(qb, psl):
                nk = (qb + 1) * 128
                QI, WI, MT = qi[qb % 2], wi[qb % 2], mkT[qb % 2]
                p.dma("sp", QI[:, :, :], qiv[:, :, qb * 128:(qb + 1) * 128], writes=[QI])
                p.dma("sp", WI[:, :], tmA[qb * 128:(qb + 1) * 128, 1664:1680], writes=[WI])
                for c in range(0, nk, 512):
                    cw = min(512, nk - c)
                    for h in range(16):
                        R = rl_[cnt_i[0] % 2]
                        PX = psl[cnt_i[0] % 2]
                        cnt_i[0] += 1
                        p.op("pe", lambda e, h=h: e.matmul(PX[:, :cw], lhsT=QI[:, h, :], rhs=kis[:, c:c + cw], start=True, stop=True),
                             reads=[QI, kis], writes=[PX])
                        p.op("act", lambda e: e.activation(out=R[:, :cw], in_=PX[:, :cw], func=AF.Relu), reads=[PX], writes=[R])
                        if h == 0:
                            p.op("dve", lambda e, h=h: e.tensor_scalar(out=acc[:, c:c + cw], in0=R[:, :cw], scalar1=WI[:, h:h + 1], scalar2=None,
                                                                       op0=ALU.mult), reads=[R, WI], writes=[acc])
                        else:
                            p.op("dve", lambda e, h=h: e.scalar_tensor_tensor(out=acc[:, c:c + cw], in0=R[:, :cw], scalar=WI[:, h:h + 1],
                                                                              in1=acc[:, c:c + cw], op0=ALU.mult, op1=ALU.add),
                                 reads=[R, WI, acc], writes=[acc])
                lo, hi, mid, cnt, ge, t1 = (sm[n] for n in ("lo", "hi", "mid", "cnt", "ge", "t1"))
                p.op("dve", lambda e: e.tensor_reduce(out=lo[:, :], in_=acc[:, :nk], axis=AX.X, op=ALU.min), reads=[acc], writes=[lo])
                p.op("dve", lambda e: e.tensor_reduce(out=hi[:, :], in_=acc[:, :nk], axis=AX.X, op=ALU.max), reads=[acc], writes=[hi])
                p.op("dve", lambda e: e.tensor_scalar(out=lo[:, :], in0=lo[:, :], scalar1=-1.0, scalar2=None, op0=ALU.add), reads=[lo], writes=[lo])
                p.op("dve", lambda e: e.tensor_scalar(out=hi[:, :], in0=hi[:, :], scalar1=1.0, scalar2=None, op0=ALU.add), reads=[hi], writes=[hi])
                p.op("dve", lambda e: e.tensor_tensor(out=acc[:, qb * 128:nk], in0=acc[:, qb * 128:nk], in1=cneg[:, :], op=ALU.add),
                     reads=[acc, cneg], writes=[acc])
                for _ in range(NIT):
                    p.op("dve", lambda e: e.tensor_tensor(out=mid[:, :], in0=lo[:, :], in1=hi[:, :], op=ALU.add), reads=[lo, hi], writes=[mid])
                    p.op("dve", lambda e: e.tensor_scalar(out=mid[:, :], in0=mid[:, :], scalar1=0.5, scalar2=None, op0=ALU.mult), reads=[mid], writes=[mid])
                    p.op("dve", lambda e: e.tensor_scalar(out=mk[:, :nk], in0=acc[:, :nk], scalar1=mid[:, 0:1], scalar2=None, op0=ALU.is_ge,
                                                          op1=ALU.add, accum_out=cnt[:, 0:1]), reads=[acc, mid], writes=[mk, cnt])
                    p.op("dve", lambda e: e.tensor_scalar(out=ge[:, :], in0=cnt[:, :], scalar1=KEEP - 0.5, scalar2=None, op0=ALU.is_ge),
                         reads=[cnt], writes=[ge])
                    p.op("dve", lambda e: e.tensor_tensor(out=t1[:, :], in0=mid[:, :], in1=lo[:, :], op=ALU.subtract), reads=[mid, lo], writes=[t1])
                    p.op("dve", lambda e: e.scalar_tensor_tensor(out=lo[:, :], in0=t1[:, :], scalar=ge[:, 0:1], in1=lo[:, :], op0=ALU.mult, op1=ALU.add),
                         reads=[t1, ge, lo], writes=[lo])
                    p.op("dve", lambda e: e.tensor_tensor(out=t1[:, :], in0=hi[:, :], in1=mid[:, :], op=ALU.subtract), reads=[hi, mid], writes=[t1])
                    p.op("dve", lambda e: e.scalar_tensor_tensor(out=hi[:, :], in0=t1[:, :], scalar=ge[:, 0:1], in1=mid[:, :], op0=ALU.mult, op1=ALU.add),
                         reads=[t1, ge, mid], writes=[hi])
                p.op("dve", lambda e: e.tensor_scalar(out=mk[:, :nk], in0=acc[:, :nk], scalar1=lo[:, 0:1], scalar2=NEG, op0=ALU.is_lt, op1=ALU.mult),
                     reads=[acc, lo], writes=[mk])
                for k0 in range(0, qb + 1, 4):
                    n4 = min(4, qb + 1 - k0)
                    for j in range(n4):
                        p.op("pe", lambda e, j=j: e.transpose(out=ptx[:, j * 128:(j + 1) * 128], in_=mk[:, (k0 + j) * 128:(k0 + j + 1) * 128],
                                                              identity=ident[:, :]), reads=[mk, ident], writes=[ptx])
                    p.op("act", lambda e: e.activation(out=MT[:, k0 * 128:(k0 + n4) * 128], in_=ptx[:, :n4 * 128], func=AF.Copy),
                         reads=[ptx], writes=[MT])

            def kbs_b(qb):
                return list(range(qb + 1))

            def bias_b(qb, kb, h0):
                if kb == qb:
                    return "tile", dbd[:, h0:h0 + 4, :].rearrange("p h n -> p (h n)"), dbd
                if kb == qb - 1:
                    return "tile", dba[:, h0:h0 + 4, :].rearrange("p h n -> p (h n)"), dba
                return "col", r31, r31

            def mask_b(qb, kb):
                return mkT[qb % 2][:, kb * 128:(kb + 1) * 128], mkT[qb % 2]

            attention(p, s, S, obT, qbT, kbT, vbTM, 16, 1, kbs_b, bias_b, mask_b, None, scale, pre_qb=indexer,
                      pe_bias=dict(ident=ident))

        gemm(p, y1T, oaT, w_pa, 2048, S, D, tm=False, mulT=sgT[0:D, :], cblk=1024)
        gemm(p, yT, obT, w_pb, 2048, S, D, tm=False, mulT=sgT[D:2 * D, :], addT=y1T, cblk=1024)
        gemm(p, x1, yT, w_out, D, S, D, tm=True, add_tm=x, dst_dt=F32, cblk=1024, wbufs=1)

        rmsnorm(p, x1, g_xattn, S, D, dstB=u2T)
        rmsnorm(p, mem, g_mem, ML, D, dstT=memT)
        gemm(p, qmT, None, w_qm, D, S, 512, tm=False, x_ap_fn=xb(u2T))
        gemm(p, kmT, memT, w_km, D, ML, 512, tm=False)
        gemm(p, vmTM, memT, w_vm, D, ML, 512, tm=True, dst_dt=BF16)
        with Stage(p) as s:
            attention(p, s, S, omT, qmT, kmT, vmTM, 4, 1, lambda qb: list(range(ML // 128)), None, None, None, scale)
        gemm(p, x2, omT, w_om, 512, S, D, tm=True, add_tm=x1, dst_dt=F32)

        rmsnorm(p, x2, g_ffn, S, D, dstB=u3T)
        gemm(p, rl[:, 0:N_GROUPS], None, w_grp, D, S, N_GROUPS, tm=True, dst_dt=F32, x_ap_fn=xb(u3T))
        gemm(p, rl[:, N_GROUPS:], None, w_exp, D, S, NEX, tm=True, dst_dt=F32, x_ap_fn=xb(u3T))
        with Stage(p) as s:
            bg = s.sb([128, N_GROUPS], F32, "bg"); be = s.sb([128, NEX], F32, "be"); gs = s.sb([128, N_GROUPS], F32, "gs")
            esl = s.sb([128, NE, NEX], F32, "esl")
            p.dma("sp", bg[:, :], b_grp.partition_broadcast(128), writes=[bg])
            p.dma("sp", be[:, :], b_exp.partition_broadcast(128), writes=[be])
            p.dma("sp", gs[:, :], gsel.partition_broadcast(128), writes=[gs])
            p.dma("sp", esl[:, :, :], esel.partition_broadcast(128), writes=[esl])
            L = s.sb([128, N_GROUPS + NEX], F32, "L")
            V = {n: s.sb([128, 1], F32, n) for n in ("gmax", "ngmax", "gsum", "top1", "top2", "d", "p1", "p2", "own")}
            goh = s.sb([128, N_GROUPS], F32, "goh"); pen = s.sb([128, N_GROUPS], F32, "pen"); gj = s.sb([128, N_GROUPS], F32, "gj")
            elm = s.sb([128, NEX], F32, "elm"); oh1 = s.sb([128, NEX], F32, "oh1"); oh2 = s.sb([128, NEX], F32, "oh2"); G = s.sb([128, NEX], F32, "G")
            prod = s.sb([128, NE, NEX], F32, "prod"); GL = s.sb([128, NE], F32, "GL")
            idf = s.sb([128, 128], F32, "idf")
            p.op("pool", lambda e: e.memset(idf[:, :], 1.0), writes=[idf])
            p.op("pool", lambda e: e.affine_select(out=idf[:, :], in_=idf[:, :], pattern=[[-1, 128]], compare_op=ALU.is_equal,
                                                   fill=0.0, base=0, channel_multiplier=1), reads=[idf], writes=[idf])
            pgt = s.ps([128, 128], F32, "pgt")
            glt = s.sb([NE, 128], F32, "glt")

            def dv(fn, reads, writes):
                p.op("dve", fn, reads=reads, writes=writes)

            for i in range(NQB):
                p.dma("sp", L[:, :], rl[i * 128:(i + 1) * 128, :], writes=[L])
                gl, el = L[:, 0:N_GROUPS], L[:, N_GROUPS:]
                dv(lambda e: e.tensor_tensor(out=gl, in0=gl, in1=bg[:, :], op=ALU.add), [L, bg], [L])
                dv(lambda e: e.tensor_tensor(out=el, in0=el, in1=be[:, :], op=ALU.add), [L, be], [L])
                dv(lambda e: e.tensor_reduce(out=V["gmax"][:, :], in_=gl, axis=AX.X, op=ALU.max), [L], [V["gmax"]])
                dv(lambda e: e.tensor_scalar(out=goh[:, :], in0=gl, scalar1=V["gmax"][:, 0:1], scalar2=None, op0=ALU.is_equal), [L, V["gmax"]], [goh])
                dv(lambda e: e.tensor_scalar(out=V["ngmax"][:, :], in0=V["gmax"][:, :], scalar1=-1.0, scalar2=None, op0=ALU.mult), [V["gmax"]], [V["ngmax"]])
                p.op("act", lambda e: e.activation(out=gj[:, :], in_=gl, func=AF.Exp, bias=V["ngmax"][:, 0:1], accum_out=V["gsum"][:, 0:1]),
                     reads=[L, V["ngmax"]], writes=[gj, V["gsum"]])
                dv(lambda e: e.tensor_scalar(out=pen[:, :], in0=goh[:, :], scalar1=-1.0, scalar2=1e9, op0=ALU.add, op1=ALU.mult), [goh], [pen])
                dv(lambda e: e.tensor_tensor(out=elm[:, :].rearrange("p (g j) -> p g j", j=EPG), in0=el.rearrange("p (g j) -> p g j", j=EPG),
                                             in1=pen[:, :].unsqueeze(2).broadcast_to([128, N_GROUPS, EPG]), op=ALU.add), [L, pen], [elm])
                dv(lambda e: e.tensor_reduce(out=V["top1"][:, :], in_=elm[:, :], axis=AX.X, op=ALU.max), [elm], [V["top1"]])
                dv(lambda e: e.tensor_scalar(out=oh1[:, :], in0=elm[:, :], scalar1=V["top1"][:, 0:1], scalar2=None, op0=ALU.is_equal), [elm, V["top1"]], [oh1])
                dv(lambda e: e.scalar_tensor_tensor(out=elm[:, :], in0=oh1[:, :], scalar=-1e9, in1=elm[:, :], op0=ALU.mult, op1=ALU.add), [oh1, elm], [elm])
                dv(lambda e: e.tensor_reduce(out=V["top2"][:, :], in_=elm[:, :], axis=AX.X, op=ALU.max), [elm], [V["top2"]])
                dv(lambda e: e.tensor_scalar(out=oh2[:, :], in0=elm[:, :], scalar1=V["top2"][:, 0:1], scalar2=None, op0=ALU.is_equal), [elm, V["top2"]], [oh2])
                dv(lambda e: e.tensor_tensor(out=V["d"][:, :], in0=V["top2"][:, :], in1=V["top1"][:, :], op=ALU.subtract), [V["top1"], V["top2"]], [V["d"]])
                p.op("act", lambda e: e.activation(out=V["d"][:, :], in_=V["d"][:, :], func=AF.Exp), reads=[V["d"]], writes=[V["d"]])
                dv(lambda e: e.tensor_scalar(out=V["p1"][:, :], in0=V["d"][:, :], scalar1=1.0, scalar2=None, op0=ALU.add), [V["d"]], [V["p1"]])
                dv(lambda e: e.tensor_tensor(out=V["p1"][:, :], in0=V["p1"][:, :], in1=V["gsum"][:, :], op=ALU.mult), [V["p1"], V["gsum"]], [V["p1"]])
                dv(lambda e: e.reciprocal(out=V["p1"][:, :], in_=V["p1"][:, :]), [V["p1"]], [V["p1"]])
                dv(lambda e: e.tensor_tensor(out=V["p2"][:, :], in0=V["p1"][:, :], in1=V["d"][:, :], op=ALU.mult), [V["p1"], V["d"]], [V["p2"]])
                dv(lambda e: e.tensor_scalar(out=G[:, :], in0=oh1[:, :], scalar1=V["p1"][:, 0:1], scalar2=None, op0=ALU.mult), [oh1, V["p1"]], [G])
                dv(lambda e: e.scalar_tensor_tensor(out=G[:, :], in0=oh2[:, :], scalar=V["p2"][:, 0:1], in1=G[:, :], op0=ALU.mult, op1=ALU.add), [oh2, V["p2"], G], [G])
                dv(lambda e: e.tensor_tensor(out=prod[:, :, :], in0=esl[:, :, :], in1=G[:, :].unsqueeze(1).broadcast_to([128, NE, NEX]), op=ALU.mult), [esl, G], [prod])
                dv(lambda e: e.tensor_reduce(out=GL[:, :], in_=prod[:, :, :], axis=AX.X, op=ALU.add), [prod], [GL])
                dv(lambda e: e.tensor_tensor(out=gj[:, :], in0=goh[:, :], in1=gs[:, :], op=ALU.mult), [goh, gs], [gj])
                dv(lambda e: e.tensor_reduce(out=V["own"][:, :], in_=gj[:, :], axis=AX.X, op=ALU.add), [gj], [V["own"]])
                p.op("pe", lambda e: e.transpose(out=pgt[0:NE, :], in_=GL[:, :], identity=idf[:, :]), reads=[GL, idf], writes=[pgt])
                p.op("act", lambda e: e.activation(out=glt[:, :], in_=pgt[0:NE, :], func=AF.Copy), reads=[pgt], writes=[glt])
                p.dma("sp", GTt[:, i * 128:(i + 1) * 128], glt[:, :], reads=[glt])
                p.dma("sp", own_o[i * 128:(i + 1) * 128, :], V["own"][:, :], reads=[V["own"]])

        for le in range(NE):
            gemm_gated(p, lambda cc, nb, nw, le=le: HT4[nb // 128:(nb + nw) // 128, :, le * (FF // 128) + cc, :].rearrange("t p n -> p t n"),
                       xb(u3T), w_e1[le], w_e3[le], GTt[le], D, S, FF)
        prev = dscr("macc", [S, D], F32)
        gemm(p, prev, None, w_e2.rearrange("e f d -> (e f) d"), NE * FF, S, D, tm=True, add_tm=x2, dst_dt=F32,
             cblk=512, wbufs=1, nblk=128, x_ap_fn=lambda nb, nw: HT4[nb // 128])

        rmsnorm(p, prev, g_final, S, D, dstT=None, dst_tm=out, dst_tm_dt=F32)
        p.barrier()
        STATS.update(cnt=dict(p.cnt), nsem=p.nsem)
    return nc


def _bucket(d):
    d = np.maximum(d, 0)
    df = np.maximum(d, 1).astype(np.float32)
    large = 16 + (np.log(df / np.float32(16)) / np.float32(math.log(128 / 16)) * np.float32(16)).astype(np.int32)
    large = np.minimum(large, 31)
    return np.where(d < 16, d, large)


def _bias_tiles(rel_bias):
    s = np.arange(128)[:, None]
    t = np.arange(128)[None, :]
    sb = np.full((128, 16, 2, 128), NEG, np.float32)
    db = np.full((128, 16, 2, 128), NEG, np.float32)
    for j in range(2):
        dist = t + 128 - (j * 128 + s)
        valid = (dist >= 0) & (dist < 128)
        g = rel_bias[_bucket(dist)]
        for h in range(16):
            sb[:, h, j, :] = np.where(valid, g[:, :, h], np.float32(NEG))
        dist = t - s + (128 if j == 1 else 0)
        valid = dist >= 0
        g = rel_bias[_bucket(dist)]
        for h in range(16):
            db[:, h, j, :] = np.where(valid, g[:, :, 16 + h], np.float32(NEG))
    return sb, db


def make_in_maps(inputs, cfg, n_cores):
    f = lambda a: np.ascontiguousarray(np.asarray(a, dtype=np.float32))
    NE, EPG = cfg["NE"], cfg["EPG"]
    NEX = N_GROUPS * EPG
    rel_bias = f(inputs["rel_bias"])
    sb, db = _bias_tiles(rel_bias)
    cpb = cfg["cores_per_batch"]
    gpc = N_GROUPS // cpb
    shared = dict(
        rel31=f(rel_bias[31, 16:32]), sbias=sb, dbias=db,
        g_mix=f(inputs["g_mix"][0]), w_in=f(inputs["w_in"][0]), g_cq=f(inputs["g_cq"][0]), w_uq=f(inputs["w_uq"][0]),
        w_qidx=f(inputs["w_qidx"][0]), g_ckv=f(inputs["g_ckv"][0]), w_uk=f(inputs["w_uk"][0]).reshape(KV_RANK, 2048),
        w_uv=f(inputs["w_uv"][0]).reshape(KV_RANK, 2048), g_kidx=f(inputs["g_kidx"][0]), sink_a=f(inputs["sink_a"][0]),
        w_pa=f(inputs["w_pa"][0]), w_pb=f(inputs["w_pb"][0]), w_out=f(inputs["w_out"][0]), g_xattn=f(inputs["g_xattn"][0]),
        g_mem=f(inputs["g_mem"][0]), w_qm=f(inputs["w_qm"][0]), w_km=f(inputs["w_km"][0]), w_vm=f(inputs["w_vm"][0]),
        w_om=f(inputs["w_om"][0]), g_ffn=f(inputs["g_ffn"][0]), w_grp=f(inputs["w_grp"][0]), b_grp=f(inputs["b_grp"][0]),
        w_exp=f(inputs["w_exp"][0]), b_exp=f(inputs["b_exp"][0]), g_final=f(inputs["g_final"]),
    )
    maps = []
    for c in range(n_cores):
        b, pp = c // cpb, c % cpb
        e0 = pp * gpc * EPG
        gsel = np.zeros(N_GROUPS, np.float32)
        gsel[pp * gpc:(pp + 1) * gpc] = 1.0
        esel = np.zeros((NE, NEX), np.float32)
        esel[np.arange(NE), e0 + np.arange(NE)] = 1.0
        m = dict(shared)
        m.update(x=f(inputs["x"][b]), mem=f(inputs["mem"][b]), gsel=gsel, esel=esel,
                 w_e1=f(inputs["w_e1"][0][e0:e0 + NE]), w_e3=f(inputs["w_e3"][0][e0:e0 + NE]), w_e2=f(inputs["w_e2"][0][e0:e0 + NE]))
        maps.append(m)
    return maps


def assemble(results, cfg, n_cores, B, S, D):
    cpb = cfg["cores_per_batch"]
    out = np.zeros((B, S, D), np.float32)
    for c in range(n_cores):
        b = c // cpb
        own = results[c]["own"][:, 0] > 0.5
        out[b][own] = results[c]["out"][own]
    return out


STATS = {}
CFG = dict(D=4096, S=8192, ML=256, NE=16, EPG=8, FF=768, KEEP=256, NIT=26, cores_per_batch=4)


def kernel(**inputs):
    cfg = CFG
    nc = build(cfg)
    maps = make_in_maps(inputs, cfg, 8)
    res = run_bass_kernel_spmd(nc, maps, core_ids=list(range(8)))
    return assemble(res.results, cfg, 8, 2, cfg["S"], cfg["D"])
```

```python
import math
from contextlib import ExitStack
import numpy as np
import concourse.bass as bass
import concourse.mybir as mybir
from concourse.bass_utils import run_bass_kernel_spmd

F32 = mybir.dt.float32
BF16 = mybir.dt.bfloat16
AF = mybir.ActivationFunctionType
ALU = mybir.AluOpType
AX = mybir.AxisListType

HD = 128
A_HEADS, A_KV = 16, 4
B_HEADS = 16
Q_RANK, KV_RANK = 1024, 512
IDX_HEADS, IDX_DIM = 16, 128
MEM_HEADS = 4
N_GROUPS = 8
EPS = 1e-6
NEG = -30000.0
EPOCH = 30000
DMA_EPOCH = 1800


class Buf:
    __slots__ = ("w", "r")

    def __init__(self):
        self.w = None
        self.r = {}


class T:
    def __init__(self, t):
        self.t = t
        self.b = Buf()

    def __getitem__(self, k):
        return self.t[k]


class Prog:
    def __init__(self, nc, es):
        self.nc = nc
        self.es = es
        self.E = dict(pe=nc.tensor, act=nc.scalar, dve=nc.vector, pool=nc.gpsimd, sp=nc.sync)
        self.cs = {e: [] for e in self.E}
        self.cnt = {e: 0 for e in self.E}
        self.seen = {}
        self.nds = 24
        self.ds = [None] * self.nds
        self.dc = [0] * self.nds
        self.did = [0] * self.nds
        self.dn = 0
        self.nsem = 0
        self.live_d = {}

    def _newsem(self):
        self.nsem += 1
        return self.es.enter_context(self.nc.semaphore(f"s{self.nsem}"))

    def _csem(self, e, ep):
        while len(self.cs[e]) <= ep:
            self.cs[e].append(self._newsem())
        return self.cs[e][ep]

    def _wait(self, e, key, val):
        kind = key[0]
        if kind == "c" and key[1] == e and e == "pe":
            return
        k = (e, key)
        if self.seen.get(k, 0) >= val:
            return
        self.seen[k] = val
        if kind == "c":
            sem = self.cs[key[1]][key[2]]
        else:
            sem = self.live_d[key[1]]
        self.E[e].wait_ge(sem, val)

    def _deps(self, e, reads, writes):
        for b in reads:
            b = b.b if isinstance(b, T) else b
            if b.w is not None:
                self._wait(e, *b.w)
        for b in writes:
            b = b.b if isinstance(b, T) else b
            if b.w is not None:
                self._wait(e, *b.w)
            for key, val in b.r.items():
                self._wait(e, key, val)

    def _mark(self, tok, reads, writes):
        key, val = tok
        for b in reads:
            b = b.b if isinstance(b, T) else b
            if b.r.get(key, 0) < val:
                b.r[key] = val
        for b in writes:
            b = b.b if isinstance(b, T) else b
            b.w = tok
            b.r = {}

    def op(self, e, fn, reads=(), writes=()):
        self._deps(e, reads, writes)
        ins = fn(self.E[e])
        i = self.cnt[e]
        self.cnt[e] += 1
        ep, v = i // EPOCH, i % EPOCH + 1
        ins.then_inc(self._csem(e, ep), 1)
        tok = (("c", e, ep), v)
        self._mark(tok, reads, writes)
        return tok

    def dma(self, q, out, in_, reads=(), writes=(), **kw):
        k = self.dn
        self.dn = (k + 1) % self.nds
        if self.ds[k] is not None and self.dc[k] > 0:
            self._wait(q, ("d", self.did[k]), 16 * self.dc[k])
        if self.ds[k] is None or self.dc[k] >= DMA_EPOCH:
            self.ds[k] = self._newsem()
            self.dc[k] = 0
            self.did[k] = self.nsem
            self.live_d[self.nsem] = self.ds[k]
        self._deps(q, reads, writes)
        ins = self.E[q].dma_start(out=out, in_=in_, **kw)
        self.dc[k] += 1
        ins.then_inc(self.ds[k], 16)
        tok = (("d", self.did[k]), 16 * self.dc[k])
        self._mark(tok, reads, writes)
        return tok

    def barrier(self):
        for e in self.E:
            for f in self.E:
                if f == e or self.cnt[f] == 0:
                    continue
                i = self.cnt[f] - 1
                self._wait(e, ("c", f, i // EPOCH), i % EPOCH + 1)
            for k in range(self.nds):
                if self.ds[k] is not None and self.dc[k] > 0:
                    self._wait(e, ("d", self.did[k]), 16 * self.dc[k])


class Stage:
    uid = 0

    def __init__(self, p):
        self.p = p
        self.es = ExitStack()
        self.n = 0

    def __enter__(self):
        self.es.__enter__()
        return self

    def __exit__(self, *a):
        self.p.barrier()
        return self.es.__exit__(*a)

    def sb(self, shape, dt, name="t"):
        Stage.uid += 1
        return T(self.es.enter_context(self.p.nc.sbuf_tensor(f"{name}_{Stage.uid}", list(shape), dt)))

    def ps(self, shape, dt=F32, name="p"):
        Stage.uid += 1
        return T(self.es.enter_context(self.p.nc.psum_tensor(f"{name}_{Stage.uid}", list(shape), dt)))


def kc_view(ap):
    return ap.rearrange("(kc p) n -> p kc n", p=128)


def gemm(p, dst, srcT, w, K, N, cols, tm, act=None, mulT=None, addT=None, add_tm=None,
         rowscale=None, dst_dt=BF16, cblk=512, wbufs=2, nblk=512, x_ap_fn=None):
    KC = K // 128
    with Stage(p) as s:
        wt = [s.sb([128, KC, cblk], BF16, "wt") for _ in range(wbufs)]
        xt = [s.sb([128, KC, nblk], BF16, "xt") for _ in range(2)]
        ps = [s.ps([128, 512]) for _ in range(2)]
        ot = [s.sb([128, 512], dst_dt, "ot") for _ in range(2)]
        t1 = [s.sb([128, 512], F32, "t1") for _ in range(2)]
        mt = [s.sb([128, 512], BF16, "mt") for _ in range(2)]
        at = [s.sb([128, 512], BF16 if not tm else F32, "at") for _ in range(2)]
        rs = [s.sb([128, 1], F32, "rs") for _ in range(2)]
        wv = kc_view(w)
        xv = kc_view(srcT) if x_ap_fn is None else None
        it = 0
        iw = 0
        ix = 0
        for cb in range(0, cols, cblk):
            cw = min(cblk, cols - cb)
            W = wt[iw % wbufs]
            iw += 1
            for c5 in range(0, cw, 512):
                c5w = min(512, cw - c5)
                p.dma("pool", W[:, :, c5:c5 + c5w], wv[:, :, cb + c5:cb + c5 + c5w], writes=[W])
            for nb in range(0, N, nblk):
                nw = min(nblk, N - nb)
                X = xt[ix % 2]
                ix += 1
                p.dma("sp", X[:, :, :nw], xv[:, :, nb:nb + nw] if x_ap_fn is None else x_ap_fn(nb, nw), writes=[X])
                if not tm:
                    for cc in range(cw // 128):
                        P_, O, T1, M, A = ps[it % 2], ot[it % 2], t1[it % 2], mt[it % 2], at[it % 2]
                        it += 1
                        r0 = cb + cc * 128
                        for kc in range(KC):
                            p.op("pe", lambda e, kc=kc: e.matmul(P_[:, :nw], lhsT=W[:, kc, cc * 128:(cc + 1) * 128],
                                                                  rhs=X[:, kc, :nw], start=(kc == 0), stop=(kc == KC - 1)),
                                 reads=[W, X], writes=[P_])
                        f = act if act is not None else AF.Copy
                        if mulT is None and addT is None:
                            p.op("act", lambda e: e.activation(out=O[:, :nw], in_=P_[:, :nw], func=f), reads=[P_], writes=[O])
                        else:
                            p.op("act", lambda e: e.activation(out=T1[:, :nw], in_=P_[:, :nw], func=f), reads=[P_], writes=[T1])
                            if mulT is not None:
                                p.dma("sp", M[:, :nw], mulT[r0:r0 + 128, nb:nb + nw], writes=[M])
                                tgt = O if addT is None else T1
                                p.op("dve", lambda e: e.tensor_tensor(out=tgt[:, :nw], in0=T1[:, :nw], in1=M[:, :nw], op=ALU.mult),
                                     reads=[T1, M], writes=[tgt])
                            if addT is not None:
                                p.dma("sp", A[:, :nw], addT[r0:r0 + 128, nb:nb + nw], writes=[A])
                                p.op("dve", lambda e: e.tensor_tensor(out=O[:, :nw], in0=T1[:, :nw], in1=A[:, :nw], op=ALU.add),
                                     reads=[T1, A], writes=[O])
                        p.dma("sp", dst[r0:r0 + 128, nb:nb + nw], O[:, :nw], reads=[O])
                else:
                    for ti in range(nw // 128):
                        t0 = nb + ti * 128
                        for c5 in range(0, cw, 512):
                            c5w = min(512, cw - c5)
                            P_, O, A, R = ps[it % 2], ot[it % 2], at[it % 2], rs[it % 2]
                            it += 1
                            for kc in range(KC):
                                p.op("pe", lambda e, kc=kc: e.matmul(P_[:, :c5w], lhsT=X[:, kc, ti * 128:(ti + 1) * 128],
                                                                      rhs=W[:, kc, c5:c5 + c5w], start=(kc == 0), stop=(kc == KC - 1)),
                                     reads=[W, X], writes=[P_])
                            if add_tm is not None:
                                p.dma("sp", A[:, :c5w], add_tm[t0:t0 + 128, cb + c5:cb + c5 + c5w], writes=[A])
                            if rowscale is not None:
                                p.dma("sp", R[:, :], rowscale[t0:t0 + 128, :], writes=[R], allow_slow_non_contiguous=True)
                                p.op("dve", lambda e: e.scalar_tensor_tensor(out=O[:, :c5w], in0=P_[:, :c5w], scalar=R[:, 0:1], in1=A[:, :c5w],
                                                                             op0=ALU.mult, op1=ALU.add), reads=[P_, R, A], writes=[O])
                            elif add_tm is not None:
                                p.op("dve", lambda e: e.tensor_tensor(out=O[:, :c5w], in0=P_[:, :c5w], in1=A[:, :c5w], op=ALU.add),
                                     reads=[P_, A], writes=[O])
                            else:
                                p.op("act", lambda e: e.activation(out=O[:, :c5w], in_=P_[:, :c5w], func=AF.Copy), reads=[P_], writes=[O])
                            p.dma("sp", dst[t0:t0 + 128, cb + c5:cb + c5 + c5w], O[:, :c5w], reads=[O])


def gemm_gated(p, dst_fn, x_ap_fn, w1, w3, gT_row, K, N, FF):
    KC = K // 128
    with Stage(p) as s:
        W1 = s.sb([128, KC, FF], BF16, "w1")
        W3 = s.sb([128, KC, FF], BF16, "w3")
        xt = [s.sb([128, KC, 512], BF16, "xt") for _ in range(2)]
        gb = [s.sb([128, 512], F32, "gb") for _ in range(2)]
        ps1 = [s.ps([128, 512]) for _ in range(2)]
        ps3 = [s.ps([128, 512]) for _ in range(2)]
        t1 = [s.sb([128, 512], F32, "t1") for _ in range(2)]
        ot = [s.sb([128, 512], BF16, "ot") for _ in range(2)]
        for c5 in range(0, FF, 512):
            c5w = min(512, FF - c5)
            p.dma("pool", W1[:, :, c5:c5 + c5w], kc_view(w1)[:, :, c5:c5 + c5w], writes=[W1])
            p.dma("pool", W3[:, :, c5:c5 + c5w], kc_view(w3)[:, :, c5:c5 + c5w], writes=[W3])
        it = 0
        for ib, nb in enumerate(range(0, N, 512)):
            nw = min(512, N - nb)
            X, GB = xt[ib % 2], gb[ib % 2]
            p.dma("sp", X[:, :, :nw], x_ap_fn(nb, nw), writes=[X])
            p.dma("sp", GB[:, :nw], gT_row[nb:nb + nw].partition_broadcast(128), writes=[GB])
            for cc in range(FF // 128):
                P1, P3, T1, O = ps1[it % 2], ps3[it % 2], t1[it % 2], ot[it % 2]
                it += 1
                for kc in range(KC):
                    p.op("pe", lambda e, kc=kc: e.matmul(P1[:, :nw], lhsT=W1[:, kc, cc * 128:(cc + 1) * 128], rhs=X[:, kc, :nw],
                                                          start=(kc == 0), stop=(kc == KC - 1)), reads=[W1, X], writes=[P1])
                for kc in range(KC):
                    p.op("pe", lambda e, kc=kc: e.matmul(P3[:, :nw], lhsT=W3[:, kc, cc * 128:(cc + 1) * 128], rhs=X[:, kc, :nw],
                                                          start=(kc == 0), stop=(kc == KC - 1)), reads=[W3, X], writes=[P3])
                p.op("act", lambda e: e.activation(out=T1[:, :nw], in_=P1[:, :nw], func=AF.Silu), reads=[P1], writes=[T1])
                p.op("dve", lambda e: e.tensor_tensor(out=T1[:, :nw], in0=T1[:, :nw], in1=P3[:, :nw], op=ALU.mult), reads=[T1, P3], writes=[T1])
                p.op("dve", lambda e: e.tensor_tensor(out=O[:, :nw], in0=T1[:, :nw], in1=GB[:, :nw], op=ALU.mult), reads=[T1, GB], writes=[O])
                p.dma("sp", dst_fn(cc, nb, nw), O[:, :nw].rearrange("p (t n) -> p t n", n=128), reads=[O])


def rmsnorm(p, src, g, n, W, dstT=None, dst_tm=None, dst_tm_dt=BF16, dstB=None):
    KC = W // 128
    with Stage(p) as s:
        ident = s.sb([128, 128], BF16, "id")
        idf = s.sb([128, 128], F32, "idf")
        p.op("pool", lambda e: e.memset(idf[:, :], 1.0), writes=[idf])
        p.op("pool", lambda e: e.affine_select(out=idf[:, :], in_=idf[:, :], pattern=[[-1, 128]], compare_op=ALU.is_equal,
                                               fill=0.0, base=0, channel_multiplier=1), reads=[idf], writes=[idf])
        p.op("dve", lambda e: e.tensor_copy(out=ident[:, :], in_=idf[:, :]), reads=[idf], writes=[ident])
        gt = s.sb([128, W], F32, "g")
        p.dma("sp", gt[:, :], g.partition_broadcast(128), writes=[gt])
        xt = [s.sb([128, W], F32, "x") for _ in range(2)]
        junk = s.sb([128, W], BF16, "junk")
        un = [s.sb([128, W], dst_tm_dt if dst_tm is not None else BF16, "un") for _ in range(2)]
        unb = [s.sb([128, W], BF16, "unb") for _ in range(2)] if (dst_tm is not None and dst_tm_dt != BF16 and (dstT is not None or dstB is not None)) else None
        ss = [s.sb([128, 1], F32, "ss") for _ in range(2)]
        rstd = [s.sb([128, 1], F32, "rstd") for _ in range(2)]
        uT = [s.sb([128, KC, 128], BF16, "uT") for _ in range(2)]
        pt = [s.ps([128, 512], BF16, "pt") for _ in range(2)]
        ip = 0
        for i in range(n // 128):
            X, U, SS, R, UT = xt[i % 2], un[i % 2], ss[i % 2], rstd[i % 2], uT[i % 2]
            p.dma("sp", X[:, :], src[i * 128:(i + 1) * 128, :], writes=[X])
            p.op("act", lambda e: e.activation(out=junk[:, :], in_=X[:, :], func=AF.Square, accum_out=SS[:, 0:1]),
                 reads=[X], writes=[junk, SS])
            p.op("dve", lambda e: e.tensor_scalar(out=R[:, :], in0=SS[:, :], scalar1=1.0 / W, scalar2=EPS, op0=ALU.mult, op1=ALU.add),
                 reads=[SS], writes=[R])
            p.op("act", lambda e: e.activation(out=R[:, :], in_=R[:, :], func=AF.Sqrt), reads=[R], writes=[R])
            p.op("dve", lambda e: e.reciprocal(out=R[:, :], in_=R[:, :]), reads=[R], writes=[R])
            p.op("dve", lambda e: e.scalar_tensor_tensor(out=U[:, :], in0=X[:, :], scalar=R[:, 0:1], in1=gt[:, :], op0=ALU.mult, op1=ALU.mult),
                 reads=[X, R, gt], writes=[U])
            if dst_tm is not None:
                p.dma("sp", dst_tm[i * 128:(i + 1) * 128, :], U[:, :], reads=[U])
            if dstT is None and dstB is None:
                continue
            UB = U
            if unb is not None:
                UB = unb[i % 2]
                p.op("act", lambda e: e.activation(out=UB[:, :], in_=U[:, :], func=AF.Copy), reads=[U], writes=[UB])
            for c0 in range(0, KC, 4):
                PT = pt[ip % 2]
                ip += 1
                nc_ = min(4, KC - c0)
                for j in range(nc_):
                    c = c0 + j
                    p.op("pe", lambda e, c=c, j=j: e.transpose(out=PT[:, j * 128:(j + 1) * 128], in_=UB[:, c * 128:(c + 1) * 128],
                                                               identity=ident[:, :]), reads=[UB, ident], writes=[PT])
                p.op("dve", lambda e: e.tensor_copy(out=UT[:, c0:c0 + nc_, :], in_=PT[:, :nc_ * 128].rearrange("p (c n) -> p c n", n=128)),
                     reads=[PT], writes=[UT])
            if dstB is not None:
                p.dma("sp", dstB[i // 4, :, :, (i % 4) * 128:(i % 4 + 1) * 128], UT[:, :, :], reads=[UT])
            else:
                p.dma("sp", kc_view(dstT)[:, :, i * 128:(i + 1) * 128], UT[:, :, :], reads=[UT])


def attention(p, s, S, outT, qT, kT, vtm, nheads, gqa, kbs_of, bias_of, mask_of, esink, scale, pre_qb=None, pe_bias=None, hg_hook=None, post_qb=None):
    ones = s.sb([128, 128], BF16, "ones")
    p.op("pool", lambda e: e.memset(ones[:, :], 1.0), writes=[ones])
    qq = [s.sb([128, nheads, 128], BF16, "qq") for _ in range(2)]
    KCH = 8
    kk = [s.sb([128, 4 if gqa == 1 else 1, KCH * 128], BF16, "kk") for _ in range(2)]
    vv = [s.sb([128, KCH, 4 if gqa == 1 else 1, 128], BF16, "vv") for _ in range(2)]
    psl = [s.ps([128, 512], F32, "psl") for _ in range(2)]
    pso = [s.ps([128, 512], F32, "pso") for _ in range(4)]
    psd = s.ps([128, 512], F32, "psd")
    tmp = [s.sb([128, 512], F32, "tmp") for _ in range(2)]
    eT = [s.sb([128, 512], BF16, "eT") for _ in range(2)]
    rec = s.sb([128, 512], F32, "rec")
    oT = [s.sb([128, 4, 128], BF16, "oT") for _ in range(2)]
    qv = qT.rearrange("(h d) n -> d h n", d=128)
    kv = kT.rearrange("(h d) n -> d h n", d=128)
    vvw = vtm.rearrange("(kb s) (h d) -> s kb h d", s=128, d=128)
    ov = outT.rearrange("(h d) n -> d h n", d=128)
    it = 0
    ik = 0
    io = 0
    pending = [None]
    if pre_qb is not None:
        pre_qb(psl)
    for qb in range(S // 128):
        Q = qq[qb % 2]
        p.dma("sp", Q[:, :, :], qv[:, :, qb * 128:(qb + 1) * 128], writes=[Q])
        kbs = kbs_of(qb)
        for hg in range(nheads // 4):
            h0 = hg * 4
            first = True
            if hg_hook is not None:
                hg_hook(qb, hg, psl)
            for c0 in range(0, len(kbs), KCH):
                ch = kbs[c0:c0 + KCH]
                kb0, nkb = ch[0], len(ch)
                KK, VV = kk[ik % 2], vv[ik % 2]
                ik += 1
                if gqa == 1:
                    p.dma("sp", KK[:, :, :nkb * 128], kv[:, h0:h0 + 4, kb0 * 128:(kb0 + nkb) * 128], writes=[KK])
                    p.dma("sp", VV[:, :nkb, :, :], vvw[:, kb0:kb0 + nkb, h0:h0 + 4, :], writes=[VV])
                else:
                    p.dma("sp", KK[:, :, :nkb * 128], kv[:, hg:hg + 1, kb0 * 128:(kb0 + nkb) * 128], writes=[KK])
                    p.dma("sp", VV[:, :nkb, :, :], vvw[:, kb0:kb0 + nkb, hg:hg + 1, :], writes=[VV])
                for j, kb in enumerate(ch):
                    last = (c0 + j == len(kbs) - 1)
                    PL, TM, E = psl[it % 2], tmp[it % 2], eT[it % 2]
                    it += 1
                    bz = bias_of(qb, kb, h0) if bias_of is not None else None
                    mk = mask_of(qb, kb) if mask_of is not None else None
                    if pe_bias is not None:
                        kind, bap, bbuf = bz
                        map_, mbuf = mk
                        mrep = map_.unsqueeze(1).broadcast_to([128, 4, 128])
                        if kind == "tile":
                            p.op("pe", lambda e: e.matmul(PL[:, :], lhsT=pe_bias["ident"][:, :], rhs=bap, start=True, stop=False, skip_group_check=True),
                                 reads=[pe_bias["ident"], bbuf], writes=[PL])
                        p.op("pe", lambda e: e.matmul(PL[:, :].rearrange("p (h n) -> p h n", n=128), lhsT=pe_bias["ident"][:, :], rhs=mrep,
                                                      start=(kind != "tile"), stop=False, skip_group_check=True),
                             reads=[pe_bias["ident"], mbuf], writes=[PL])
                        for hh in range(4):
                            p.op("pe", lambda e, hh=hh: e.matmul(PL[:, hh * 128:(hh + 1) * 128], lhsT=KK[:, hh, j * 128:(j + 1) * 128],
                                                                 rhs=Q[:, h0 + hh, :], start=False, stop=(hh == 3), skip_group_check=True),
                                 reads=[KK, Q], writes=[PL])
                        if kind == "tile":
                            p.op("act", lambda e: e.activation(out=E[:, :], in_=PL[:, :], func=AF.Exp, scale=scale), reads=[PL], writes=[E])
                        else:
                            for hh in range(4):
                                p.op("act", lambda e, hh=hh: e.activation(out=E[:, hh * 128:(hh + 1) * 128], in_=PL[:, hh * 128:(hh + 1) * 128], func=AF.Exp,
                                                                          scale=scale, bias=bap[:, h0 + hh:h0 + hh + 1]), reads=[PL, bbuf], writes=[E])
                    else:
                        for hh in range(4):
                            kh = hh if gqa == 1 else 0
                            p.op("pe", lambda e, hh=hh, kh=kh: e.matmul(PL[:, hh * 128:(hh + 1) * 128], lhsT=KK[:, kh, j * 128:(j + 1) * 128],
                                                                        rhs=Q[:, h0 + hh, :], start=True, stop=True),
                                 reads=[KK, Q], writes=[PL])
                        if bz is None and mk is None:
                            p.op("act", lambda e: e.activation(out=E[:, :], in_=PL[:, :], func=AF.Exp, scale=scale), reads=[PL], writes=[E])
                        else:
                            bap, bbuf = bz
                            p.op("dve", lambda e: e.scalar_tensor_tensor(out=TM[:, :].rearrange("p (h n) -> p h n", n=128),
                                                                         in0=PL[:, :].rearrange("p (h n) -> p h n", n=128), scalar=scale, in1=bap,
                                                                         op0=ALU.mult, op1=ALU.add), reads=[PL, bbuf], writes=[TM])
                            p.op("act", lambda e: e.activation(out=E[:, :], in_=TM[:, :], func=AF.Exp), reads=[TM], writes=[E])

                    def pv(E=E, VV=VV, j=j, first=first, last=last):
                        for hh in range(4):
                            kh = hh if gqa == 1 else 0
                            PO = pso[hh]
                            p.op("pe", lambda e, hh=hh, kh=kh, PO=PO: e.matmul(PO[:, :128], lhsT=VV[:, j, kh, :], rhs=E[:, hh * 128:(hh + 1) * 128],
                                                                               start=first, stop=last), reads=[VV, E], writes=[PO])
                        p.op("pe", lambda e: e.matmul(psd[:, :], lhsT=ones[:, :], rhs=E[:, :], start=first, stop=last),
                             reads=[ones, E], writes=[psd])

                    if pending[0] is not None:
                        pending[0]()
                    pending[0] = pv
                    first = False
            if pending[0] is not None:
                pending[0]()
                pending[0] = None
            O = oT[io % 2]
            io += 1
            if esink is not None:
                for hh in range(4):
                    p.op("dve", lambda e, hh=hh: e.tensor_scalar(out=rec[:, hh * 128:(hh + 1) * 128], in0=psd[:, hh * 128:(hh + 1) * 128],
                                                                 scalar1=esink[:, h0 + hh:h0 + hh + 1], scalar2=None, op0=ALU.add),
                         reads=[psd, esink], writes=[rec])
                p.op("dve", lambda e: e.reciprocal(out=rec[:, :], in_=rec[:, :]), reads=[rec], writes=[rec])
            else:
                p.op("dve", lambda e: e.reciprocal(out=rec[:, :], in_=psd[:, :]), reads=[psd], writes=[rec])
            for hh in range(4):
                PO = pso[hh]
                p.op("dve", lambda e, hh=hh, PO=PO: e.tensor_tensor(out=O[:, hh, :], in0=PO[:, :128], in1=rec[:, hh * 128:(hh + 1) * 128], op=ALU.mult),
                     reads=[PO, rec], writes=[O])
            p.dma("sp", ov[:, h0:h0 + 4, qb * 128:(qb + 1) * 128], O[:, :, :], reads=[O])
        if post_qb is not None:
            post_qb(qb)


IN_OFF = {}
_o = 0
for _n, _w in (("q_a", 2048), ("k_a", 512), ("v_a", 512), ("c_q", 1024), ("c_kv", 512), ("k_i", 128), ("w_i", 16),
               ("gate_a", None), ("gate_b", None)):
    IN_OFF[_n] = _o
    if _w is not None:
        _o += _w


def xb(B):
    return lambda nb, nw: B[nb // 512][:, :, :nw]


def build(cfg):
    D, S, ML, NE, EPG, FF, KEEP, NIT = cfg["D"], cfg["S"], cfg["ML"], cfg["NE"], cfg["EPG"], cfg["FF"], cfg["KEEP"], cfg["NIT"]
    NEX = N_GROUPS * EPG
    INW = 4752 + 2 * D
    OFF_GA, OFF_GB = 4752, 4752 + D
    NQB = S // 128
    nc = bass.Bass("TRN2", target_bir_lowering=False)

    def din(name, shape, dt=F32):
        return nc.dram_tensor(name, list(shape), dt, kind="ExternalInput").ap()

    def dscr(name, shape, dt):
        return nc.dram_tensor(name, list(shape), dt, kind="Internal").ap()

    x = din("x", [S, D]); mem = din("mem", [ML, D])
    rel31 = din("rel31", [16]); sbias = din("sbias", [128, 16, 2, 128]); dbias = din("dbias", [128, 16, 2, 128])
    g_mix = din("g_mix", [D]); w_in = din("w_in", [D, INW]); g_cq = din("g_cq", [Q_RANK]); w_uq = din("w_uq", [Q_RANK, 2048])
    w_qidx = din("w_qidx", [Q_RANK, 2048]); g_ckv = din("g_ckv", [KV_RANK]); w_uk = din("w_uk", [KV_RANK, 2048])
    w_uv = din("w_uv", [KV_RANK, 2048]); g_kidx = din("g_kidx", [IDX_DIM]); sink_a = din("sink_a", [16])
    w_pa = din("w_pa", [2048, D]); w_pb = din("w_pb", [2048, D]); w_out = din("w_out", [D, D])
    g_xattn = din("g_xattn", [D]); g_mem = din("g_mem", [D]); w_qm = din("w_qm", [D, 512]); w_km = din("w_km", [D, 512])
    w_vm = din("w_vm", [D, 512]); w_om = din("w_om", [512, D]); g_ffn = din("g_ffn", [D])
    w_grp = din("w_grp", [D, N_GROUPS]); b_grp = din("b_grp", [N_GROUPS]); w_exp = din("w_exp", [D, NEX]); b_exp = din("b_exp", [NEX])
    w_e1 = din("w_e1", [NE, D, FF]); w_e3 = din("w_e3", [NE, D, FF]); w_e2 = din("w_e2", [NE, FF, D]); g_final = din("g_final", [D])
    gsel = din("gsel", [N_GROUPS])
    esel = din("esel", [NE, NEX])
    out = nc.dram_tensor("out", [S, D], F32, kind="ExternalOutput").ap()
    own_o = nc.dram_tensor("own", [S, 1], F32, kind="ExternalOutput").ap()

    uT = dscr("uT", [S // 512, 128, D // 128, 512], BF16)
    qkT = dscr("qkT", [2560, S], BF16)
    sgT = dscr("sgT", [2 * D, S], BF16)
    vaTM = dscr("vaTM", [S, 512], BF16)
    tmA = dscr("tmA", [S, 1680], F32)
    cqT = dscr("cqT", [S // 512, 128, Q_RANK // 128, 512], BF16)
    ckvT = dscr("ckvT", [S // 512, 128, KV_RANK // 128, 512], BF16)
    kiT = dscr("kiT", [IDX_DIM, S], BF16)
    qbT = dscr("qbT", [2048, S], BF16)
    qiT = dscr("qiT", [2048, S], BF16)
    kbT = dscr("kbT", [2048, S], BF16)
    vbTM = dscr("vbTM", [S, 2048], BF16)
    oaT = dscr("oaT", [2048, S], BF16)
    obT = dscr("obT", [2048, S], BF16)
    y1T = dscr("y1T", [D, S], BF16)
    yT = dscr("yT", [D, S], BF16)
    x1 = dscr("x1", [S, D], F32)
    u2T = dscr("u2T", [S // 512, 128, D // 128, 512], BF16)
    memT = dscr("memT", [D, ML], BF16)
    qmT = dscr("qmT", [512, S], BF16)
    kmT = dscr("kmT", [512, ML], BF16)
    vmTM = dscr("vmTM", [ML, 512], BF16)
    omT = dscr("omT", [512, S], BF16)
    x2 = dscr("x2", [S, D], F32)
    u3T = dscr("u3T", [S // 512, 128, D // 128, 512], BF16)
    rl = dscr("rl", [S, N_GROUPS + NEX], F32)
    GTt = dscr("GTt", [NE, S], F32)
    HT4 = dscr("HT4", [S // 128, 128, NE * FF // 128, 128], BF16)

    with ExitStack() as es:
        p = Prog(nc, es)
        scale = HD ** -0.5

        rmsnorm(p, x, g_mix, S, D, dstB=uT)
        gemm(p, qkT, None, w_in[:, 0:2560], D, S, 2560, tm=False, cblk=1024, wbufs=1, x_ap_fn=xb(uT))
        gemm(p, sgT, None, w_in[:, OFF_GA:OFF_GA + 2 * D], D, S, 2 * D, tm=False, act=AF.Sigmoid, cblk=1024, wbufs=1, x_ap_fn=xb(uT))
        gemm(p, vaTM, None, w_in[:, 2560:3072], D, S, 512, tm=True, dst_dt=BF16, x_ap_fn=xb(uT))
        gemm(p, tmA, None, w_in[:, 3072:4752], D, S, 1680, tm=True, dst_dt=F32, cblk=1024, wbufs=1, x_ap_fn=xb(uT))
        rmsnorm(p, tmA[:, 0:1024], g_cq, S, 1024, dstB=cqT)
        rmsnorm(p, tmA[:, 1024:1536], g_ckv, S, 512, dstB=ckvT)
        rmsnorm(p, tmA[:, 1536:1664], g_kidx, S, 128, dstT=kiT)
        gemm(p, qbT, None, w_uq, Q_RANK, S, 2048, tm=False, x_ap_fn=xb(cqT))
        gemm(p, qiT, None, w_qidx, Q_RANK, S, 2048, tm=False, x_ap_fn=xb(cqT))
        gemm(p, kbT, None, w_uk, KV_RANK, S, 2048, tm=False, x_ap_fn=xb(ckvT))
        gemm(p, vbTM, None, w_uv, KV_RANK, S, 2048, tm=True, dst_dt=BF16, x_ap_fn=xb(ckvT))

        with Stage(p) as s:
            sb_ = s.sb([128, 16, 2, 128], F32, "sbias")
            p.dma("sp", sb_[:, :, :, :], sbias, writes=[sb_])
            es_ = s.sb([128, 16], F32, "esink")
            p.dma("sp", es_[:, :], sink_a.partition_broadcast(128), writes=[es_])
            p.op("act", lambda e: e.activation(out=es_[:, :], in_=es_[:, :], func=AF.Exp), reads=[es_], writes=[es_])

            def kbs_a(qb):
                return [qb] if qb == 0 else [qb - 1, qb]

            def bias_a(qb, kb, h0):
                j = 1 if kb == qb else 0
                return sb_[:, h0:h0 + 4, j, :], sb_

            attention(p, s, S, oaT, qkT[0:2048, :], qkT[2048:2560, :], vaTM, 16, 4, kbs_a, bias_a, None, es_, scale)

        with Stage(p) as s:
            inv = 1.0 / scale
            db_ = s.sb([128, 16, 2, 128], F32, "dbias")
            p.dma("sp", db_[:, :, :, :], dbias, writes=[db_])
            dbb = s.sb([128, 16, 2, 128], BF16, "dbb")
            p.op("dve", lambda e: e.tensor_scalar(out=dbb[:, :, :, :], in0=db_[:, :, :, :], scalar1=inv, scalar2=None, op0=ALU.mult),
                 reads=[db_], writes=[dbb])
            dbd = s.sb([128, 16, 128], BF16, "dbd")
            dba = s.sb([128, 16, 128], BF16, "dba")
            p.op("dve", lambda e: e.tensor_copy(out=dbd[:, :, :], in_=dbb[:, :, 0, :]), reads=[dbb], writes=[dbd])
            p.op("dve", lambda e: e.tensor_copy(out=dba[:, :, :], in_=dbb[:, :, 1, :]), reads=[dbb], writes=[dba])
            r31 = s.sb([128, 16], F32, "r31")
            p.dma("sp", r31[:, :], rel31.partition_broadcast(128), writes=[r31])
            ident = s.sb([128, 128], BF16, "id")
            idf = s.sb([128, 128], F32, "idf")
            p.op("pool", lambda e: e.memset(idf[:, :], 1.0), writes=[idf])
            p.op("pool", lambda e: e.affine_select(out=idf[:, :], in_=idf[:, :], pattern=[[-1, 128]], compare_op=ALU.is_equal,
                                                   fill=0.0, base=0, channel_multiplier=1), reads=[idf], writes=[idf])
            p.op("dve", lambda e: e.tensor_copy(out=ident[:, :], in_=idf[:, :]), reads=[idf], writes=[ident])
            cneg = s.sb([128, 128], F32, "cneg")
            p.op("pool", lambda e: e.memset(cneg[:, :], 0.0), writes=[cneg])
            p.op("pool", lambda e: e.affine_select(out=cneg[:, :], in_=cneg[:, :], pattern=[[-1, 128]], compare_op=ALU.is_ge,
                                                   fill=-1e30, base=0, channel_multiplier=1), reads=[cneg], writes=[cneg])
            kis = s.sb([128, S], BF16, "kis")
            p.dma("sp", kis[:, :], kiT, writes=[kis])
            acc = s.sb([128, S], F32, "acc")
            mk = s.sb([128, S], BF16, "mk")
            mkT = [s.sb([128, S], BF16, "mkT") for _ in range(2)]
            qi = [s.sb([128, 16, 128], BF16, "qi") for _ in range(2)]
            wi = [s.sb([128, 16], F32, "wi") for _ in range(2)]
            rl_ = [s.sb([128, 512], F32, "relu") for _ in range(2)]
            ptx = s.ps([128, 512], BF16, "ptx")
            sm = {n: s.sb([128, 1], F32, n) for n in ("lo", "hi", "mid", "cnt", "ge", "t1")}
            qiv = qiT.rearrange("(h d) n -> d h n", d=128)
            cnt_i = [0]

            def indexer(qb, psl):
                nk = (qb + 1) * 128
                QI, WI = qi[qb % 2], wi[qb % 2]
                p.dma("sp", QI[:, :, :], qiv[:, :, qb * 128:(qb + 1) * 128], writes=[QI])
                p.dma("sp", WI[:, :], tmA[qb * 128:(qb + 1) * 128, 1664:1680], writes=[WI])
                for c in range(0, nk, 512):
                    cw = min(512, nk - c)
                    for h in range(16):
                        R = rl_[cnt_i[0] % 2]
                        PX = psl[cnt_i[0] % 2]
                        cnt_i[0] += 1
                        p.op("pe", lambda e, h=h: e.matmul(PX[:, :cw], lhsT=QI[:, h, :], rhs=kis[:, c:c + cw], start=True, stop=True),
                             reads=[QI, kis], writes=[PX])
                        p.op("act", lambda e: e.activation(out=R[:, :cw], in_=PX[:, :cw], func=AF.Relu), reads=[PX], writes=[R])
                        if h == 0:
                            p.op("dve", lambda e, h=h: e.tensor_scalar(out=acc[:, c:c + cw], in0=R[:, :cw], scalar1=WI[:, h:h + 1], scalar2=None,
                                                                       op0=ALU.mult), reads=[R, WI], writes=[acc])
                        else:
                            p.op("dve", lambda e, h=h: e.scalar_tensor_tensor(out=acc[:, c:c + cw], in0=R[:, :cw], scalar=WI[:, h:h + 1],
                                                                              in1=acc[:, c:c + cw], op0=ALU.mult, op1=ALU.add),
                                 reads=[R, WI, acc], writes=[acc])
                lo, hi, mid, cnt, ge, t1 = (sm[n] for n in ("lo", "hi", "mid", "cnt", "ge", "t1"))
                p.op("dve", lambda e: e.tensor_reduce(out=lo[:, :], in_=acc[:, :nk], axis=AX.X, op=ALU.min), reads=[acc], writes=[lo])
                p.op("dve", lambda e: e.tensor_reduce(out=hi[:, :], in_=acc[:, :nk], axis=AX.X, op=ALU.max), reads=[acc], writes=[hi])
                p.op("dve", lambda e: e.tensor_scalar(out=lo[:, :], in0=lo[:, :], scalar1=-1.0, scalar2=None, op0=ALU.add), reads=[lo], writes=[lo])
                p.op("dve", lambda e: e.tensor_scalar(out=hi[:, :], in0=hi[:, :], scalar1=1.0, scalar2=None, op0=ALU.add), reads=[hi], writes=[hi])
                p.op("dve", lambda e: e.tensor_tensor(out=acc[:, qb * 128:nk], in0=acc[:, qb * 128:nk], in1=cneg[:, :], op=ALU.add),
                     reads=[acc, cneg], writes=[acc])
                per = (NIT + 3) // 4
                for i_ in range(NIT):
                    if i_ % per == 0:
                        yield
                    p.op("dve", lambda e: e.tensor_tensor(out=mid[:, :], in0=lo[:, :], in1=hi[:, :], op=ALU.add), reads=[lo, hi], writes=[mid])
                    p.op("dve", lambda e: e.tensor_scalar(out=mid[:, :], in0=mid[:, :], scalar1=0.5, scalar2=None, op0=ALU.mult), reads=[mid], writes=[mid])
                    p.op("dve", lambda e: e.tensor_scalar(out=mk[:, :nk], in0=acc[:, :nk], scalar1=mid[:, 0:1], scalar2=None, op0=ALU.is_ge,
                                                          op1=ALU.add, accum_out=cnt[:, 0:1]), reads=[acc, mid], writes=[mk, cnt])
                    p.op("dve", lambda e: e.tensor_scalar(out=ge[:, :], in0=cnt[:, :], scalar1=KEEP - 0.5, scalar2=None, op0=ALU.is_ge),
                         reads=[cnt], writes=[ge])
                    p.op("dve", lambda e: e.tensor_tensor(out=t1[:, :], in0=mid[:, :], in1=lo[:, :], op=ALU.subtract), reads=[mid, lo], writes=[t1])
                    p.op("dve", lambda e: e.scalar_tensor_tensor(out=lo[:, :], in0=t1[:, :], scalar=ge[:, 0:1], in1=lo[:, :], op0=ALU.mult, op1=ALU.add),
                         reads=[t1, ge, lo], writes=[lo])
                    p.op("dve", lambda e: e.tensor_tensor(out=t1[:, :], in0=hi[:, :], in1=mid[:, :], op=ALU.subtract), reads=[hi, mid], writes=[t1])
                    p.op("dve", lambda e: e.scalar_tensor_tensor(out=hi[:, :], in0=t1[:, :], scalar=ge[:, 0:1], in1=mid[:, :], op0=ALU.mult, op1=ALU.add),
                         reads=[t1, ge, mid], writes=[hi])
                p.op("dve", lambda e: e.tensor_scalar(out=mk[:, :nk], in0=acc[:, :nk], scalar1=lo[:, 0:1], scalar2=NEG, op0=ALU.is_lt, op1=ALU.mult),
                     reads=[acc, lo], writes=[mk])

            def mask_transposes(qb):
                MT = mkT[qb % 2]
                for k0 in range(0, qb + 1, 4):
                    n4 = min(4, qb + 1 - k0)
                    for j in range(n4):
                        p.op("pe", lambda e, j=j: e.transpose(out=ptx[:, j * 128:(j + 1) * 128], in_=mk[:, (k0 + j) * 128:(k0 + j + 1) * 128],
                                                              identity=ident[:, :]), reads=[mk, ident], writes=[ptx])
                    p.op("act", lambda e: e.activation(out=MT[:, k0 * 128:(k0 + n4) * 128], in_=ptx[:, :n4 * 128], func=AF.Copy),
                         reads=[ptx], writes=[MT])

            gen = [None]

            def prologue(psl):
                for _ in indexer(0, psl):
                    pass
                mask_transposes(0)

            def hg_hook(qb, hg, psl):
                if qb + 1 >= NQB:
                    return
                if hg == 0:
                    gen[0] = indexer(qb + 1, psl)
                try:
                    next(gen[0])
                    if hg == 3:
                        for _ in gen[0]:
                            pass
                except StopIteration:
                    pass

            def post_qb(qb):
                if qb + 1 < NQB:
                    if gen[0] is not None:
                        for _ in gen[0]:
                            pass
                    mask_transposes(qb + 1)

            def kbs_b(qb):
                return list(range(qb + 1))

            def bias_b(qb, kb, h0):
                if kb == qb:
                    return "tile", dbd[:, h0:h0 + 4, :].rearrange("p h n -> p (h n)"), dbd
                if kb == qb - 1:
                    return "tile", dba[:, h0:h0 + 4, :].rearrange("p h n -> p (h n)"), dba
                return "col", r31, r31

            def mask_b(qb, kb):
                return mkT[qb % 2][:, kb * 128:(kb + 1) * 128], mkT[qb % 2]

            attention(p, s, S, obT, qbT, kbT, vbTM, 16, 1, kbs_b, bias_b, mask_b, None, scale, pre_qb=prologue,
                      pe_bias=dict(ident=ident), hg_hook=hg_hook, post_qb=post_qb)

        gemm(p, y1T, oaT, w_pa, 2048, S, D, tm=False, mulT=sgT[0:D, :], cblk=1024)
        gemm(p, yT, obT, w_pb, 2048, S, D, tm=False, mulT=sgT[D:2 * D, :], addT=y1T, cblk=1024)
        gemm(p, x1, yT, w_out, D, S, D, tm=True, add_tm=x, dst_dt=F32, cblk=1024, wbufs=1)

        rmsnorm(p, x1, g_xattn, S, D, dstB=u2T)
        rmsnorm(p, mem, g_mem, ML, D, dstT=memT)
        gemm(p, qmT, None, w_qm, D, S, 512, tm=False, x_ap_fn=xb(u2T))
        gemm(p, kmT, memT, w_km, D, ML, 512, tm=False)
        gemm(p, vmTM, memT, w_vm, D, ML, 512, tm=True, dst_dt=BF16)
        with Stage(p) as s:
            attention(p, s, S, omT, qmT, kmT, vmTM, 4, 1, lambda qb: list(range(ML // 128)), None, None, None, scale)
        gemm(p, x2, omT, w_om, 512, S, D, tm=True, add_tm=x1, dst_dt=F32)

        rmsnorm(p, x2, g_ffn, S, D, dstB=u3T)
        gemm(p, rl[:, 0:N_GROUPS], None, w_grp, D, S, N_GROUPS, tm=True, dst_dt=F32, x_ap_fn=xb(u3T))
        gemm(p, rl[:, N_GROUPS:], None, w_exp, D, S, NEX, tm=True, dst_dt=F32, x_ap_fn=xb(u3T))
        with Stage(p) as s:
            bg = s.sb([128, N_GROUPS], F32, "bg"); be = s.sb([128, NEX], F32, "be"); gs = s.sb([128, N_GROUPS], F32, "gs")
            esl = s.sb([128, NE, NEX], F32, "esl")
            p.dma("sp", bg[:, :], b_grp.partition_broadcast(128), writes=[bg])
            p.dma("sp", be[:, :], b_exp.partition_broadcast(128), writes=[be])
            p.dma("sp", gs[:, :], gsel.partition_broadcast(128), writes=[gs])
            p.dma("sp", esl[:, :, :], esel.partition_broadcast(128), writes=[esl])
            L = s.sb([128, N_GROUPS + NEX], F32, "L")
            V = {n: s.sb([128, 1], F32, n) for n in ("gmax", "ngmax", "gsum", "top1", "top2", "d", "p1", "p2", "own")}
            goh = s.sb([128, N_GROUPS], F32, "goh"); pen = s.sb([128, N_GROUPS], F32, "pen"); gj = s.sb([128, N_GROUPS], F32, "gj")
            elm = s.sb([128, NEX], F32, "elm"); oh1 = s.sb([128, NEX], F32, "oh1"); oh2 = s.sb([128, NEX], F32, "oh2"); G = s.sb([128, NEX], F32, "G")
            prod = s.sb([128, NE, NEX], F32, "prod"); GL = s.sb([128, NE], F32, "GL")
            idf = s.sb([128, 128], F32, "idf")
            p.op("pool", lambda e: e.memset(idf[:, :], 1.0), writes=[idf])
            p.op("pool", lambda e: e.affine_select(out=idf[:, :], in_=idf[:, :], pattern=[[-1, 128]], compare_op=ALU.is_equal,
                                                   fill=0.0, base=0, channel_multiplier=1), reads=[idf], writes=[idf])
            pgt = s.ps([128, 128], F32, "pgt")
            glt = s.sb([NE, 128], F32, "glt")

            def dv(fn, reads, writes):
                p.op("dve", fn, reads=reads, writes=writes)

            for i in range(NQB):
                p.dma("sp", L[:, :], rl[i * 128:(i + 1) * 128, :], writes=[L])
                gl, el = L[:, 0:N_GROUPS], L[:, N_GROUPS:]
                dv(lambda e: e.tensor_tensor(out=gl, in0=gl, in1=bg[:, :], op=ALU.add), [L, bg], [L])
                dv(lambda e: e.tensor_tensor(out=el, in0=el, in1=be[:, :], op=ALU.add), [L, be], [L])
                dv(lambda e: e.tensor_reduce(out=V["gmax"][:, :], in_=gl, axis=AX.X, op=ALU.max), [L], [V["gmax"]])
                dv(lambda e: e.tensor_scalar(out=goh[:, :], in0=gl, scalar1=V["gmax"][:, 0:1], scalar2=None, op0=ALU.is_equal), [L, V["gmax"]], [goh])
                dv(lambda e: e.tensor_scalar(out=V["ngmax"][:, :], in0=V["gmax"][:, :], scalar1=-1.0, scalar2=None, op0=ALU.mult), [V["gmax"]], [V["ngmax"]])
                p.op("act", lambda e: e.activation(out=gj[:, :], in_=gl, func=AF.Exp, bias=V["ngmax"][:, 0:1], accum_out=V["gsum"][:, 0:1]),
                     reads=[L, V["ngmax"]], writes=[gj, V["gsum"]])
                dv(lambda e: e.tensor_scalar(out=pen[:, :], in0=goh[:, :], scalar1=-1.0, scalar2=1e9, op0=ALU.add, op1=ALU.mult), [goh], [pen])
                dv(lambda e: e.tensor_tensor(out=elm[:, :].rearrange("p (g j) -> p g j", j=EPG), in0=el.rearrange("p (g j) -> p g j", j=EPG),
                                             in1=pen[:, :].unsqueeze(2).broadcast_to([128, N_GROUPS, EPG]), op=ALU.add), [L, pen], [elm])
                dv(lambda e: e.tensor_reduce(out=V["top1"][:, :], in_=elm[:, :], axis=AX.X, op=ALU.max), [elm], [V["top1"]])
                dv(lambda e: e.tensor_scalar(out=oh1[:, :], in0=elm[:, :], scalar1=V["top1"][:, 0:1], scalar2=None, op0=ALU.is_equal), [elm, V["top1"]], [oh1])
                dv(lambda e: e.scalar_tensor_tensor(out=elm[:, :], in0=oh1[:, :], scalar=-1e9, in1=elm[:, :], op0=ALU.mult, op1=ALU.add), [oh1, elm], [elm])
                dv(lambda e: e.tensor_reduce(out=V["top2"][:, :], in_=elm[:, :], axis=AX.X, op=ALU.max), [elm], [V["top2"]])
                dv(lambda e: e.tensor_scalar(out=oh2[:, :], in0=elm[:, :], scalar1=V["top2"][:, 0:1], scalar2=None, op0=ALU.is_equal), [elm, V["top2"]], [oh2])
                dv(lambda e: e.tensor_tensor(out=V["d"][:, :], in0=V["top2"][:, :], in1=V["top1"][:, :], op=ALU.subtract), [V["top1"], V["top2"]], [V["d"]])
                p.op("act", lambda e: e.activation(out=V["d"][:, :], in_=V["d"][:, :], func=AF.Exp), reads=[V["d"]], writes=[V["d"]])
                dv(lambda e: e.tensor_scalar(out=V["p1"][:, :], in0=V["d"][:, :], scalar1=1.0, scalar2=None, op0=ALU.add), [V["d"]], [V["p1"]])
                dv(lambda e: e.tensor_tensor(out=V["p1"][:, :], in0=V["p1"][:, :], in1=V["gsum"][:, :], op=ALU.mult), [V["p1"], V["gsum"]], [V["p1"]])
                dv(lambda e: e.reciprocal(out=V["p1"][:, :], in_=V["p1"][:, :]), [V["p1"]], [V["p1"]])
                dv(lambda e: e.tensor_tensor(out=V["p2"][:, :], in0=V["p1"][:, :], in1=V["d"][:, :], op=ALU.mult), [V["p1"], V["d"]], [V["p2"]])
                dv(lambda e: e.tensor_scalar(out=G[:, :], in0=oh1[:, :], scalar1=V["p1"][:, 0:1], scalar2=None, op0=ALU.mult), [oh1, V["p1"]], [G])
                dv(lambda e: e.scalar_tensor_tensor(out=G[:, :], in0=oh2[:, :], scalar=V["p2"][:, 0:1], in1=G[:, :], op0=ALU.mult, op1=ALU.add), [oh2, V["p2"], G], [G])
                dv(lambda e: e.tensor_tensor(out=prod[:, :, :], in0=esl[:, :, :], in1=G[:, :].unsqueeze(1).broadcast_to([128, NE, NEX]), op=ALU.mult), [esl, G], [prod])
                dv(lambda e: e.tensor_reduce(out=GL[:, :], in_=prod[:, :, :], axis=AX.X, op=ALU.add), [prod], [GL])
                dv(lambda e: e.tensor_tensor(out=gj[:, :], in0=goh[:, :], in1=gs[:, :], op=ALU.mult), [goh, gs], [gj])
                dv(lambda e: e.tensor_reduce(out=V["own"][:, :], in_=gj[:, :], axis=AX.X, op=ALU.add), [gj], [V["own"]])
                p.op("pe", lambda e: e.transpose(out=pgt[0:NE, :], in_=GL[:, :], identity=idf[:, :]), reads=[GL, idf], writes=[pgt])
                p.op("act", lambda e: e.activation(out=glt[:, :], in_=pgt[0:NE, :], func=AF.Copy), reads=[pgt], writes=[glt])
                p.dma("sp", GTt[:, i * 128:(i + 1) * 128], glt[:, :], reads=[glt])
                p.dma("sp", own_o[i * 128:(i + 1) * 128, :], V["own"][:, :], reads=[V["own"]])

        for le in range(NE):
            gemm_gated(p, lambda cc, nb, nw, le=le: HT4[nb // 128:(nb + nw) // 128, :, le * (FF // 128) + cc, :].rearrange("t p n -> p t n"),
                       xb(u3T), w_e1[le], w_e3[le], GTt[le], D, S, FF)
        KCT = NE * FF // 128
        KH = KCT // 2
        w2cat = w_e2.rearrange("e f d -> (e f) d")
        part = dscr("mpart", [S, D], F32)
        prev = dscr("macc", [S, D], F32)
        gemm(p, part, None, w2cat[0:KH * 128, :], KH * 128, S, D, tm=True, add_tm=x2, dst_dt=F32,
             cblk=1024, wbufs=1, nblk=128, x_ap_fn=lambda nb, nw: HT4[nb // 128][:, 0:KH, :])
        gemm(p, prev, None, w2cat[KH * 128:, :], (KCT - KH) * 128, S, D, tm=True, add_tm=part, dst_dt=F32,
             cblk=1024, wbufs=1, nblk=128, x_ap_fn=lambda nb, nw: HT4[nb // 128][:, KH:KCT, :])

        rmsnorm(p, prev, g_final, S, D, dstT=None, dst_tm=out, dst_tm_dt=F32)
        p.barrier()
        STATS.update(cnt=dict(p.cnt), nsem=p.nsem)
    return nc


def _bucket(d):
    d = np.maximum(d, 0)
    df = np.maximum(d, 1).astype(np.float32)
    large = 16 + (np.log(df / np.float32(16)) / np.float32(math.log(128 / 16)) * np.float32(16)).astype(np.int32)
    large = np.minimum(large, 31)
    return np.where(d < 16, d, large)


def _bias_tiles(rel_bias):
    s = np.arange(128)[:, None]
    t = np.arange(128)[None, :]
    sb = np.full((128, 16, 2, 128), NEG, np.float32)
    db = np.full((128, 16, 2, 128), NEG, np.float32)
    for j in range(2):
        dist = t + 128 - (j * 128 + s)
        valid = (dist >= 0) & (dist < 128)
        g = rel_bias[_bucket(dist)]
        for h in range(16):
            sb[:, h, j, :] = np.where(valid, g[:, :, h], np.float32(NEG))
        dist = t - s + (128 if j == 1 else 0)
        valid = dist >= 0
        g = rel_bias[_bucket(dist)]
        for h in range(16):
            db[:, h, j, :] = np.where(valid, g[:, :, 16 + h], np.float32(NEG))
    return sb, db


def make_in_maps(inputs, cfg, n_cores):
    f = lambda a: np.ascontiguousarray(np.asarray(a, dtype=np.float32))
    NE, EPG = cfg["NE"], cfg["EPG"]
    NEX = N_GROUPS * EPG
    rel_bias = f(inputs["rel_bias"])
    sb, db = _bias_tiles(rel_bias)
    cpb = cfg["cores_per_batch"]
    gpc = N_GROUPS // cpb
    shared = dict(
        rel31=f(rel_bias[31, 16:32]), sbias=sb, dbias=db,
        g_mix=f(inputs["g_mix"][0]), w_in=f(inputs["w_in"][0]), g_cq=f(inputs["g_cq"][0]), w_uq=f(inputs["w_uq"][0]),
        w_qidx=f(inputs["w_qidx"][0]), g_ckv=f(inputs["g_ckv"][0]), w_uk=f(inputs["w_uk"][0]).reshape(KV_RANK, 2048),
        w_uv=f(inputs["w_uv"][0]).reshape(KV_RANK, 2048), g_kidx=f(inputs["g_kidx"][0]), sink_a=f(inputs["sink_a"][0]),
        w_pa=f(inputs["w_pa"][0]), w_pb=f(inputs["w_pb"][0]), w_out=f(inputs["w_out"][0]), g_xattn=f(inputs["g_xattn"][0]),
        g_mem=f(inputs["g_mem"][0]), w_qm=f(inputs["w_qm"][0]), w_km=f(inputs["w_km"][0]), w_vm=f(inputs["w_vm"][0]),
        w_om=f(inputs["w_om"][0]), g_ffn=f(inputs["g_ffn"][0]), w_grp=f(inputs["w_grp"][0]), b_grp=f(inputs["b_grp"][0]),
        w_exp=f(inputs["w_exp"][0]), b_exp=f(inputs["b_exp"][0]), g_final=f(inputs["g_final"]),
    )
    maps = []
    for c in range(n_cores):
        b, pp = c // cpb, c % cpb
        e0 = pp * gpc * EPG
        gsel = np.zeros(N_GROUPS, np.float32)
        gsel[pp * gpc:(pp + 1) * gpc] = 1.0
        esel = np.zeros((NE, NEX), np.float32)
        esel[np.arange(NE), e0 + np.arange(NE)] = 1.0
        m = dict(shared)
        m.update(x=f(inputs["x"][b]), mem=f(inputs["mem"][b]), gsel=gsel, esel=esel,
                 w_e1=f(inputs["w_e1"][0][e0:e0 + NE]), w_e3=f(inputs["w_e3"][0][e0:e0 + NE]), w_e2=f(inputs["w_e2"][0][e0:e0 + NE]))
        maps.append(m)
    return maps


def assemble(results, cfg, n_cores, B, S, D):
    cpb = cfg["cores_per_batch"]
    out = np.zeros((B, S, D), np.float32)
    for c in range(n_cores):
        b = c // cpb
        own = results[c]["own"][:, 0] > 0.5
        out[b][own] = results[c]["out"][own]
    return out


STATS = {}
CFG = dict(D=4096, S=8192, ML=256, NE=16, EPG=8, FF=768, KEEP=256, NIT=26, cores_per_batch=4)


def kernel(**inputs):
    cfg = CFG
    nc = build(cfg)
    maps = make_in_maps(inputs, cfg, 8)
    res = run_bass_kernel_spmd(nc, maps, core_ids=list(range(8)))
    return assemble(res.results, cfg, 8, 2, cfg["S"], cfg["D"])
```

```python
import math
from contextlib import ExitStack
import numpy as np
import concourse.bass as bass
import concourse.mybir as mybir
from concourse.bass_utils import run_bass_kernel_spmd

F32 = mybir.dt.float32
BF16 = mybir.dt.bfloat16
AF = mybir.ActivationFunctionType
ALU = mybir.AluOpType
AX = mybir.AxisListType

HD = 128
A_HEADS, A_KV = 16, 4
B_HEADS = 16
Q_RANK, KV_RANK = 1024, 512
IDX_HEADS, IDX_DIM = 16, 128
MEM_HEADS = 4
N_GROUPS = 8
EPS = 1e-6
NEG = -30000.0
EPOCH = 30000
DMA_EPOCH = 1800


class Buf:
    __slots__ = ("w", "r")

    def __init__(self):
        self.w = None
        self.r = {}


class T:
    def __init__(self, t):
        self.t = t
        self.b = Buf()

    def __getitem__(self, k):
        return self.t[k]


class Prog:
    def __init__(self, nc, es):
        self.nc = nc
        self.es = es
        self.E = dict(pe=nc.tensor, act=nc.scalar, dve=nc.vector, pool=nc.gpsimd, sp=nc.sync)
        self.cs = {e: [] for e in self.E}
        self.cnt = {e: 0 for e in self.E}
        self.seen = {}
        self.nds = 24
        self.ds = [None] * self.nds
        self.dc = [0] * self.nds
        self.did = [0] * self.nds
        self.dn = 0
        self.nsem = 0
        self.live_d = {}

    def _newsem(self):
        self.nsem += 1
        return self.es.enter_context(self.nc.semaphore(f"s{self.nsem}"))

    def _csem(self, e, ep):
        while len(self.cs[e]) <= ep:
            self.cs[e].append(self._newsem())
        return self.cs[e][ep]

    def _wait(self, e, key, val):
        kind = key[0]
        if kind == "c" and key[1] == e and e == "pe":
            return
        k = (e, key)
        if self.seen.get(k, 0) >= val:
            return
        self.seen[k] = val
        if kind == "c":
            sem = self.cs[key[1]][key[2]]
        else:
            sem = self.live_d[key[1]]
        self.E[e].wait_ge(sem, val)

    def _deps(self, e, reads, writes):
        for b in reads:
            b = b.b if isinstance(b, T) else b
            if b.w is not None:
                self._wait(e, *b.w)
        for b in writes:
            b = b.b if isinstance(b, T) else b
            if b.w is not None:
                self._wait(e, *b.w)
            for key, val in b.r.items():
                self._wait(e, key, val)

    def _mark(self, tok, reads, writes):
        key, val = tok
        for b in reads:
            b = b.b if isinstance(b, T) else b
            if b.r.get(key, 0) < val:
                b.r[key] = val
        for b in writes:
            b = b.b if isinstance(b, T) else b
            b.w = tok
            b.r = {}

    def op(self, e, fn, reads=(), writes=()):
        self._deps(e, reads, writes)
        ins = fn(self.E[e])
        i = self.cnt[e]
        self.cnt[e] += 1
        ep, v = i // EPOCH, i % EPOCH + 1
        ins.then_inc(self._csem(e, ep), 1)
        tok = (("c", e, ep), v)
        self._mark(tok, reads, writes)
        return tok

    def dma(self, q, out, in_, reads=(), writes=(), **kw):
        k = self.dn
        self.dn = (k + 1) % self.nds
        if self.ds[k] is not None and self.dc[k] > 0:
            self._wait(q, ("d", self.did[k]), 16 * self.dc[k])
        if self.ds[k] is None or self.dc[k] >= DMA_EPOCH:
            self.ds[k] = self._newsem()
            self.dc[k] = 0
            self.did[k] = self.nsem
            self.live_d[self.nsem] = self.ds[k]
        self._deps(q, reads, writes)
        ins = self.E[q].dma_start(out=out, in_=in_, **kw)
        self.dc[k] += 1
        ins.then_inc(self.ds[k], 16)
        tok = (("d", self.did[k]), 16 * self.dc[k])
        self._mark(tok, reads, writes)
        return tok

    def barrier(self):
        for e in self.E:
            for f in self.E:
                if f == e or self.cnt[f] == 0:
                    continue
                i = self.cnt[f] - 1
                self._wait(e, ("c", f, i // EPOCH), i % EPOCH + 1)
            for k in range(self.nds):
                if self.ds[k] is not None and self.dc[k] > 0:
                    self._wait(e, ("d", self.did[k]), 16 * self.dc[k])


class Stage:
    uid = 0

    def __init__(self, p):
        self.p = p
        self.es = ExitStack()
        self.n = 0

    def __enter__(self):
        self.es.__enter__()
        return self

    def __exit__(self, *a):
        self.p.barrier()
        return self.es.__exit__(*a)

    def sb(self, shape, dt, name="t"):
        Stage.uid += 1
        return T(self.es.enter_context(self.p.nc.sbuf_tensor(f"{name}_{Stage.uid}", list(shape), dt)))

    def ps(self, shape, dt=F32, name="p"):
        Stage.uid += 1
        return T(self.es.enter_context(self.p.nc.psum_tensor(f"{name}_{Stage.uid}", list(shape), dt)))


def kc_view(ap):
    return ap.rearrange("(kc p) n -> p kc n", p=128)


def gemm(p, dst, srcT, w, K, N, cols, tm, act=None, mulT=None, addT=None, add_tm=None,
         rowscale=None, dst_dt=BF16, cblk=512, wbufs=2, nblk=512, x_ap_fn=None):
    KC = K // 128
    with Stage(p) as s:
        wt = [s.sb([128, KC, cblk], BF16, "wt") for _ in range(wbufs)]
        xt = [s.sb([128, KC, nblk], BF16, "xt") for _ in range(2)]
        ps = [s.ps([128, 512]) for _ in range(2)]
        ot = [s.sb([128, 512], dst_dt, "ot") for _ in range(2)]
        t1 = [s.sb([128, 512], F32, "t1") for _ in range(2)]
        mt = [s.sb([128, 512], BF16, "mt") for _ in range(2)]
        at = [s.sb([128, 512], BF16 if not tm else F32, "at") for _ in range(2)]
        rs = [s.sb([128, 1], F32, "rs") for _ in range(2)]
        wv = kc_view(w)
        xv = kc_view(srcT) if x_ap_fn is None else None
        it = 0
        iw = 0
        ix = 0
        for cb in range(0, cols, cblk):
            cw = min(cblk, cols - cb)
            W = wt[iw % wbufs]
            iw += 1
            for c5 in range(0, cw, 512):
                c5w = min(512, cw - c5)
                p.dma("pool", W[:, :, c5:c5 + c5w], wv[:, :, cb + c5:cb + c5 + c5w], writes=[W])
            for nb in range(0, N, nblk):
                nw = min(nblk, N - nb)
                X = xt[ix % 2]
                ix += 1
                p.dma("sp", X[:, :, :nw], xv[:, :, nb:nb + nw] if x_ap_fn is None else x_ap_fn(nb, nw), writes=[X])
                if not tm:
                    for cc in range(cw // 128):
                        P_, O, T1, M, A = ps[it % 2], ot[it % 2], t1[it % 2], mt[it % 2], at[it % 2]
                        it += 1
                        r0 = cb + cc * 128
                        for kc in range(KC):
                            p.op("pe", lambda e, kc=kc: e.matmul(P_[:, :nw], lhsT=W[:, kc, cc * 128:(cc + 1) * 128],
                                                                  rhs=X[:, kc, :nw], start=(kc == 0), stop=(kc == KC - 1)),
                                 reads=[W, X], writes=[P_])
                        f = act if act is not None else AF.Copy
                        if mulT is None and addT is None:
                            p.op("act", lambda e: e.activation(out=O[:, :nw], in_=P_[:, :nw], func=f), reads=[P_], writes=[O])
                        else:
                            p.op("act", lambda e: e.activation(out=T1[:, :nw], in_=P_[:, :nw], func=f), reads=[P_], writes=[T1])
                            if mulT is not None:
                                p.dma("sp", M[:, :nw], mulT[r0:r0 + 128, nb:nb + nw], writes=[M])
                                tgt = O if addT is None else T1
                                p.op("dve", lambda e: e.tensor_tensor(out=tgt[:, :nw], in0=T1[:, :nw], in1=M[:, :nw], op=ALU.mult),
                                     reads=[T1, M], writes=[tgt])
                            if addT is not None:
                                p.dma("sp", A[:, :nw], addT[r0:r0 + 128, nb:nb + nw], writes=[A])
                                p.op("dve", lambda e: e.tensor_tensor(out=O[:, :nw], in0=T1[:, :nw], in1=A[:, :nw], op=ALU.add),
                                     reads=[T1, A], writes=[O])
                        p.dma("sp", dst[r0:r0 + 128, nb:nb + nw], O[:, :nw], reads=[O])
                else:
                    for ti in range(nw // 128):
                        t0 = nb + ti * 128
                        for c5 in range(0, cw, 512):
                            c5w = min(512, cw - c5)
                            P_, O, A, R = ps[it % 2], ot[it % 2], at[it % 2], rs[it % 2]
                            it += 1
                            for kc in range(KC):
                                p.op("pe", lambda e, kc=kc: e.matmul(P_[:, :c5w], lhsT=X[:, kc, ti * 128:(ti + 1) * 128],
                                                                      rhs=W[:, kc, c5:c5 + c5w], start=(kc == 0), stop=(kc == KC - 1)),
                                     reads=[W, X], writes=[P_])
                            if add_tm is not None:
                                p.dma("sp", A[:, :c5w], add_tm[t0:t0 + 128, cb + c5:cb + c5 + c5w], writes=[A])
                            if rowscale is not None:
                                p.dma("sp", R[:, :], rowscale[t0:t0 + 128, :], writes=[R], allow_slow_non_contiguous=True)
                                p.op("dve", lambda e: e.scalar_tensor_tensor(out=O[:, :c5w], in0=P_[:, :c5w], scalar=R[:, 0:1], in1=A[:, :c5w],
                                                                             op0=ALU.mult, op1=ALU.add), reads=[P_, R, A], writes=[O])
                            elif add_tm is not None:
                                p.op("dve", lambda e: e.tensor_tensor(out=O[:, :c5w], in0=P_[:, :c5w], in1=A[:, :c5w], op=ALU.add),
                                     reads=[P_, A], writes=[O])
                            else:
                                p.op("act", lambda e: e.activation(out=O[:, :c5w], in_=P_[:, :c5w], func=AF.Copy), reads=[P_], writes=[O])
                            p.dma("sp", dst[t0:t0 + 128, cb + c5:cb + c5 + c5w], O[:, :c5w], reads=[O])


def gemm_gated(p, dst_fn, x_ap_fn, w1, w3, gT_row, K, N, FF):
    KC = K // 128
    with Stage(p) as s:
        W1 = s.sb([128, KC, FF], BF16, "w1")
        W3 = s.sb([128, KC, FF], BF16, "w3")
        xt = [s.sb([128, KC, 512], BF16, "xt") for _ in range(2)]
        gb = [s.sb([128, 512], F32, "gb") for _ in range(2)]
        ps1 = [s.ps([128, 512]) for _ in range(2)]
        ps3 = [s.ps([128, 512]) for _ in range(2)]
        t1 = [s.sb([128, 512], F32, "t1") for _ in range(2)]
        ot = [s.sb([128, 512], BF16, "ot") for _ in range(2)]
        for c5 in range(0, FF, 512):
            c5w = min(512, FF - c5)
            p.dma("pool", W1[:, :, c5:c5 + c5w], kc_view(w1)[:, :, c5:c5 + c5w], writes=[W1])
            p.dma("pool", W3[:, :, c5:c5 + c5w], kc_view(w3)[:, :, c5:c5 + c5w], writes=[W3])
        it = 0
        for ib, nb in enumerate(range(0, N, 512)):
            nw = min(512, N - nb)
            X, GB = xt[ib % 2], gb[ib % 2]
            p.dma("sp", X[:, :, :nw], x_ap_fn(nb, nw), writes=[X])
            p.dma("sp", GB[:, :nw], gT_row[nb:nb + nw].partition_broadcast(128), writes=[GB])
            for cc in range(FF // 128):
                P1, P3, T1, O = ps1[it % 2], ps3[it % 2], t1[it % 2], ot[it % 2]
                it += 1
                for kc in range(KC):
                    p.op("pe", lambda e, kc=kc: e.matmul(P1[:, :nw], lhsT=W1[:, kc, cc * 128:(cc + 1) * 128], rhs=X[:, kc, :nw],
                                                          start=(kc == 0), stop=(kc == KC - 1)), reads=[W1, X], writes=[P1])
                for kc in range(KC):
                    p.op("pe", lambda e, kc=kc: e.matmul(P3[:, :nw], lhsT=W3[:, kc, cc * 128:(cc + 1) * 128], rhs=X[:, kc, :nw],
                                                          start=(kc == 0), stop=(kc == KC - 1)), reads=[W3, X], writes=[P3])
                p.op("act", lambda e: e.activation(out=T1[:, :nw], in_=P1[:, :nw], func=AF.Silu), reads=[P1], writes=[T1])
                p.op("dve", lambda e: e.tensor_tensor(out=T1[:, :nw], in0=T1[:, :nw], in1=P3[:, :nw], op=ALU.mult), reads=[T1, P3], writes=[T1])
                p.op("dve", lambda e: e.tensor_tensor(out=O[:, :nw], in0=T1[:, :nw], in1=GB[:, :nw], op=ALU.mult), reads=[T1, GB], writes=[O])
                p.dma("sp", dst_fn(cc, nb, nw), O[:, :nw].rearrange("p (t n) -> p t n", n=128), reads=[O])


def rmsnorm(p, src, g, n, W, dstT=None, dst_tm=None, dst_tm_dt=BF16, dstB=None):
    KC = W // 128
    with Stage(p) as s:
        ident = s.sb([128, 128], BF16, "id")
        idf = s.sb([128, 128], F32, "idf")
        p.op("pool", lambda e: e.memset(idf[:, :], 1.0), writes=[idf])
        p.op("pool", lambda e: e.affine_select(out=idf[:, :], in_=idf[:, :], pattern=[[-1, 128]], compare_op=ALU.is_equal,
                                               fill=0.0, base=0, channel_multiplier=1), reads=[idf], writes=[idf])
        p.op("dve", lambda e: e.tensor_copy(out=ident[:, :], in_=idf[:, :]), reads=[idf], writes=[ident])
        gt = s.sb([128, W], F32, "g")
        p.dma("sp", gt[:, :], g.partition_broadcast(128), writes=[gt])
        xt = [s.sb([128, W], F32, "x") for _ in range(2)]
        junk = s.sb([128, W], BF16, "junk")
        un = [s.sb([128, W], dst_tm_dt if dst_tm is not None else BF16, "un") for _ in range(2)]
        unb = [s.sb([128, W], BF16, "unb") for _ in range(2)] if (dst_tm is not None and dst_tm_dt != BF16 and (dstT is not None or dstB is not None)) else None
        ss = [s.sb([128, 1], F32, "ss") for _ in range(2)]
        rstd = [s.sb([128, 1], F32, "rstd") for _ in range(2)]
        uT = [s.sb([128, KC, 128], BF16, "uT") for _ in range(2)]
        pt = [s.ps([128, 512], BF16, "pt") for _ in range(2)]
        ip = 0
        for i in range(n // 128):
            X, U, SS, R, UT = xt[i % 2], un[i % 2], ss[i % 2], rstd[i % 2], uT[i % 2]
            p.dma("sp", X[:, :], src[i * 128:(i + 1) * 128, :], writes=[X])
            p.op("act", lambda e: e.activation(out=junk[:, :], in_=X[:, :], func=AF.Square, accum_out=SS[:, 0:1]),
                 reads=[X], writes=[junk, SS])
            p.op("dve", lambda e: e.tensor_scalar(out=R[:, :], in0=SS[:, :], scalar1=1.0 / W, scalar2=EPS, op0=ALU.mult, op1=ALU.add),
                 reads=[SS], writes=[R])
            p.op("act", lambda e: e.activation(out=R[:, :], in_=R[:, :], func=AF.Sqrt), reads=[R], writes=[R])
            p.op("dve", lambda e: e.reciprocal(out=R[:, :], in_=R[:, :]), reads=[R], writes=[R])
            p.op("dve", lambda e: e.scalar_tensor_tensor(out=U[:, :], in0=X[:, :], scalar=R[:, 0:1], in1=gt[:, :], op0=ALU.mult, op1=ALU.mult),
                 reads=[X, R, gt], writes=[U])
            if dst_tm is not None:
                p.dma("sp", dst_tm[i * 128:(i + 1) * 128, :], U[:, :], reads=[U])
            if dstT is None and dstB is None:
                continue
            UB = U
            if unb is not None:
                UB = unb[i % 2]
                p.op("act", lambda e: e.activation(out=UB[:, :], in_=U[:, :], func=AF.Copy), reads=[U], writes=[UB])
            for c0 in range(0, KC, 4):
                PT = pt[ip % 2]
                ip += 1
                nc_ = min(4, KC - c0)
                for j in range(nc_):
                    c = c0 + j
                    p.op("pe", lambda e, c=c, j=j: e.transpose(out=PT[:, j * 128:(j + 1) * 128], in_=UB[:, c * 128:(c + 1) * 128],
                                                               identity=ident[:, :]), reads=[UB, ident], writes=[PT])
                p.op("dve", lambda e: e.tensor_copy(out=UT[:, c0:c0 + nc_, :], in_=PT[:, :nc_ * 128].rearrange("p (c n) -> p c n", n=128)),
                     reads=[PT], writes=[UT])
            if dstB is not None:
                p.dma("sp", dstB[i // 4, :, :, (i % 4) * 128:(i % 4 + 1) * 128], UT[:, :, :], reads=[UT])
            else:
                p.dma("sp", kc_view(dstT)[:, :, i * 128:(i + 1) * 128], UT[:, :, :], reads=[UT])


def attention(p, s, S, outT, qT, kT, vtm, nheads, gqa, kbs_of, bias_of, mask_of, esink, scale, pre_qb=None, pe_bias=None, hg_hook=None, post_qb=None):
    ones = s.sb([128, 128], BF16, "ones")
    p.op("pool", lambda e: e.memset(ones[:, :], 1.0), writes=[ones])
    qq = [s.sb([128, nheads, 128], BF16, "qq") for _ in range(2)]
    KCH = 8
    kk = [s.sb([128, 4 if gqa == 1 else 1, KCH * 128], BF16, "kk") for _ in range(2)]
    vv = [s.sb([128, KCH, 4 if gqa == 1 else 1, 128], BF16, "vv") for _ in range(2)]
    psl = [s.ps([128, 512], F32, "psl") for _ in range(2)]
    pso = [s.ps([128, 512], F32, "pso") for _ in range(4)]
    psd = s.ps([128, 512], F32, "psd")
    tmp = [s.sb([128, 512], F32, "tmp") for _ in range(2)]
    eT = [s.sb([128, 512], BF16, "eT") for _ in range(2)]
    rec = s.sb([128, 512], F32, "rec")
    oT = [s.sb([128, 4, 128], BF16, "oT") for _ in range(2)]
    qv = qT.rearrange("(h d) n -> d h n", d=128)
    kv = kT.rearrange("(h d) n -> d h n", d=128)
    vvw = vtm.rearrange("(kb s) (h d) -> s kb h d", s=128, d=128)
    ov = outT.rearrange("(h d) n -> d h n", d=128)
    it = 0
    ik = 0
    io = 0
    pending = [None]
    if pre_qb is not None:
        pre_qb(psl)
    for qb in range(S // 128):
        Q = qq[qb % 2]
        p.dma("sp", Q[:, :, :], qv[:, :, qb * 128:(qb + 1) * 128], writes=[Q])
        kbs = kbs_of(qb)
        for hg in range(nheads // 4):
            h0 = hg * 4
            first = True
            if hg_hook is not None:
                hg_hook(qb, hg, psl)
            for c0 in range(0, len(kbs), KCH):
                ch = kbs[c0:c0 + KCH]
                kb0, nkb = ch[0], len(ch)
                KK, VV = kk[ik % 2], vv[ik % 2]
                ik += 1
                if gqa == 1:
                    p.dma("sp", KK[:, :, :nkb * 128], kv[:, h0:h0 + 4, kb0 * 128:(kb0 + nkb) * 128], writes=[KK])
                    p.dma("sp", VV[:, :nkb, :, :], vvw[:, kb0:kb0 + nkb, h0:h0 + 4, :], writes=[VV])
                else:
                    p.dma("sp", KK[:, :, :nkb * 128], kv[:, hg:hg + 1, kb0 * 128:(kb0 + nkb) * 128], writes=[KK])
                    p.dma("sp", VV[:, :nkb, :, :], vvw[:, kb0:kb0 + nkb, hg:hg + 1, :], writes=[VV])
                for j, kb in enumerate(ch):
                    last = (c0 + j == len(kbs) - 1)
                    PL, TM, E = psl[it % 2], tmp[it % 2], eT[it % 2]
                    it += 1
                    bz = bias_of(qb, kb, h0) if bias_of is not None else None
                    mk = mask_of(qb, kb) if mask_of is not None else None
                    if pe_bias is not None:
                        kind, bap, bbuf = bz
                        map_, mbuf = mk
                        mrep = map_.unsqueeze(1).broadcast_to([128, 4, 128])
                        if kind == "tile":
                            p.op("pe", lambda e: e.matmul(PL[:, :], lhsT=pe_bias["ident"][:, :], rhs=bap, start=True, stop=False, skip_group_check=True),
                                 reads=[pe_bias["ident"], bbuf], writes=[PL])
                        else:
                            p.op("pe", lambda e: e.matmul(PL[:, :], lhsT=pe_bias["onesrow"][0:1, :], rhs=bap, start=True, stop=False, skip_group_check=True),
                                 reads=[pe_bias["onesrow"], bbuf], writes=[PL])
                        p.op("pe", lambda e: e.matmul(PL[:, :].rearrange("p (h n) -> p h n", n=128), lhsT=pe_bias["ident"][:, :], rhs=mrep,
                                                      start=False, stop=False, skip_group_check=True),
                             reads=[pe_bias["ident"], mbuf], writes=[PL])
                        for hh in range(4):
                            p.op("pe", lambda e, hh=hh: e.matmul(PL[:, hh * 128:(hh + 1) * 128], lhsT=KK[:, hh, j * 128:(j + 1) * 128],
                                                                 rhs=Q[:, h0 + hh, :], start=False, stop=(hh == 3), skip_group_check=True),
                                 reads=[KK, Q], writes=[PL])
                        p.op("act", lambda e: e.activation(out=E[:, :], in_=PL[:, :], func=AF.Exp, scale=scale), reads=[PL], writes=[E])
                    else:
                        for hh in range(4):
                            kh = hh if gqa == 1 else 0
                            p.op("pe", lambda e, hh=hh, kh=kh: e.matmul(PL[:, hh * 128:(hh + 1) * 128], lhsT=KK[:, kh, j * 128:(j + 1) * 128],
                                                                        rhs=Q[:, h0 + hh, :], start=True, stop=True),
                                 reads=[KK, Q], writes=[PL])
                        if bz is None and mk is None:
                            p.op("act", lambda e: e.activation(out=E[:, :], in_=PL[:, :], func=AF.Exp, scale=scale), reads=[PL], writes=[E])
                        else:
                            bap, bbuf = bz
                            p.op("dve", lambda e: e.scalar_tensor_tensor(out=TM[:, :].rearrange("p (h n) -> p h n", n=128),
                                                                         in0=PL[:, :].rearrange("p (h n) -> p h n", n=128), scalar=scale, in1=bap,
                                                                         op0=ALU.mult, op1=ALU.add), reads=[PL, bbuf], writes=[TM])
                            p.op("act", lambda e: e.activation(out=E[:, :], in_=TM[:, :], func=AF.Exp), reads=[TM], writes=[E])

                    def pv(E=E, VV=VV, j=j, first=first, last=last):
                        for hh in range(4):
                            kh = hh if gqa == 1 else 0
                            PO = pso[hh]
                            p.op("pe", lambda e, hh=hh, kh=kh, PO=PO: e.matmul(PO[:, :128], lhsT=VV[:, j, kh, :], rhs=E[:, hh * 128:(hh + 1) * 128],
                                                                               start=first, stop=last), reads=[VV, E], writes=[PO])
                        p.op("pe", lambda e: e.matmul(psd[:, :], lhsT=ones[:, :], rhs=E[:, :], start=first, stop=last),
                             reads=[ones, E], writes=[psd])

                    if pending[0] is not None:
                        pending[0]()
                    pending[0] = pv
                    first = False
            if pending[0] is not None:
                pending[0]()
                pending[0] = None
            O = oT[io % 2]
            io += 1
            if esink is not None:
                for hh in range(4):
                    p.op("dve", lambda e, hh=hh: e.tensor_scalar(out=rec[:, hh * 128:(hh + 1) * 128], in0=psd[:, hh * 128:(hh + 1) * 128],
                                                                 scalar1=esink[:, h0 + hh:h0 + hh + 1], scalar2=None, op0=ALU.add),
                         reads=[psd, esink], writes=[rec])
                p.op("dve", lambda e: e.reciprocal(out=rec[:, :], in_=rec[:, :]), reads=[rec], writes=[rec])
            else:
                p.op("dve", lambda e: e.reciprocal(out=rec[:, :], in_=psd[:, :]), reads=[psd], writes=[rec])
            for hh in range(4):
                PO = pso[hh]
                p.op("dve", lambda e, hh=hh, PO=PO: e.tensor_tensor(out=O[:, hh, :], in0=PO[:, :128], in1=rec[:, hh * 128:(hh + 1) * 128], op=ALU.mult),
                     reads=[PO, rec], writes=[O])
            p.dma("sp", ov[:, h0:h0 + 4, qb * 128:(qb + 1) * 128], O[:, :, :], reads=[O])
        if post_qb is not None:
            post_qb(qb)


IN_OFF = {}
_o = 0
for _n, _w in (("q_a", 2048), ("k_a", 512), ("v_a", 512), ("c_q", 1024), ("c_kv", 512), ("k_i", 128), ("w_i", 16),
               ("gate_a", None), ("gate_b", None)):
    IN_OFF[_n] = _o
    if _w is not None:
        _o += _w


def xb(B):
    return lambda nb, nw: B[nb // 512][:, :, :nw]


def build(cfg):
    D, S, ML, NE, EPG, FF, KEEP, NIT = cfg["D"], cfg["S"], cfg["ML"], cfg["NE"], cfg["EPG"], cfg["FF"], cfg["KEEP"], cfg["NIT"]
    NEX = N_GROUPS * EPG
    INW = 4752 + 2 * D
    OFF_GA, OFF_GB = 4752, 4752 + D
    NQB = S // 128
    nc = bass.Bass("TRN2", target_bir_lowering=False)

    def din(name, shape, dt=F32):
        return nc.dram_tensor(name, list(shape), dt, kind="ExternalInput").ap()

    def dscr(name, shape, dt):
        return nc.dram_tensor(name, list(shape), dt, kind="Internal").ap()

    x = din("x", [S, D]); mem = din("mem", [ML, D])
    rel31 = din("rel31", [16]); sbias = din("sbias", [128, 16, 2, 128]); dbias = din("dbias", [128, 16, 2, 128])
    g_mix = din("g_mix", [D]); w_in = din("w_in", [D, INW]); g_cq = din("g_cq", [Q_RANK]); w_uq = din("w_uq", [Q_RANK, 2048])
    w_qidx = din("w_qidx", [Q_RANK, 2048]); g_ckv = din("g_ckv", [KV_RANK]); w_uk = din("w_uk", [KV_RANK, 2048])
    w_uv = din("w_uv", [KV_RANK, 2048]); g_kidx = din("g_kidx", [IDX_DIM]); sink_a = din("sink_a", [16])
    w_pa = din("w_pa", [2048, D]); w_pb = din("w_pb", [2048, D]); w_out = din("w_out", [D, D])
    g_xattn = din("g_xattn", [D]); g_mem = din("g_mem", [D]); w_qm = din("w_qm", [D, 512]); w_km = din("w_km", [D, 512])
    w_vm = din("w_vm", [D, 512]); w_om = din("w_om", [512, D]); g_ffn = din("g_ffn", [D])
    w_grp = din("w_grp", [D, N_GROUPS]); b_grp = din("b_grp", [N_GROUPS]); w_exp = din("w_exp", [D, NEX]); b_exp = din("b_exp", [NEX])
    w_e1 = din("w_e1", [NE, D, FF]); w_e3 = din("w_e3", [NE, D, FF]); w_e2 = din("w_e2", [NE, FF, D]); g_final = din("g_final", [D])
    gsel = din("gsel", [N_GROUPS])
    esel = din("esel", [NE, NEX])
    out = nc.dram_tensor("out", [S, D], F32, kind="ExternalOutput").ap()
    own_o = nc.dram_tensor("own", [S, 1], F32, kind="ExternalOutput").ap()

    uT = dscr("uT", [S // 512, 128, D // 128, 512], BF16)
    qkT = dscr("qkT", [2560, S], BF16)
    sgT = dscr("sgT", [2 * D, S], BF16)
    vaTM = dscr("vaTM", [S, 512], BF16)
    tmA = dscr("tmA", [S, 1680], F32)
    cqT = dscr("cqT", [S // 512, 128, Q_RANK // 128, 512], BF16)
    ckvT = dscr("ckvT", [S // 512, 128, KV_RANK // 128, 512], BF16)
    kiT = dscr("kiT", [IDX_DIM, S], BF16)
    qbT = dscr("qbT", [2048, S], BF16)
    qiT = dscr("qiT", [2048, S], BF16)
    kbT = dscr("kbT", [2048, S], BF16)
    vbTM = dscr("vbTM", [S, 2048], BF16)
    oaT = dscr("oaT", [2048, S], BF16)
    obT = dscr("obT", [2048, S], BF16)
    y1T = dscr("y1T", [D, S], BF16)
    yT = dscr("yT", [D, S], BF16)
    x1 = dscr("x1", [S, D], F32)
    u2T = dscr("u2T", [S // 512, 128, D // 128, 512], BF16)
    memT = dscr("memT", [D, ML], BF16)
    qmT = dscr("qmT", [512, S], BF16)
    kmT = dscr("kmT", [512, ML], BF16)
    vmTM = dscr("vmTM", [ML, 512], BF16)
    omT = dscr("omT", [512, S], BF16)
    x2 = dscr("x2", [S, D], F32)
    u3T = dscr("u3T", [S // 512, 128, D // 128, 512], BF16)
    rl = dscr("rl", [S, N_GROUPS + NEX], F32)
    GTt = dscr("GTt", [NE, S], F32)
    HT4 = dscr("HT4", [S // 128, 128, NE * FF // 128, 128], BF16)

    with ExitStack() as es:
        p = Prog(nc, es)
        scale = HD ** -0.5

        rmsnorm(p, x, g_mix, S, D, dstB=uT)
        gemm(p, qkT, None, w_in[:, 0:2560], D, S, 2560, tm=False, cblk=1024, wbufs=1, x_ap_fn=xb(uT))
        gemm(p, sgT, None, w_in[:, OFF_GA:OFF_GA + 2 * D], D, S, 2 * D, tm=False, act=AF.Sigmoid, cblk=1024, wbufs=1, x_ap_fn=xb(uT))
        gemm(p, vaTM, None, w_in[:, 2560:3072], D, S, 512, tm=True, dst_dt=BF16, x_ap_fn=xb(uT))
        gemm(p, tmA, None, w_in[:, 3072:4752], D, S, 1680, tm=True, dst_dt=F32, cblk=1024, wbufs=1, x_ap_fn=xb(uT))
        rmsnorm(p, tmA[:, 0:1024], g_cq, S, 1024, dstB=cqT)
        rmsnorm(p, tmA[:, 1024:1536], g_ckv, S, 512, dstB=ckvT)
        rmsnorm(p, tmA[:, 1536:1664], g_kidx, S, 128, dstT=kiT)
        gemm(p, qbT, None, w_uq, Q_RANK, S, 2048, tm=False, x_ap_fn=xb(cqT))
        gemm(p, qiT, None, w_qidx, Q_RANK, S, 2048, tm=False, x_ap_fn=xb(cqT))
        gemm(p, kbT, None, w_uk, KV_RANK, S, 2048, tm=False, x_ap_fn=xb(ckvT))
        gemm(p, vbTM, None, w_uv, KV_RANK, S, 2048, tm=True, dst_dt=BF16, x_ap_fn=xb(ckvT))

        with Stage(p) as s:
            sb_ = s.sb([128, 16, 2, 128], F32, "sbias")
            p.dma("sp", sb_[:, :, :, :], sbias, writes=[sb_])
            es_ = s.sb([128, 16], F32, "esink")
            p.dma("sp", es_[:, :], sink_a.partition_broadcast(128), writes=[es_])
            p.op("act", lambda e: e.activation(out=es_[:, :], in_=es_[:, :], func=AF.Exp), reads=[es_], writes=[es_])

            def kbs_a(qb):
                return [qb] if qb == 0 else [qb - 1, qb]

            def bias_a(qb, kb, h0):
                j = 1 if kb == qb else 0
                return sb_[:, h0:h0 + 4, j, :], sb_

            attention(p, s, S, oaT, qkT[0:2048, :], qkT[2048:2560, :], vaTM, 16, 4, kbs_a, bias_a, None, es_, scale)

        with Stage(p) as s:
            inv = 1.0 / scale
            db_ = s.sb([128, 16, 2, 128], F32, "dbias")
            p.dma("sp", db_[:, :, :, :], dbias, writes=[db_])
            dbb = s.sb([128, 16, 2, 128], BF16, "dbb")
            p.op("dve", lambda e: e.tensor_scalar(out=dbb[:, :, :, :], in0=db_[:, :, :, :], scalar1=inv, scalar2=None, op0=ALU.mult),
                 reads=[db_], writes=[dbb])
            dbd = s.sb([128, 16, 128], BF16, "dbd")
            dba = s.sb([128, 16, 128], BF16, "dba")
            p.op("dve", lambda e: e.tensor_copy(out=dbd[:, :, :], in_=dbb[:, :, 0, :]), reads=[dbb], writes=[dbd])
            p.op("dve", lambda e: e.tensor_copy(out=dba[:, :, :], in_=dbb[:, :, 1, :]), reads=[dbb], writes=[dba])
            r31 = s.sb([1, 16], F32, "r31")
            p.dma("sp", r31[:, :], rel31.partition_broadcast(1), writes=[r31])
            p.op("dve", lambda e: e.tensor_scalar(out=r31[:, :], in0=r31[:, :], scalar1=inv, scalar2=None, op0=ALU.mult), reads=[r31], writes=[r31])
            bfar = s.sb([1, 16, 128], BF16, "bfar")
            p.op("dve", lambda e: e.tensor_copy(out=bfar[:, :, :], in_=r31[:, :].unsqueeze(2).broadcast_to([1, 16, 128])),
                 reads=[r31], writes=[bfar])
            onesrow = s.sb([1, 128], BF16, "onesrow")
            p.op("pool", lambda e: e.memset(onesrow[:, :], 1.0), writes=[onesrow])
            ident = s.sb([128, 128], BF16, "id")
            idf = s.sb([128, 128], F32, "idf")
            p.op("pool", lambda e: e.memset(idf[:, :], 1.0), writes=[idf])
            p.op("pool", lambda e: e.affine_select(out=idf[:, :], in_=idf[:, :], pattern=[[-1, 128]], compare_op=ALU.is_equal,
                                                   fill=0.0, base=0, channel_multiplier=1), reads=[idf], writes=[idf])
            p.op("dve", lambda e: e.tensor_copy(out=ident[:, :], in_=idf[:, :]), reads=[idf], writes=[ident])
            cneg = s.sb([128, 128], F32, "cneg")
            p.op("pool", lambda e: e.memset(cneg[:, :], 0.0), writes=[cneg])
            p.op("pool", lambda e: e.affine_select(out=cneg[:, :], in_=cneg[:, :], pattern=[[-1, 128]], compare_op=ALU.is_ge,
                                                   fill=-1e30, base=0, channel_multiplier=1), reads=[cneg], writes=[cneg])
            kis = s.sb([128, S], BF16, "kis")
            p.dma("sp", kis[:, :], kiT, writes=[kis])
            acc = s.sb([128, S], F32, "acc")
            mk = s.sb([128, S], BF16, "mk")
            mkT = [s.sb([128, S], BF16, "mkT") for _ in range(2)]
            qi = [s.sb([128, 16, 128], BF16, "qi") for _ in range(2)]
            wi = [s.sb([128, 16], F32, "wi") for _ in range(2)]
            rl_ = [s.sb([128, 512], F32, "relu") for _ in range(2)]
            ptx = s.ps([128, 512], BF16, "ptx")
            sm = {n: s.sb([128, 1], F32, n) for n in ("lo", "hi", "mid", "cnt", "ge", "t1")}
            qiv = qiT.rearrange("(h d) n -> d h n", d=128)
            cnt_i = [0]

            def indexer(qb, psl):
                nk = (qb + 1) * 128
                QI, WI = qi[qb % 2], wi[qb % 2]
                p.dma("sp", QI[:, :, :], qiv[:, :, qb * 128:(qb + 1) * 128], writes=[QI])
                p.dma("sp", WI[:, :], tmA[qb * 128:(qb + 1) * 128, 1664:1680], writes=[WI])
                for c in range(0, nk, 512):
                    cw = min(512, nk - c)
                    for h in range(16):
                        R = rl_[cnt_i[0] % 2]
                        PX = psl[cnt_i[0] % 2]
                        cnt_i[0] += 1
                        p.op("pe", lambda e, h=h: e.matmul(PX[:, :cw], lhsT=QI[:, h, :], rhs=kis[:, c:c + cw], start=True, stop=True),
                             reads=[QI, kis], writes=[PX])
                        p.op("act", lambda e: e.activation(out=R[:, :cw], in_=PX[:, :cw], func=AF.Relu), reads=[PX], writes=[R])
                        if h == 0:
                            p.op("dve", lambda e, h=h: e.tensor_scalar(out=acc[:, c:c + cw], in0=R[:, :cw], scalar1=WI[:, h:h + 1], scalar2=None,
                                                                       op0=ALU.mult), reads=[R, WI], writes=[acc])
                        else:
                            p.op("dve", lambda e, h=h: e.scalar_tensor_tensor(out=acc[:, c:c + cw], in0=R[:, :cw], scalar=WI[:, h:h + 1],
                                                                              in1=acc[:, c:c + cw], op0=ALU.mult, op1=ALU.add),
                                 reads=[R, WI, acc], writes=[acc])
                lo, hi, mid, cnt, ge, t1 = (sm[n] for n in ("lo", "hi", "mid", "cnt", "ge", "t1"))
                p.op("dve", lambda e: e.tensor_reduce(out=lo[:, :], in_=acc[:, :nk], axis=AX.X, op=ALU.min), reads=[acc], writes=[lo])
                p.op("dve", lambda e: e.tensor_reduce(out=hi[:, :], in_=acc[:, :nk], axis=AX.X, op=ALU.max), reads=[acc], writes=[hi])
                p.op("dve", lambda e: e.tensor_scalar(out=lo[:, :], in0=lo[:, :], scalar1=-1.0, scalar2=None, op0=ALU.add), reads=[lo], writes=[lo])
                p.op("dve", lambda e: e.tensor_scalar(out=hi[:, :], in0=hi[:, :], scalar1=1.0, scalar2=None, op0=ALU.add), reads=[hi], writes=[hi])
                p.op("dve", lambda e: e.tensor_tensor(out=acc[:, qb * 128:nk], in0=acc[:, qb * 128:nk], in1=cneg[:, :], op=ALU.add),
                     reads=[acc, cneg], writes=[acc])
                per = (NIT + 3) // 4
                for i_ in range(NIT):
                    if i_ % per == 0:
                        yield
                    p.op("dve", lambda e: e.tensor_tensor(out=mid[:, :], in0=lo[:, :], in1=hi[:, :], op=ALU.add), reads=[lo, hi], writes=[mid])
                    p.op("dve", lambda e: e.tensor_scalar(out=mid[:, :], in0=mid[:, :], scalar1=0.5, scalar2=None, op0=ALU.mult), reads=[mid], writes=[mid])
                    p.op("dve", lambda e: e.tensor_scalar(out=mk[:, :nk], in0=acc[:, :nk], scalar1=mid[:, 0:1], scalar2=None, op0=ALU.is_ge,
                                                          op1=ALU.add, accum_out=cnt[:, 0:1]), reads=[acc, mid], writes=[mk, cnt])
                    p.op("dve", lambda e: e.tensor_scalar(out=ge[:, :], in0=cnt[:, :], scalar1=KEEP - 0.5, scalar2=None, op0=ALU.is_ge),
                         reads=[cnt], writes=[ge])
                    p.op("dve", lambda e: e.tensor_tensor(out=t1[:, :], in0=mid[:, :], in1=lo[:, :], op=ALU.subtract), reads=[mid, lo], writes=[t1])
                    p.op("dve", lambda e: e.scalar_tensor_tensor(out=lo[:, :], in0=t1[:, :], scalar=ge[:, 0:1], in1=lo[:, :], op0=ALU.mult, op1=ALU.add),
                         reads=[t1, ge, lo], writes=[lo])
                    p.op("dve", lambda e: e.tensor_tensor(out=t1[:, :], in0=hi[:, :], in1=mid[:, :], op=ALU.subtract), reads=[hi, mid], writes=[t1])
                    p.op("dve", lambda e: e.scalar_tensor_tensor(out=hi[:, :], in0=t1[:, :], scalar=ge[:, 0:1], in1=mid[:, :], op0=ALU.mult, op1=ALU.add),
                         reads=[t1, ge, mid], writes=[hi])
                p.op("dve", lambda e: e.tensor_scalar(out=mk[:, :nk], in0=acc[:, :nk], scalar1=lo[:, 0:1], scalar2=NEG, op0=ALU.is_lt, op1=ALU.mult),
                     reads=[acc, lo], writes=[mk])

            def mask_transposes(qb):
                MT = mkT[qb % 2]
                for k0 in range(0, qb + 1, 4):
                    n4 = min(4, qb + 1 - k0)
                    for j in range(n4):
                        p.op("pe", lambda e, j=j: e.transpose(out=ptx[:, j * 128:(j + 1) * 128], in_=mk[:, (k0 + j) * 128:(k0 + j + 1) * 128],
                                                              identity=ident[:, :]), reads=[mk, ident], writes=[ptx])
                    p.op("act", lambda e: e.activation(out=MT[:, k0 * 128:(k0 + n4) * 128], in_=ptx[:, :n4 * 128], func=AF.Copy),
                         reads=[ptx], writes=[MT])

            gen = [None]

            def prologue(psl):
                for _ in indexer(0, psl):
                    pass
                mask_transposes(0)

            def hg_hook(qb, hg, psl):
                if qb + 1 >= NQB:
                    return
                if hg == 0:
                    gen[0] = indexer(qb + 1, psl)
                try:
                    next(gen[0])
                    if hg == 3:
                        for _ in gen[0]:
                            pass
                except StopIteration:
                    pass

            def post_qb(qb):
                if qb + 1 < NQB:
                    if gen[0] is not None:
                        for _ in gen[0]:
                            pass
                    mask_transposes(qb + 1)

            def kbs_b(qb):
                return list(range(qb + 1))

            def bias_b(qb, kb, h0):
                if kb == qb:
                    return "tile", dbd[:, h0:h0 + 4, :].rearrange("p h n -> p (h n)"), dbd
                if kb == qb - 1:
                    return "tile", dba[:, h0:h0 + 4, :].rearrange("p h n -> p (h n)"), dba
                return "row", bfar[0:1, h0:h0 + 4, :].rearrange("p h n -> p (h n)"), bfar

            def mask_b(qb, kb):
                return mkT[qb % 2][:, kb * 128:(kb + 1) * 128], mkT[qb % 2]

            attention(p, s, S, obT, qbT, kbT, vbTM, 16, 1, kbs_b, bias_b, mask_b, None, scale, pre_qb=prologue,
                      pe_bias=dict(ident=ident, onesrow=onesrow), hg_hook=hg_hook, post_qb=post_qb)

        gemm(p, y1T, oaT, w_pa, 2048, S, D, tm=False, mulT=sgT[0:D, :], cblk=1024)
        gemm(p, yT, obT, w_pb, 2048, S, D, tm=False, mulT=sgT[D:2 * D, :], addT=y1T, cblk=1024)
        gemm(p, x1, yT, w_out, D, S, D, tm=True, add_tm=x, dst_dt=F32, cblk=1024, wbufs=1)

        rmsnorm(p, x1, g_xattn, S, D, dstB=u2T)
        rmsnorm(p, mem, g_mem, ML, D, dstT=memT)
        gemm(p, qmT, None, w_qm, D, S, 512, tm=False, x_ap_fn=xb(u2T))
        gemm(p, kmT, memT, w_km, D, ML, 512, tm=False)
        gemm(p, vmTM, memT, w_vm, D, ML, 512, tm=True, dst_dt=BF16)
        with Stage(p) as s:
            attention(p, s, S, omT, qmT, kmT, vmTM, 4, 1, lambda qb: list(range(ML // 128)), None, None, None, scale)
        gemm(p, x2, omT, w_om, 512, S, D, tm=True, add_tm=x1, dst_dt=F32)

        rmsnorm(p, x2, g_ffn, S, D, dstB=u3T)
        gemm(p, rl[:, 0:N_GROUPS], None, w_grp, D, S, N_GROUPS, tm=True, dst_dt=F32, x_ap_fn=xb(u3T))
        gemm(p, rl[:, N_GROUPS:], None, w_exp, D, S, NEX, tm=True, dst_dt=F32, x_ap_fn=xb(u3T))
        with Stage(p) as s:
            bg = s.sb([128, N_GROUPS], F32, "bg"); be = s.sb([128, NEX], F32, "be"); gs = s.sb([128, N_GROUPS], F32, "gs")
            esl = s.sb([128, NE, NEX], F32, "esl")
            p.dma("sp", bg[:, :], b_grp.partition_broadcast(128), writes=[bg])
            p.dma("sp", be[:, :], b_exp.partition_broadcast(128), writes=[be])
            p.dma("sp", gs[:, :], gsel.partition_broadcast(128), writes=[gs])
            p.dma("sp", esl[:, :, :], esel.partition_broadcast(128), writes=[esl])
            L = s.sb([128, N_GROUPS + NEX], F32, "L")
            V = {n: s.sb([128, 1], F32, n) for n in ("gmax", "ngmax", "gsum", "top1", "top2", "d", "p1", "p2", "own")}
            goh = s.sb([128, N_GROUPS], F32, "goh"); pen = s.sb([128, N_GROUPS], F32, "pen"); gj = s.sb([128, N_GROUPS], F32, "gj")
            elm = s.sb([128, NEX], F32, "elm"); oh1 = s.sb([128, NEX], F32, "oh1"); oh2 = s.sb([128, NEX], F32, "oh2"); G = s.sb([128, NEX], F32, "G")
            prod = s.sb([128, NE, NEX], F32, "prod"); GL = s.sb([128, NE], F32, "GL")
            idf = s.sb([128, 128], F32, "idf")
            p.op("pool", lambda e: e.memset(idf[:, :], 1.0), writes=[idf])
            p.op("pool", lambda e: e.affine_select(out=idf[:, :], in_=idf[:, :], pattern=[[-1, 128]], compare_op=ALU.is_equal,
                                                   fill=0.0, base=0, channel_multiplier=1), reads=[idf], writes=[idf])
            pgt = s.ps([128, 128], F32, "pgt")
            glt = s.sb([NE, 128], F32, "glt")

            def dv(fn, reads, writes):
                p.op("dve", fn, reads=reads, writes=writes)

            for i in range(NQB):
                p.dma("sp", L[:, :], rl[i * 128:(i + 1) * 128, :], writes=[L])
                gl, el = L[:, 0:N_GROUPS], L[:, N_GROUPS:]
                dv(lambda e: e.tensor_tensor(out=gl, in0=gl, in1=bg[:, :], op=ALU.add), [L, bg], [L])
                dv(lambda e: e.tensor_tensor(out=el, in0=el, in1=be[:, :], op=ALU.add), [L, be], [L])
                dv(lambda e: e.tensor_reduce(out=V["gmax"][:, :], in_=gl, axis=AX.X, op=ALU.max), [L], [V["gmax"]])
                dv(lambda e: e.tensor_scalar(out=goh[:, :], in0=gl, scalar1=V["gmax"][:, 0:1], scalar2=None, op0=ALU.is_equal), [L, V["gmax"]], [goh])
                dv(lambda e: e.tensor_scalar(out=V["ngmax"][:, :], in0=V["gmax"][:, :], scalar1=-1.0, scalar2=None, op0=ALU.mult), [V["gmax"]], [V["ngmax"]])
                p.op("act", lambda e: e.activation(out=gj[:, :], in_=gl, func=AF.Exp, bias=V["ngmax"][:, 0:1], accum_out=V["gsum"][:, 0:1]),
                     reads=[L, V["ngmax"]], writes=[gj, V["gsum"]])
                dv(lambda e: e.tensor_scalar(out=pen[:, :], in0=goh[:, :], scalar1=-1.0, scalar2=1e9, op0=ALU.add, op1=ALU.mult), [goh], [pen])
                dv(lambda e: e.tensor_tensor(out=elm[:, :].rearrange("p (g j) -> p g j", j=EPG), in0=el.rearrange("p (g j) -> p g j", j=EPG),
                                             in1=pen[:, :].unsqueeze(2).broadcast_to([128, N_GROUPS, EPG]), op=ALU.add), [L, pen], [elm])
                dv(lambda e: e.tensor_reduce(out=V["top1"][:, :], in_=elm[:, :], axis=AX.X, op=ALU.max), [elm], [V["top1"]])
                dv(lambda e: e.tensor_scalar(out=oh1[:, :], in0=elm[:, :], scalar1=V["top1"][:, 0:1], scalar2=None, op0=ALU.is_equal), [elm, V["top1"]], [oh1])
                dv(lambda e: e.scalar_tensor_tensor(out=elm[:, :], in0=oh1[:, :], scalar=-1e9, in1=elm[:, :], op0=ALU.mult, op1=ALU.add), [oh1, elm], [elm])
                dv(lambda e: e.tensor_reduce(out=V["top2"][:, :], in_=elm[:, :], axis=AX.X, op=ALU.max), [elm], [V["top2"]])
                dv(lambda e: e.tensor_scalar(out=oh2[:, :], in0=elm[:, :], scalar1=V["top2"][:, 0:1], scalar2=None, op0=ALU.is_equal), [elm, V["top2"]], [oh2])
                dv(lambda e: e.tensor_tensor(out=V["d"][:, :], in0=V["top2"][:, :], in1=V["top1"][:, :], op=ALU.subtract), [V["top1"], V["top2"]], [V["d"]])
                p.op("act", lambda e: e.activation(out=V["d"][:, :], in_=V["d"][:, :], func=AF.Exp), reads=[V["d"]], writes=[V["d"]])
                dv(lambda e: e.tensor_scalar(out=V["p1"][:, :], in0=V["d"][:, :], scalar1=1.0, scalar2=None, op0=ALU.add), [V["d"]], [V["p1"]])
                dv(lambda e: e.tensor_tensor(out=V["p1"][:, :], in0=V["p1"][:, :], in1=V["gsum"][:, :], op=ALU.mult), [V["p1"], V["gsum"]], [V["p1"]])
                dv(lambda e: e.reciprocal(out=V["p1"][:, :], in_=V["p1"][:, :]), [V["p1"]], [V["p1"]])
                dv(lambda e: e.tensor_tensor(out=V["p2"][:, :], in0=V["p1"][:, :], in1=V["d"][:, :], op=ALU.mult), [V["p1"], V["d"]], [V["p2"]])
                dv(lambda e: e.tensor_scalar(out=G[:, :], in0=oh1[:, :], scalar1=V["p1"][:, 0:1], scalar2=None, op0=ALU.mult), [oh1, V["p1"]], [G])
                dv(lambda e: e.scalar_tensor_tensor(out=G[:, :], in0=oh2[:, :], scalar=V["p2"][:, 0:1], in1=G[:, :], op0=ALU.mult, op1=ALU.add), [oh2, V["p2"], G], [G])
                dv(lambda e: e.tensor_tensor(out=prod[:, :, :], in0=esl[:, :, :], in1=G[:, :].unsqueeze(1).broadcast_to([128, NE, NEX]), op=ALU.mult), [esl, G], [prod])
                dv(lambda e: e.tensor_reduce(out=GL[:, :], in_=prod[:, :, :], axis=AX.X, op=ALU.add), [prod], [GL])
                dv(lambda e: e.tensor_tensor(out=gj[:, :], in0=goh[:, :], in1=gs[:, :], op=ALU.mult), [goh, gs], [gj])
                dv(lambda e: e.tensor_reduce(out=V["own"][:, :], in_=gj[:, :], axis=AX.X, op=ALU.add), [gj], [V["own"]])
                p.op("pe", lambda e: e.transpose(out=pgt[0:NE, :], in_=GL[:, :], identity=idf[:, :]), reads=[GL, idf], writes=[pgt])
                p.op("act", lambda e: e.activation(out=glt[:, :], in_=pgt[0:NE, :], func=AF.Copy), reads=[pgt], writes=[glt])
                p.dma("sp", GTt[:, i * 128:(i + 1) * 128], glt[:, :], reads=[glt])
                p.dma("sp", own_o[i * 128:(i + 1) * 128, :], V["own"][:, :], reads=[V["own"]])

        for le in range(NE):
            gemm_gated(p, lambda cc, nb, nw, le=le: HT4[nb // 128:(nb + nw) // 128, :, le * (FF // 128) + cc, :].rearrange("t p n -> p t n"),
                       xb(u3T), w_e1[le], w_e3[le], GTt[le], D, S, FF)
        KCT = NE * FF // 128
        KH = KCT // 2
        w2cat = w_e2.rearrange("e f d -> (e f) d")
        part = dscr("mpart", [S, D], F32)
        prev = dscr("macc", [S, D], F32)
        gemm(p, part, None, w2cat[0:KH * 128, :], KH * 128, S, D, tm=True, add_tm=x2, dst_dt=F32,
             cblk=1024, wbufs=1, nblk=128, x_ap_fn=lambda nb, nw: HT4[nb // 128][:, 0:KH, :])
        gemm(p, prev, None, w2cat[KH * 128:, :], (KCT - KH) * 128, S, D, tm=True, add_tm=part, dst_dt=F32,
             cblk=1024, wbufs=1, nblk=128, x_ap_fn=lambda nb, nw: HT4[nb // 128][:, KH:KCT, :])

        rmsnorm(p, prev, g_final, S, D, dstT=None, dst_tm=out, dst_tm_dt=F32)
        p.barrier()
        STATS.update(cnt=dict(p.cnt), nsem=p.nsem)
    return nc


def _bucket(d):
    d = np.maximum(d, 0)
    df = np.maximum(d, 1).astype(np.float32)
    large = 16 + (np.log(df / np.float32(16)) / np.float32(math.log(128 / 16)) * np.float32(16)).astype(np.int32)
    large = np.minimum(large, 31)
    return np.where(d < 16, d, large)


def _bias_tiles(rel_bias):
    s = np.arange(128)[:, None]
    t = np.arange(128)[None, :]
    sb = np.full((128, 16, 2, 128), NEG, np.float32)
    db = np.full((128, 16, 2, 128), NEG, np.float32)
    for j in range(2):
        dist = t + 128 - (j * 128 + s)
        valid = (dist >= 0) & (dist < 128)
        g = rel_bias[_bucket(dist)]
        for h in range(16):
            sb[:, h, j, :] = np.where(valid, g[:, :, h], np.float32(NEG))
        dist = t - s + (128 if j == 1 else 0)
        valid = dist >= 0
        g = rel_bias[_bucket(dist)]
        for h in range(16):
            db[:, h, j, :] = np.where(valid, g[:, :, 16 + h], np.float32(NEG))
    return sb, db


def make_in_maps(inputs, cfg, n_cores):
    f = lambda a: np.ascontiguousarray(np.asarray(a, dtype=np.float32))
    NE, EPG = cfg["NE"], cfg["EPG"]
    NEX = N_GROUPS * EPG
    rel_bias = f(inputs["rel_bias"])
    sb, db = _bias_tiles(rel_bias)
    cpb = cfg["cores_per_batch"]
    gpc = N_GROUPS // cpb
    shared = dict(
        rel31=f(rel_bias[31, 16:32]), sbias=sb, dbias=db,
        g_mix=f(inputs["g_mix"][0]), w_in=f(inputs["w_in"][0]), g_cq=f(inputs["g_cq"][0]), w_uq=f(inputs["w_uq"][0]),
        w_qidx=f(inputs["w_qidx"][0]), g_ckv=f(inputs["g_ckv"][0]), w_uk=f(inputs["w_uk"][0]).reshape(KV_RANK, 2048),
        w_uv=f(inputs["w_uv"][0]).reshape(KV_RANK, 2048), g_kidx=f(inputs["g_kidx"][0]), sink_a=f(inputs["sink_a"][0]),
        w_pa=f(inputs["w_pa"][0]), w_pb=f(inputs["w_pb"][0]), w_out=f(inputs["w_out"][0]), g_xattn=f(inputs["g_xattn"][0]),
        g_mem=f(inputs["g_mem"][0]), w_qm=f(inputs["w_qm"][0]), w_km=f(inputs["w_km"][0]), w_vm=f(inputs["w_vm"][0]),
        w_om=f(inputs["w_om"][0]), g_ffn=f(inputs["g_ffn"][0]), w_grp=f(inputs["w_grp"][0]), b_grp=f(inputs["b_grp"][0]),
        w_exp=f(inputs["w_exp"][0]), b_exp=f(inputs["b_exp"][0]), g_final=f(inputs["g_final"]),
    )
    maps = []
    for c in range(n_cores):
        b, pp = c // cpb, c % cpb
        e0 = pp * gpc * EPG
        gsel = np.zeros(N_GROUPS, np.float32)
        gsel[pp * gpc:(pp + 1) * gpc] = 1.0
        esel = np.zeros((NE, NEX), np.float32)
        esel[np.arange(NE), e0 + np.arange(NE)] = 1.0
        m = dict(shared)
        m.update(x=f(inputs["x"][b]), mem=f(inputs["mem"][b]), gsel=gsel, esel=esel,
                 w_e1=f(inputs["w_e1"][0][e0:e0 + NE]), w_e3=f(inputs["w_e3"][0][e0:e0 + NE]), w_e2=f(inputs["w_e2"][0][e0:e0 + NE]))
        maps.append(m)
    return maps


def assemble(results, cfg, n_cores, B, S, D):
    cpb = cfg["cores_per_batch"]
    out = np.zeros((B, S, D), np.float32)
    for c in range(n_cores):
        b = c // cpb
        own = results[c]["own"][:, 0] > 0.5
        out[b][own] = results[c]["out"][own]
    return out


STATS = {}
CFG = dict(D=4096, S=8192, ML=256, NE=16, EPG=8, FF=768, KEEP=256, NIT=26, cores_per_batch=4)


def kernel(**inputs):
    cfg = CFG
    nc = build(cfg)
    maps = make_in_maps(inputs, cfg, 8)
    res = run_bass_kernel_spmd(nc, maps, core_ids=list(range(8)))
    return assemble(res.results, cfg, 8, 2, cfg["S"], cfg["D"])
```

```python
import math
from contextlib import ExitStack
import numpy as np
import concourse.bass as bass
import concourse.mybir as mybir
from concourse.bass_utils import run_bass_kernel_spmd

F32 = mybir.dt.float32
BF16 = mybir.dt.bfloat16
AF = mybir.ActivationFunctionType
ALU = mybir.AluOpType
AX = mybir.AxisListType

HD = 128
A_HEADS, A_KV = 16, 4
B_HEADS = 16
Q_RANK, KV_RANK = 1024, 512
IDX_HEADS, IDX_DIM = 16, 128
MEM_HEADS = 4
N_GROUPS = 8
EPS = 1e-6
NEG = -30000.0
EPOCH = 30000
DMA_EPOCH = 1800


class Buf:
    __slots__ = ("w", "r")

    def __init__(self):
        self.w = None
        self.r = {}


class T:
    def __init__(self, t):
        self.t = t
        self.b = Buf()

    def __getitem__(self, k):
        return self.t[k]


class Prog:
    def __init__(self, nc, es):
        self.nc = nc
        self.es = es
        self.E = dict(pe=nc.tensor, act=nc.scalar, dve=nc.vector, pool=nc.gpsimd, sp=nc.sync)
        self.cs = {e: [] for e in self.E}
        self.cnt = {e: 0 for e in self.E}
        self.seen = {}
        self.nds = 24
        self.ds = [None] * self.nds
        self.dc = [0] * self.nds
        self.did = [0] * self.nds
        self.dn = 0
        self.nsem = 0
        self.live_d = {}

    def _newsem(self):
        self.nsem += 1
        return self.es.enter_context(self.nc.semaphore(f"s{self.nsem}"))

    def _csem(self, e, ep):
        while len(self.cs[e]) <= ep:
            self.cs[e].append(self._newsem())
        return self.cs[e][ep]

    def _wait(self, e, key, val):
        kind = key[0]
        if kind == "c" and key[1] == e and e == "pe":
            return
        k = (e, key)
        if self.seen.get(k, 0) >= val:
            return
        self.seen[k] = val
        if kind == "c":
            sem = self.cs[key[1]][key[2]]
        else:
            sem = self.live_d[key[1]]
        self.E[e].wait_ge(sem, val)

    def _deps(self, e, reads, writes):
        for b in reads:
            b = b.b if isinstance(b, T) else b
            if b.w is not None:
                self._wait(e, *b.w)
        for b in writes:
            b = b.b if isinstance(b, T) else b
            if b.w is not None:
                self._wait(e, *b.w)
            for key, val in b.r.items():
                self._wait(e, key, val)

    def _mark(self, tok, reads, writes):
        key, val = tok
        for b in reads:
            b = b.b if isinstance(b, T) else b
            if b.r.get(key, 0) < val:
                b.r[key] = val
        for b in writes:
            b = b.b if isinstance(b, T) else b
            b.w = tok
            b.r = {}

    def op(self, e, fn, reads=(), writes=()):
        self._deps(e, reads, writes)
        ins = fn(self.E[e])
        i = self.cnt[e]
        self.cnt[e] += 1
        ep, v = i // EPOCH, i % EPOCH + 1
        ins.then_inc(self._csem(e, ep), 1)
        tok = (("c", e, ep), v)
        self._mark(tok, reads, writes)
        return tok

    def dma(self, q, out, in_, reads=(), writes=(), **kw):
        k = self.dn
        self.dn = (k + 1) % self.nds
        if self.ds[k] is not None and self.dc[k] > 0:
            self._wait(q, ("d", self.did[k]), 16 * self.dc[k])
        if self.ds[k] is None or self.dc[k] >= DMA_EPOCH:
            self.ds[k] = self._newsem()
            self.dc[k] = 0
            self.did[k] = self.nsem
            self.live_d[self.nsem] = self.ds[k]
        self._deps(q, reads, writes)
        ins = self.E[q].dma_start(out=out, in_=in_, **kw)
        self.dc[k] += 1
        ins.then_inc(self.ds[k], 16)
        tok = (("d", self.did[k]), 16 * self.dc[k])
        self._mark(tok, reads, writes)
        return tok

    def barrier(self):
        for e in self.E:
            for f in self.E:
                if f == e or self.cnt[f] == 0:
                    continue
                i = self.cnt[f] - 1
                self._wait(e, ("c", f, i // EPOCH), i % EPOCH + 1)
            for k in range(self.nds):
                if self.ds[k] is not None and self.dc[k] > 0:
                    self._wait(e, ("d", self.did[k]), 16 * self.dc[k])


class Stage:
    uid = 0

    def __init__(self, p):
        self.p = p
        self.es = ExitStack()
        self.n = 0

    def __enter__(self):
        self.es.__enter__()
        return self

    def __exit__(self, *a):
        self.p.barrier()
        return self.es.__exit__(*a)

    def sb(self, shape, dt, name="t"):
        Stage.uid += 1
        return T(self.es.enter_context(self.p.nc.sbuf_tensor(f"{name}_{Stage.uid}", list(shape), dt)))

    def ps(self, shape, dt=F32, name="p"):
        Stage.uid += 1
        return T(self.es.enter_context(self.p.nc.psum_tensor(f"{name}_{Stage.uid}", list(shape), dt)))


def kc_view(ap):
    return ap.rearrange("(kc p) n -> p kc n", p=128)


def gemm(p, dst, srcT, w, K, N, cols, tm, act=None, mulT=None, addT=None, add_tm=None,
         rowscale=None, dst_dt=BF16, cblk=512, wbufs=2, nblk=512, x_ap_fn=None):
    KC = K // 128
    with Stage(p) as s:
        wt = [s.sb([128, KC, cblk], BF16, "wt") for _ in range(wbufs)]
        xt = [s.sb([128, KC, nblk], BF16, "xt") for _ in range(2)]
        ps = [s.ps([128, 512]) for _ in range(2)]
        ot = [s.sb([128, 512], dst_dt, "ot") for _ in range(2)]
        t1 = [s.sb([128, 512], F32, "t1") for _ in range(2)]
        mt = [s.sb([128, 512], BF16, "mt") for _ in range(2)]
        at = [s.sb([128, 512], BF16 if not tm else F32, "at") for _ in range(2)]
        rs = [s.sb([128, 1], F32, "rs") for _ in range(2)]
        wv = kc_view(w)
        xv = kc_view(srcT) if x_ap_fn is None else None
        it = 0
        iw = 0
        ix = 0
        for cb in range(0, cols, cblk):
            cw = min(cblk, cols - cb)
            W = wt[iw % wbufs]
            iw += 1
            for c5 in range(0, cw, 512):
                c5w = min(512, cw - c5)
                p.dma("pool", W[:, :, c5:c5 + c5w], wv[:, :, cb + c5:cb + c5 + c5w], writes=[W])
            for nb in range(0, N, nblk):
                nw = min(nblk, N - nb)
                X = xt[ix % 2]
                ix += 1
                p.dma("sp", X[:, :, :nw], xv[:, :, nb:nb + nw] if x_ap_fn is None else x_ap_fn(nb, nw), writes=[X])
                if not tm:
                    for cc in range(cw // 128):
                        P_, O, T1, M, A = ps[it % 2], ot[it % 2], t1[it % 2], mt[it % 2], at[it % 2]
                        it += 1
                        r0 = cb + cc * 128
                        for kc in range(KC):
                            p.op("pe", lambda e, kc=kc: e.matmul(P_[:, :nw], lhsT=W[:, kc, cc * 128:(cc + 1) * 128],
                                                                  rhs=X[:, kc, :nw], start=(kc == 0), stop=(kc == KC - 1)),
                                 reads=[W, X], writes=[P_])
                        f = act if act is not None else AF.Copy
                        if mulT is None and addT is None:
                            p.op("act", lambda e: e.activation(out=O[:, :nw], in_=P_[:, :nw], func=f), reads=[P_], writes=[O])
                        else:
                            p.op("act", lambda e: e.activation(out=T1[:, :nw], in_=P_[:, :nw], func=f), reads=[P_], writes=[T1])
                            if mulT is not None:
                                p.dma("sp", M[:, :nw], mulT[r0:r0 + 128, nb:nb + nw], writes=[M])
                                tgt = O if addT is None else T1
                                p.op("dve", lambda e: e.tensor_tensor(out=tgt[:, :nw], in0=T1[:, :nw], in1=M[:, :nw], op=ALU.mult),
                                     reads=[T1, M], writes=[tgt])
                            if addT is not None:
                                p.dma("sp", A[:, :nw], addT[r0:r0 + 128, nb:nb + nw], writes=[A])
                                p.op("dve", lambda e: e.tensor_tensor(out=O[:, :nw], in0=T1[:, :nw], in1=A[:, :nw], op=ALU.add),
                                     reads=[T1, A], writes=[O])
                        p.dma("act", dst[r0:r0 + 128, nb:nb + nw], O[:, :nw], reads=[O])
                else:
                    for ti in range(nw // 128):
                        t0 = nb + ti * 128
                        for c5 in range(0, cw, 512):
                            c5w = min(512, cw - c5)
                            P_, O, A, R = ps[it % 2], ot[it % 2], at[it % 2], rs[it % 2]
                            it += 1
                            for kc in range(KC):
                                p.op("pe", lambda e, kc=kc: e.matmul(P_[:, :c5w], lhsT=X[:, kc, ti * 128:(ti + 1) * 128],
                                                                      rhs=W[:, kc, c5:c5 + c5w], start=(kc == 0), stop=(kc == KC - 1)),
                                     reads=[W, X], writes=[P_])
                            if add_tm is not None:
                                p.dma("sp", A[:, :c5w], add_tm[t0:t0 + 128, cb + c5:cb + c5 + c5w], writes=[A])
                            if rowscale is not None:
                                p.dma("sp", R[:, :], rowscale[t0:t0 + 128, :], writes=[R], allow_slow_non_contiguous=True)
                                p.op("dve", lambda e: e.scalar_tensor_tensor(out=O[:, :c5w], in0=P_[:, :c5w], scalar=R[:, 0:1], in1=A[:, :c5w],
                                                                             op0=ALU.mult, op1=ALU.add), reads=[P_, R, A], writes=[O])
                            elif add_tm is not None:
                                p.op("dve", lambda e: e.tensor_tensor(out=O[:, :c5w], in0=P_[:, :c5w], in1=A[:, :c5w], op=ALU.add),
                                     reads=[P_, A], writes=[O])
                            else:
                                p.op("act", lambda e: e.activation(out=O[:, :c5w], in_=P_[:, :c5w], func=AF.Copy), reads=[P_], writes=[O])
                            p.dma("act", dst[t0:t0 + 128, cb + c5:cb + c5 + c5w], O[:, :c5w], reads=[O])


def gemm_gated(p, dst_fn, x_ap_fn, w1, w3, gT_row, K, N, FF):
    KC = K // 128
    with Stage(p) as s:
        W1 = s.sb([128, KC, FF], BF16, "w1")
        W3 = s.sb([128, KC, FF], BF16, "w3")
        xt = [s.sb([128, KC, 512], BF16, "xt") for _ in range(2)]
        gb = [s.sb([128, 512], F32, "gb") for _ in range(2)]
        ps1 = [s.ps([128, 512]) for _ in range(2)]
        ps3 = [s.ps([128, 512]) for _ in range(2)]
        t1 = [s.sb([128, 512], F32, "t1") for _ in range(2)]
        ot = [s.sb([128, 512], BF16, "ot") for _ in range(2)]
        for c5 in range(0, FF, 512):
            c5w = min(512, FF - c5)
            p.dma("pool", W1[:, :, c5:c5 + c5w], kc_view(w1)[:, :, c5:c5 + c5w], writes=[W1])
            p.dma("pool", W3[:, :, c5:c5 + c5w], kc_view(w3)[:, :, c5:c5 + c5w], writes=[W3])
        it = 0
        for ib, nb in enumerate(range(0, N, 512)):
            nw = min(512, N - nb)
            X, GB = xt[ib % 2], gb[ib % 2]
            p.dma("sp", X[:, :, :nw], x_ap_fn(nb, nw), writes=[X])
            p.dma("sp", GB[:, :nw], gT_row[nb:nb + nw].partition_broadcast(128), writes=[GB])
            for cc in range(FF // 128):
                P1, P3, T1, O = ps1[it % 2], ps3[it % 2], t1[it % 2], ot[it % 2]
                it += 1
                for kc in range(KC):
                    p.op("pe", lambda e, kc=kc: e.matmul(P1[:, :nw], lhsT=W1[:, kc, cc * 128:(cc + 1) * 128], rhs=X[:, kc, :nw],
                                                          start=(kc == 0), stop=(kc == KC - 1)), reads=[W1, X], writes=[P1])
                for kc in range(KC):
                    p.op("pe", lambda e, kc=kc: e.matmul(P3[:, :nw], lhsT=W3[:, kc, cc * 128:(cc + 1) * 128], rhs=X[:, kc, :nw],
                                                          start=(kc == 0), stop=(kc == KC - 1)), reads=[W3, X], writes=[P3])
                p.op("act", lambda e: e.activation(out=T1[:, :nw], in_=P1[:, :nw], func=AF.Silu), reads=[P1], writes=[T1])
                p.op("dve", lambda e: e.tensor_tensor(out=T1[:, :nw], in0=T1[:, :nw], in1=P3[:, :nw], op=ALU.mult), reads=[T1, P3], writes=[T1])
                p.op("dve", lambda e: e.tensor_tensor(out=O[:, :nw], in0=T1[:, :nw], in1=GB[:, :nw], op=ALU.mult), reads=[T1, GB], writes=[O])
                p.dma("act", dst_fn(cc, nb, nw), O[:, :nw].rearrange("p (t n) -> p t n", n=128), reads=[O])


def rmsnorm(p, src, g, n, W, dstT=None, dst_tm=None, dst_tm_dt=BF16, dstB=None):
    KC = W // 128
    with Stage(p) as s:
        ident = s.sb([128, 128], BF16, "id")
        idf = s.sb([128, 128], F32, "idf")
        p.op("pool", lambda e: e.memset(idf[:, :], 1.0), writes=[idf])
        p.op("pool", lambda e: e.affine_select(out=idf[:, :], in_=idf[:, :], pattern=[[-1, 128]], compare_op=ALU.is_equal,
                                               fill=0.0, base=0, channel_multiplier=1), reads=[idf], writes=[idf])
        p.op("dve", lambda e: e.tensor_copy(out=ident[:, :], in_=idf[:, :]), reads=[idf], writes=[ident])
        gt = s.sb([128, W], F32, "g")
        p.dma("sp", gt[:, :], g.partition_broadcast(128), writes=[gt])
        xt = [s.sb([128, W], F32, "x") for _ in range(2)]
        junk = s.sb([128, W], BF16, "junk")
        un = [s.sb([128, W], dst_tm_dt if dst_tm is not None else BF16, "un") for _ in range(2)]
        unb = [s.sb([128, W], BF16, "unb") for _ in range(2)] if (dst_tm is not None and dst_tm_dt != BF16 and (dstT is not None or dstB is not None)) else None
        ss = [s.sb([128, 1], F32, "ss") for _ in range(2)]
        rstd = [s.sb([128, 1], F32, "rstd") for _ in range(2)]
        uT = [s.sb([128, KC, 128], BF16, "uT") for _ in range(2)]
        pt = [s.ps([128, 512], BF16, "pt") for _ in range(2)]
        ip = 0
        for i in range(n // 128):
            X, U, SS, R, UT = xt[i % 2], un[i % 2], ss[i % 2], rstd[i % 2], uT[i % 2]
            p.dma("sp", X[:, :], src[i * 128:(i + 1) * 128, :], writes=[X])
            p.op("act", lambda e: e.activation(out=junk[:, :], in_=X[:, :], func=AF.Square, accum_out=SS[:, 0:1]),
                 reads=[X], writes=[junk, SS])
            p.op("dve", lambda e: e.tensor_scalar(out=R[:, :], in0=SS[:, :], scalar1=1.0 / W, scalar2=EPS, op0=ALU.mult, op1=ALU.add),
                 reads=[SS], writes=[R])
            p.op("act", lambda e: e.activation(out=R[:, :], in_=R[:, :], func=AF.Sqrt), reads=[R], writes=[R])
            p.op("dve", lambda e: e.reciprocal(out=R[:, :], in_=R[:, :]), reads=[R], writes=[R])
            p.op("dve", lambda e: e.scalar_tensor_tensor(out=U[:, :], in0=X[:, :], scalar=R[:, 0:1], in1=gt[:, :], op0=ALU.mult, op1=ALU.mult),
                 reads=[X, R, gt], writes=[U])
            if dst_tm is not None:
                p.dma("sp", dst_tm[i * 128:(i + 1) * 128, :], U[:, :], reads=[U])
            if dstT is None and dstB is None:
                continue
            UB = U
            if unb is not None:
                UB = unb[i % 2]
                p.op("act", lambda e: e.activation(out=UB[:, :], in_=U[:, :], func=AF.Copy), reads=[U], writes=[UB])
            for c0 in range(0, KC, 4):
                PT = pt[ip % 2]
                ip += 1
                nc_ = min(4, KC - c0)
                for j in range(nc_):
                    c = c0 + j
                    p.op("pe", lambda e, c=c, j=j: e.transpose(out=PT[:, j * 128:(j + 1) * 128], in_=UB[:, c * 128:(c + 1) * 128],
                                                               identity=ident[:, :]), reads=[UB, ident], writes=[PT])
                p.op("dve", lambda e: e.tensor_copy(out=UT[:, c0:c0 + nc_, :], in_=PT[:, :nc_ * 128].rearrange("p (c n) -> p c n", n=128)),
                     reads=[PT], writes=[UT])
            if dstB is not None:
                p.dma("sp", dstB[i // 4, :, :, (i % 4) * 128:(i % 4 + 1) * 128], UT[:, :, :], reads=[UT])
            else:
                p.dma("sp", kc_view(dstT)[:, :, i * 128:(i + 1) * 128], UT[:, :, :], reads=[UT])


def attention(p, s, S, outT, qT, kT, vtm, nheads, gqa, kbs_of, bias_of, mask_of, esink, scale, pre_qb=None, pe_bias=None, hg_hook=None, post_qb=None):
    ones = s.sb([128, 128], BF16, "ones")
    p.op("pool", lambda e: e.memset(ones[:, :], 1.0), writes=[ones])
    qq = [s.sb([128, nheads, 128], BF16, "qq") for _ in range(2)]
    KCH = 8
    kk = [s.sb([128, 4 if gqa == 1 else 1, KCH * 128], BF16, "kk") for _ in range(2)]
    vv = [s.sb([128, KCH, 4 if gqa == 1 else 1, 128], BF16, "vv") for _ in range(2)]
    psl = [s.ps([128, 512], F32, "psl") for _ in range(2)]
    pso = [s.ps([128, 512], F32, "pso") for _ in range(4)]
    psd = s.ps([128, 512], F32, "psd")
    tmp = [s.sb([128, 512], F32, "tmp") for _ in range(2)]
    eT = [s.sb([128, 512], BF16, "eT") for _ in range(2)]
    rec = s.sb([128, 512], F32, "rec")
    oT = [s.sb([128, 4, 128], BF16, "oT") for _ in range(2)]
    qv = qT.rearrange("(h d) n -> d h n", d=128)
    kv = kT.rearrange("(h d) n -> d h n", d=128)
    vvw = vtm.rearrange("(kb s) (h d) -> s kb h d", s=128, d=128)
    ov = outT.rearrange("(h d) n -> d h n", d=128)
    it = 0
    ik = 0
    io = 0
    pending = [None]
    if pre_qb is not None:
        pre_qb(psl)
    for qb in range(S // 128):
        Q = qq[qb % 2]
        p.dma("sp", Q[:, :, :], qv[:, :, qb * 128:(qb + 1) * 128], writes=[Q])
        kbs = kbs_of(qb)
        for hg in range(nheads // 4):
            h0 = hg * 4
            first = True
            if hg_hook is not None:
                hg_hook(qb, hg, psl)
            for c0 in range(0, len(kbs), KCH):
                ch = kbs[c0:c0 + KCH]
                kb0, nkb = ch[0], len(ch)
                KK, VV = kk[ik % 2], vv[ik % 2]
                ik += 1
                if gqa == 1:
                    p.dma("sp", KK[:, :, :nkb * 128], kv[:, h0:h0 + 4, kb0 * 128:(kb0 + nkb) * 128], writes=[KK])
                    p.dma("sp", VV[:, :nkb, :, :], vvw[:, kb0:kb0 + nkb, h0:h0 + 4, :], writes=[VV])
                else:
                    p.dma("sp", KK[:, :, :nkb * 128], kv[:, hg:hg + 1, kb0 * 128:(kb0 + nkb) * 128], writes=[KK])
                    p.dma("sp", VV[:, :nkb, :, :], vvw[:, kb0:kb0 + nkb, hg:hg + 1, :], writes=[VV])
                for j, kb in enumerate(ch):
                    last = (c0 + j == len(kbs) - 1)
                    PL, TM, E = psl[it % 2], tmp[it % 2], eT[it % 2]
                    it += 1
                    bz = bias_of(qb, kb, h0) if bias_of is not None else None
                    mk = mask_of(qb, kb) if mask_of is not None else None
                    if pe_bias is not None:
                        kind, bap, bbuf = bz
                        map_, mbuf = mk
                        mrep = map_.unsqueeze(1).broadcast_to([128, 4, 128])
                        if kind == "tile":
                            p.op("pe", lambda e: e.matmul(PL[:, :], lhsT=pe_bias["ident"][:, :], rhs=bap, start=True, stop=False, skip_group_check=True),
                                 reads=[pe_bias["ident"], bbuf], writes=[PL])
                        else:
                            p.op("pe", lambda e: e.matmul(PL[:, :], lhsT=pe_bias["onesrow"][0:1, :], rhs=bap, start=True, stop=False, skip_group_check=True),
                                 reads=[pe_bias["onesrow"], bbuf], writes=[PL])
                        p.op("pe", lambda e: e.matmul(PL[:, :].rearrange("p (h n) -> p h n", n=128), lhsT=pe_bias["ident"][:, :], rhs=mrep,
                                                      start=False, stop=False, skip_group_check=True),
                             reads=[pe_bias["ident"], mbuf], writes=[PL])
                        for hh in range(4):
                            p.op("pe", lambda e, hh=hh: e.matmul(PL[:, hh * 128:(hh + 1) * 128], lhsT=KK[:, hh, j * 128:(j + 1) * 128],
                                                                 rhs=Q[:, h0 + hh, :], start=False, stop=(hh == 3), skip_group_check=True),
                                 reads=[KK, Q], writes=[PL])
                        p.op("act", lambda e: e.activation(out=E[:, :], in_=PL[:, :], func=AF.Exp, scale=scale), reads=[PL], writes=[E])
                    else:
                        for hh in range(4):
                            kh = hh if gqa == 1 else 0
                            p.op("pe", lambda e, hh=hh, kh=kh: e.matmul(PL[:, hh * 128:(hh + 1) * 128], lhsT=KK[:, kh, j * 128:(j + 1) * 128],
                                                                        rhs=Q[:, h0 + hh, :], start=True, stop=True),
                                 reads=[KK, Q], writes=[PL])
                        if bz is None and mk is None:
                            p.op("act", lambda e: e.activation(out=E[:, :], in_=PL[:, :], func=AF.Exp, scale=scale), reads=[PL], writes=[E])
                        else:
                            bap, bbuf = bz
                            p.op("dve", lambda e: e.scalar_tensor_tensor(out=TM[:, :].rearrange("p (h n) -> p h n", n=128),
                                                                         in0=PL[:, :].rearrange("p (h n) -> p h n", n=128), scalar=scale, in1=bap,
                                                                         op0=ALU.mult, op1=ALU.add), reads=[PL, bbuf], writes=[TM])
                            p.op("act", lambda e: e.activation(out=E[:, :], in_=TM[:, :], func=AF.Exp), reads=[TM], writes=[E])

                    def pv(E=E, VV=VV, j=j, first=first, last=last):
                        for hh in range(4):
                            kh = hh if gqa == 1 else 0
                            PO = pso[hh]
                            p.op("pe", lambda e, hh=hh, kh=kh, PO=PO: e.matmul(PO[:, :128], lhsT=VV[:, j, kh, :], rhs=E[:, hh * 128:(hh + 1) * 128],
                                                                               start=first, stop=last), reads=[VV, E], writes=[PO])
                        p.op("pe", lambda e: e.matmul(psd[:, :], lhsT=ones[:, :], rhs=E[:, :], start=first, stop=last),
                             reads=[ones, E], writes=[psd])

                    if pending[0] is not None:
                        pending[0]()
                    pending[0] = pv
                    first = False
            if pending[0] is not None:
                pending[0]()
                pending[0] = None
            O = oT[io % 2]
            io += 1
            if esink is not None:
                for hh in range(4):
                    p.op("dve", lambda e, hh=hh: e.tensor_scalar(out=rec[:, hh * 128:(hh + 1) * 128], in0=psd[:, hh * 128:(hh + 1) * 128],
                                                                 scalar1=esink[:, h0 + hh:h0 + hh + 1], scalar2=None, op0=ALU.add),
                         reads=[psd, esink], writes=[rec])
                p.op("dve", lambda e: e.reciprocal(out=rec[:, :], in_=rec[:, :]), reads=[rec], writes=[rec])
            else:
                p.op("dve", lambda e: e.reciprocal(out=rec[:, :], in_=psd[:, :]), reads=[psd], writes=[rec])
            for hh in range(4):
                PO = pso[hh]
                p.op("dve", lambda e, hh=hh, PO=PO: e.tensor_tensor(out=O[:, hh, :], in0=PO[:, :128], in1=rec[:, hh * 128:(hh + 1) * 128], op=ALU.mult),
                     reads=[PO, rec], writes=[O])
            p.dma("sp", ov[:, h0:h0 + 4, qb * 128:(qb + 1) * 128], O[:, :, :], reads=[O])
        if post_qb is not None:
            post_qb(qb)


IN_OFF = {}
_o = 0
for _n, _w in (("q_a", 2048), ("k_a", 512), ("v_a", 512), ("c_q", 1024), ("c_kv", 512), ("k_i", 128), ("w_i", 16),
               ("gate_a", None), ("gate_b", None)):
    IN_OFF[_n] = _o
    if _w is not None:
        _o += _w


def xb(B):
    return lambda nb, nw: B[nb // 512][:, :, :nw]


def build(cfg):
    D, S, ML, NE, EPG, FF, KEEP, NIT = cfg["D"], cfg["S"], cfg["ML"], cfg["NE"], cfg["EPG"], cfg["FF"], cfg["KEEP"], cfg["NIT"]
    NEX = N_GROUPS * EPG
    INW = 4752 + 2 * D
    OFF_GA, OFF_GB = 4752, 4752 + D
    NQB = S // 128
    nc = bass.Bass("TRN2", target_bir_lowering=False)

    def din(name, shape, dt=F32):
        return nc.dram_tensor(name, list(shape), dt, kind="ExternalInput").ap()

    def dscr(name, shape, dt):
        return nc.dram_tensor(name, list(shape), dt, kind="Internal").ap()

    x = din("x", [S, D]); mem = din("mem", [ML, D])
    rel31 = din("rel31", [16]); sbias = din("sbias", [128, 16, 2, 128]); dbias = din("dbias", [128, 16, 2, 128])
    g_mix = din("g_mix", [D]); w_in = din("w_in", [D, INW]); g_cq = din("g_cq", [Q_RANK]); w_uq = din("w_uq", [Q_RANK, 2048])
    w_qidx = din("w_qidx", [Q_RANK, 2048]); g_ckv = din("g_ckv", [KV_RANK]); w_uk = din("w_uk", [KV_RANK, 2048])
    w_uv = din("w_uv", [KV_RANK, 2048]); g_kidx = din("g_kidx", [IDX_DIM]); sink_a = din("sink_a", [16])
    w_pa = din("w_pa", [2048, D]); w_pb = din("w_pb", [2048, D]); w_out = din("w_out", [D, D])
    g_xattn = din("g_xattn", [D]); g_mem = din("g_mem", [D]); w_qm = din("w_qm", [D, 512]); w_km = din("w_km", [D, 512])
    w_vm = din("w_vm", [D, 512]); w_om = din("w_om", [512, D]); g_ffn = din("g_ffn", [D])
    w_grp = din("w_grp", [D, N_GROUPS]); b_grp = din("b_grp", [N_GROUPS]); w_exp = din("w_exp", [D, NEX]); b_exp = din("b_exp", [NEX])
    w_e1 = din("w_e1", [NE, D, FF]); w_e3 = din("w_e3", [NE, D, FF]); w_e2 = din("w_e2", [NE, FF, D]); g_final = din("g_final", [D])
    gsel = din("gsel", [N_GROUPS])
    esel = din("esel", [NE, NEX])
    out = nc.dram_tensor("out", [S, D], F32, kind="ExternalOutput").ap()
    own_o = nc.dram_tensor("own", [S, 1], F32, kind="ExternalOutput").ap()

    uT = dscr("uT", [S // 512, 128, D // 128, 512], BF16)
    qkT = dscr("qkT", [2560, S], BF16)
    sgT = dscr("sgT", [2 * D, S], BF16)
    vaTM = dscr("vaTM", [S, 512], BF16)
    tmA = dscr("tmA", [S, 1680], F32)
    cqT = dscr("cqT", [S // 512, 128, Q_RANK // 128, 512], BF16)
    ckvT = dscr("ckvT", [S // 512, 128, KV_RANK // 128, 512], BF16)
    kiT = dscr("kiT", [IDX_DIM, S], BF16)
    qbT = dscr("qbT", [2048, S], BF16)
    qiT = dscr("qiT", [2048, S], BF16)
    kbT = dscr("kbT", [2048, S], BF16)
    vbTM = dscr("vbTM", [S, 2048], BF16)
    oaT = dscr("oaT", [2048, S], BF16)
    obT = dscr("obT", [2048, S], BF16)
    y1T = dscr("y1T", [D, S], BF16)
    yT = dscr("yT", [D, S], BF16)
    x1 = dscr("x1", [S, D], F32)
    u2T = dscr("u2T", [S // 512, 128, D // 128, 512], BF16)
    memT = dscr("memT", [D, ML], BF16)
    qmT = dscr("qmT", [512, S], BF16)
    kmT = dscr("kmT", [512, ML], BF16)
    vmTM = dscr("vmTM", [ML, 512], BF16)
    omT = dscr("omT", [512, S], BF16)
    x2 = dscr("x2", [S, D], F32)
    u3T = dscr("u3T", [S // 512, 128, D // 128, 512], BF16)
    rl = dscr("rl", [S, N_GROUPS + NEX], F32)
    GTt = dscr("GTt", [NE, S], F32)
    HT4 = dscr("HT4", [S // 128, 128, NE * FF // 128, 128], BF16)

    with ExitStack() as es:
        p = Prog(nc, es)
        scale = HD ** -0.5

        rmsnorm(p, x, g_mix, S, D, dstB=uT)
        gemm(p, qkT, None, w_in[:, 0:2560], D, S, 2560, tm=False, cblk=1024, wbufs=1, x_ap_fn=xb(uT))
        gemm(p, sgT, None, w_in[:, OFF_GA:OFF_GA + 2 * D], D, S, 2 * D, tm=False, act=AF.Sigmoid, cblk=1024, wbufs=1, x_ap_fn=xb(uT))
        gemm(p, vaTM, None, w_in[:, 2560:3072], D, S, 512, tm=True, dst_dt=BF16, x_ap_fn=xb(uT))
        gemm(p, tmA, None, w_in[:, 3072:4752], D, S, 1680, tm=True, dst_dt=F32, cblk=1024, wbufs=1, x_ap_fn=xb(uT))
        rmsnorm(p, tmA[:, 0:1024], g_cq, S, 1024, dstB=cqT)
        rmsnorm(p, tmA[:, 1024:1536], g_ckv, S, 512, dstB=ckvT)
        rmsnorm(p, tmA[:, 1536:1664], g_kidx, S, 128, dstT=kiT)
        gemm(p, qbT, None, w_uq, Q_RANK, S, 2048, tm=False, x_ap_fn=xb(cqT))
        gemm(p, qiT, None, w_qidx, Q_RANK, S, 2048, tm=False, x_ap_fn=xb(cqT))
        gemm(p, kbT, None, w_uk, KV_RANK, S, 2048, tm=False, x_ap_fn=xb(ckvT))
        gemm(p, vbTM, None, w_uv, KV_RANK, S, 2048, tm=True, dst_dt=BF16, x_ap_fn=xb(ckvT))

        with Stage(p) as s:
            sb_ = s.sb([128, 16, 2, 128], F32, "sbias")
            p.dma("sp", sb_[:, :, :, :], sbias, writes=[sb_])
            es_ = s.sb([128, 16], F32, "esink")
            p.dma("sp", es_[:, :], sink_a.partition_broadcast(128), writes=[es_])
            p.op("act", lambda e: e.activation(out=es_[:, :], in_=es_[:, :], func=AF.Exp), reads=[es_], writes=[es_])

            def kbs_a(qb):
                return [qb] if qb == 0 else [qb - 1, qb]

            def bias_a(qb, kb, h0):
                j = 1 if kb == qb else 0
                return sb_[:, h0:h0 + 4, j, :], sb_

            attention(p, s, S, oaT, qkT[0:2048, :], qkT[2048:2560, :], vaTM, 16, 4, kbs_a, bias_a, None, es_, scale)

        with Stage(p) as s:
            inv = 1.0 / scale
            db_ = s.sb([128, 16, 2, 128], F32, "dbias")
            p.dma("sp", db_[:, :, :, :], dbias, writes=[db_])
            dbb = s.sb([128, 16, 2, 128], BF16, "dbb")
            p.op("dve", lambda e: e.tensor_scalar(out=dbb[:, :, :, :], in0=db_[:, :, :, :], scalar1=inv, scalar2=None, op0=ALU.mult),
                 reads=[db_], writes=[dbb])
            dbd = s.sb([128, 16, 128], BF16, "dbd")
            dba = s.sb([128, 16, 128], BF16, "dba")
            p.op("dve", lambda e: e.tensor_copy(out=dbd[:, :, :], in_=dbb[:, :, 0, :]), reads=[dbb], writes=[dbd])
            p.op("dve", lambda e: e.tensor_copy(out=dba[:, :, :], in_=dbb[:, :, 1, :]), reads=[dbb], writes=[dba])
            r31 = s.sb([1, 16], F32, "r31")
            p.dma("sp", r31[:, :], rel31.partition_broadcast(1), writes=[r31])
            p.op("dve", lambda e: e.tensor_scalar(out=r31[:, :], in0=r31[:, :], scalar1=inv, scalar2=None, op0=ALU.mult), reads=[r31], writes=[r31])
            bfar = s.sb([1, 16, 128], BF16, "bfar")
            p.op("dve", lambda e: e.tensor_copy(out=bfar[:, :, :], in_=r31[:, :].unsqueeze(2).broadcast_to([1, 16, 128])),
                 reads=[r31], writes=[bfar])
            onesrow = s.sb([1, 128], BF16, "onesrow")
            p.op("pool", lambda e: e.memset(onesrow[:, :], 1.0), writes=[onesrow])
            ident = s.sb([128, 128], BF16, "id")
            idf = s.sb([128, 128], F32, "idf")
            p.op("pool", lambda e: e.memset(idf[:, :], 1.0), writes=[idf])
            p.op("pool", lambda e: e.affine_select(out=idf[:, :], in_=idf[:, :], pattern=[[-1, 128]], compare_op=ALU.is_equal,
                                                   fill=0.0, base=0, channel_multiplier=1), reads=[idf], writes=[idf])
            p.op("dve", lambda e: e.tensor_copy(out=ident[:, :], in_=idf[:, :]), reads=[idf], writes=[ident])
            cneg = s.sb([128, 128], F32, "cneg")
            p.op("pool", lambda e: e.memset(cneg[:, :], 0.0), writes=[cneg])
            p.op("pool", lambda e: e.affine_select(out=cneg[:, :], in_=cneg[:, :], pattern=[[-1, 128]], compare_op=ALU.is_ge,
                                                   fill=-1e30, base=0, channel_multiplier=1), reads=[cneg], writes=[cneg])
            kis = s.sb([128, S], BF16, "kis")
            p.dma("sp", kis[:, :], kiT, writes=[kis])
            acc = s.sb([128, S], F32, "acc")
            mk = s.sb([128, S], BF16, "mk")
            mkT = [s.sb([128, S], BF16, "mkT") for _ in range(2)]
            qi = [s.sb([128, 16, 128], BF16, "qi") for _ in range(2)]
            wi = [s.sb([128, 16], F32, "wi") for _ in range(2)]
            rl_ = [s.sb([128, 512], F32, "relu") for _ in range(2)]
            ptx = s.ps([128, 512], BF16, "ptx")
            sm = {n: s.sb([128, 1], F32, n) for n in ("lo", "hi", "mid", "cnt", "ge", "t1")}
            qiv = qiT.rearrange("(h d) n -> d h n", d=128)
            cnt_i = [0]

            def indexer(qb, psl):
                nk = (qb + 1) * 128
                QI, WI = qi[qb % 2], wi[qb % 2]
                p.dma("sp", QI[:, :, :], qiv[:, :, qb * 128:(qb + 1) * 128], writes=[QI])
                p.dma("sp", WI[:, :], tmA[qb * 128:(qb + 1) * 128, 1664:1680], writes=[WI])
                for c in range(0, nk, 512):
                    cw = min(512, nk - c)
                    for h in range(16):
                        R = rl_[cnt_i[0] % 2]
                        PX = psl[cnt_i[0] % 2]
                        cnt_i[0] += 1
                        p.op("pe", lambda e, h=h: e.matmul(PX[:, :cw], lhsT=QI[:, h, :], rhs=kis[:, c:c + cw], start=True, stop=True),
                             reads=[QI, kis], writes=[PX])
                        p.op("act", lambda e: e.activation(out=R[:, :cw], in_=PX[:, :cw], func=AF.Relu), reads=[PX], writes=[R])
                        if h == 0:
                            p.op("dve", lambda e, h=h: e.tensor_scalar(out=acc[:, c:c + cw], in0=R[:, :cw], scalar1=WI[:, h:h + 1], scalar2=None,
                                                                       op0=ALU.mult), reads=[R, WI], writes=[acc])
                        else:
                            p.op("dve", lambda e, h=h: e.scalar_tensor_tensor(out=acc[:, c:c + cw], in0=R[:, :cw], scalar=WI[:, h:h + 1],
                                                                              in1=acc[:, c:c + cw], op0=ALU.mult, op1=ALU.add),
                                 reads=[R, WI, acc], writes=[acc])
                lo, hi, mid, cnt, ge, t1 = (sm[n] for n in ("lo", "hi", "mid", "cnt", "ge", "t1"))
                p.op("dve", lambda e: e.tensor_reduce(out=lo[:, :], in_=acc[:, :nk], axis=AX.X, op=ALU.min), reads=[acc], writes=[lo])
                p.op("dve", lambda e: e.tensor_reduce(out=hi[:, :], in_=acc[:, :nk], axis=AX.X, op=ALU.max), reads=[acc], writes=[hi])
                p.op("dve", lambda e: e.tensor_scalar(out=lo[:, :], in0=lo[:, :], scalar1=-1.0, scalar2=None, op0=ALU.add), reads=[lo], writes=[lo])
                p.op("dve", lambda e: e.tensor_scalar(out=hi[:, :], in0=hi[:, :], scalar1=1.0, scalar2=None, op0=ALU.add), reads=[hi], writes=[hi])
                p.op("dve", lambda e: e.tensor_tensor(out=acc[:, qb * 128:nk], in0=acc[:, qb * 128:nk], in1=cneg[:, :], op=ALU.add),
                     reads=[acc, cneg], writes=[acc])
                per = (NIT + 3) // 4
                for i_ in range(NIT):
                    if i_ % per == 0:
                        yield
                    p.op("dve", lambda e: e.tensor_tensor(out=mid[:, :], in0=lo[:, :], in1=hi[:, :], op=ALU.add), reads=[lo, hi], writes=[mid])
                    p.op("dve", lambda e: e.tensor_scalar(out=mid[:, :], in0=mid[:, :], scalar1=0.5, scalar2=None, op0=ALU.mult), reads=[mid], writes=[mid])
                    p.op("dve", lambda e: e.tensor_scalar(out=mk[:, :nk], in0=acc[:, :nk], scalar1=mid[:, 0:1], scalar2=None, op0=ALU.is_ge,
                                                          op1=ALU.add, accum_out=cnt[:, 0:1]), reads=[acc, mid], writes=[mk, cnt])
                    p.op("dve", lambda e: e.tensor_scalar(out=ge[:, :], in0=cnt[:, :], scalar1=KEEP - 0.5, scalar2=None, op0=ALU.is_ge),
                         reads=[cnt], writes=[ge])
                    p.op("dve", lambda e: e.tensor_tensor(out=t1[:, :], in0=mid[:, :], in1=lo[:, :], op=ALU.subtract), reads=[mid, lo], writes=[t1])
                    p.op("dve", lambda e: e.scalar_tensor_tensor(out=lo[:, :], in0=t1[:, :], scalar=ge[:, 0:1], in1=lo[:, :], op0=ALU.mult, op1=ALU.add),
                         reads=[t1, ge, lo], writes=[lo])
                    p.op("dve", lambda e: e.tensor_tensor(out=t1[:, :], in0=hi[:, :], in1=mid[:, :], op=ALU.subtract), reads=[hi, mid], writes=[t1])
                    p.op("dve", lambda e: e.scalar_tensor_tensor(out=hi[:, :], in0=t1[:, :], scalar=ge[:, 0:1], in1=mid[:, :], op0=ALU.mult, op1=ALU.add),
                         reads=[t1, ge, mid], writes=[hi])
                p.op("dve", lambda e: e.tensor_scalar(out=mk[:, :nk], in0=acc[:, :nk], scalar1=lo[:, 0:1], scalar2=NEG, op0=ALU.is_lt, op1=ALU.mult),
                     reads=[acc, lo], writes=[mk])

            def mask_transposes(qb):
                MT = mkT[qb % 2]
                for k0 in range(0, qb + 1, 4):
                    n4 = min(4, qb + 1 - k0)
                    for j in range(n4):
                        p.op("pe", lambda e, j=j: e.transpose(out=ptx[:, j * 128:(j + 1) * 128], in_=mk[:, (k0 + j) * 128:(k0 + j + 1) * 128],
                                                              identity=ident[:, :]), reads=[mk, ident], writes=[ptx])
                    p.op("act", lambda e: e.activation(out=MT[:, k0 * 128:(k0 + n4) * 128], in_=ptx[:, :n4 * 128], func=AF.Copy),
                         reads=[ptx], writes=[MT])

            gen = [None]

            def prologue(psl):
                for _ in indexer(0, psl):
                    pass
                mask_transposes(0)

            def hg_hook(qb, hg, psl):
                if qb + 1 >= NQB:
                    return
                if hg == 0:
                    gen[0] = indexer(qb + 1, psl)
                try:
                    next(gen[0])
                    if hg == 3:
                        for _ in gen[0]:
                            pass
                except StopIteration:
                    pass

            def post_qb(qb):
                if qb + 1 < NQB:
                    if gen[0] is not None:
                        for _ in gen[0]:
                            pass
                    mask_transposes(qb + 1)

            def kbs_b(qb):
                return list(range(qb + 1))

            def bias_b(qb, kb, h0):
                if kb == qb:
                    return "tile", dbd[:, h0:h0 + 4, :].rearrange("p h n -> p (h n)"), dbd
                if kb == qb - 1:
                    return "tile", dba[:, h0:h0 + 4, :].rearrange("p h n -> p (h n)"), dba
                return "row", bfar[0:1, h0:h0 + 4, :].rearrange("p h n -> p (h n)"), bfar

            def mask_b(qb, kb):
                return mkT[qb % 2][:, kb * 128:(kb + 1) * 128], mkT[qb % 2]

            attention(p, s, S, obT, qbT, kbT, vbTM, 16, 1, kbs_b, bias_b, mask_b, None, scale, pre_qb=prologue,
                      pe_bias=dict(ident=ident, onesrow=onesrow), hg_hook=hg_hook, post_qb=post_qb)

        gemm(p, y1T, oaT, w_pa, 2048, S, D, tm=False, mulT=sgT[0:D, :], cblk=1024)
        gemm(p, yT, obT, w_pb, 2048, S, D, tm=False, mulT=sgT[D:2 * D, :], addT=y1T, cblk=1024)
        gemm(p, x1, yT, w_out, D, S, D, tm=True, add_tm=x, dst_dt=F32, cblk=1024, wbufs=1)

        rmsnorm(p, x1, g_xattn, S, D, dstB=u2T)
        rmsnorm(p, mem, g_mem, ML, D, dstT=memT)
        gemm(p, qmT, None, w_qm, D, S, 512, tm=False, x_ap_fn=xb(u2T))
        gemm(p, kmT, memT, w_km, D, ML, 512, tm=False)
        gemm(p, vmTM, memT, w_vm, D, ML, 512, tm=True, dst_dt=BF16)
        with Stage(p) as s:
            attention(p, s, S, omT, qmT, kmT, vmTM, 4, 1, lambda qb: list(range(ML // 128)), None, None, None, scale)
        gemm(p, x2, omT, w_om, 512, S, D, tm=True, add_tm=x1, dst_dt=F32)

        rmsnorm(p, x2, g_ffn, S, D, dstB=u3T)
        gemm(p, rl[:, 0:N_GROUPS], None, w_grp, D, S, N_GROUPS, tm=True, dst_dt=F32, x_ap_fn=xb(u3T))
        gemm(p, rl[:, N_GROUPS:], None, w_exp, D, S, NEX, tm=True, dst_dt=F32, x_ap_fn=xb(u3T))
        with Stage(p) as s:
            bg = s.sb([128, N_GROUPS], F32, "bg"); be = s.sb([128, NEX], F32, "be"); gs = s.sb([128, N_GROUPS], F32, "gs")
            esl = s.sb([128, NE, NEX], F32, "esl")
            p.dma("sp", bg[:, :], b_grp.partition_broadcast(128), writes=[bg])
            p.dma("sp", be[:, :], b_exp.partition_broadcast(128), writes=[be])
            p.dma("sp", gs[:, :], gsel.partition_broadcast(128), writes=[gs])
            p.dma("sp", esl[:, :, :], esel.partition_broadcast(128), writes=[esl])
            L = s.sb([128, N_GROUPS + NEX], F32, "L")
            V = {n: s.sb([128, 1], F32, n) for n in ("gmax", "ngmax", "gsum", "top1", "top2", "d", "p1", "p2", "own")}
            goh = s.sb([128, N_GROUPS], F32, "goh"); pen = s.sb([128, N_GROUPS], F32, "pen"); gj = s.sb([128, N_GROUPS], F32, "gj")
            elm = s.sb([128, NEX], F32, "elm"); oh1 = s.sb([128, NEX], F32, "oh1"); oh2 = s.sb([128, NEX], F32, "oh2"); G = s.sb([128, NEX], F32, "G")
            prod = s.sb([128, NE, NEX], F32, "prod"); GL = s.sb([128, NE], F32, "GL")
            idf = s.sb([128, 128], F32, "idf")
            p.op("pool", lambda e: e.memset(idf[:, :], 1.0), writes=[idf])
            p.op("pool", lambda e: e.affine_select(out=idf[:, :], in_=idf[:, :], pattern=[[-1, 128]], compare_op=ALU.is_equal,
                                                   fill=0.0, base=0, channel_multiplier=1), reads=[idf], writes=[idf])
            pgt = s.ps([128, 128], F32, "pgt")
            glt = s.sb([NE, 128], F32, "glt")

            def dv(fn, reads, writes):
                p.op("dve", fn, reads=reads, writes=writes)

            for i in range(NQB):
                p.dma("sp", L[:, :], rl[i * 128:(i + 1) * 128, :], writes=[L])
                gl, el = L[:, 0:N_GROUPS], L[:, N_GROUPS:]
                dv(lambda e: e.tensor_tensor(out=gl, in0=gl, in1=bg[:, :], op=ALU.add), [L, bg], [L])
                dv(lambda e: e.tensor_tensor(out=el, in0=el, in1=be[:, :], op=ALU.add), [L, be], [L])
                dv(lambda e: e.tensor_reduce(out=V["gmax"][:, :], in_=gl, axis=AX.X, op=ALU.max), [L], [V["gmax"]])
                dv(lambda e: e.tensor_scalar(out=goh[:, :], in0=gl, scalar1=V["gmax"][:, 0:1], scalar2=None, op0=ALU.is_equal), [L, V["gmax"]], [goh])
                dv(lambda e: e.tensor_scalar(out=V["ngmax"][:, :], in0=V["gmax"][:, :], scalar1=-1.0, scalar2=None, op0=ALU.mult), [V["gmax"]], [V["ngmax"]])
                p.op("act", lambda e: e.activation(out=gj[:, :], in_=gl, func=AF.Exp, bias=V["ngmax"][:, 0:1], accum_out=V["gsum"][:, 0:1]),
                     reads=[L, V["ngmax"]], writes=[gj, V["gsum"]])
                dv(lambda e: e.tensor_scalar(out=pen[:, :], in0=goh[:, :], scalar1=-1.0, scalar2=1e9, op0=ALU.add, op1=ALU.mult), [goh], [pen])
                dv(lambda e: e.tensor_tensor(out=elm[:, :].rearrange("p (g j) -> p g j", j=EPG), in0=el.rearrange("p (g j) -> p g j", j=EPG),
                                             in1=pen[:, :].unsqueeze(2).broadcast_to([128, N_GROUPS, EPG]), op=ALU.add), [L, pen], [elm])
                dv(lambda e: e.tensor_reduce(out=V["top1"][:, :], in_=elm[:, :], axis=AX.X, op=ALU.max), [elm], [V["top1"]])
                dv(lambda e: e.tensor_scalar(out=oh1[:, :], in0=elm[:, :], scalar1=V["top1"][:, 0:1], scalar2=None, op0=ALU.is_equal), [elm, V["top1"]], [oh1])
                dv(lambda e: e.scalar_tensor_tensor(out=elm[:, :], in0=oh1[:, :], scalar=-1e9, in1=elm[:, :], op0=ALU.mult, op1=ALU.add), [oh1, elm], [elm])
                dv(lambda e: e.tensor_reduce(out=V["top2"][:, :], in_=elm[:, :], axis=AX.X, op=ALU.max), [elm], [V["top2"]])
                dv(lambda e: e.tensor_scalar(out=oh2[:, :], in0=elm[:, :], scalar1=V["top2"][:, 0:1], scalar2=None, op0=ALU.is_equal), [elm, V["top2"]], [oh2])
                dv(lambda e: e.tensor_tensor(out=V["d"][:, :], in0=V["top2"][:, :], in1=V["top1"][:, :], op=ALU.subtract), [V["top1"], V["top2"]], [V["d"]])
                p.op("act", lambda e: e.activation(out=V["d"][:, :], in_=V["d"][:, :], func=AF.Exp), reads=[V["d"]], writes=[V["d"]])
                dv(lambda e: e.tensor_scalar(out=V["p1"][:, :], in0=V["d"][:, :], scalar1=1.0, scalar2=None, op0=ALU.add), [V["d"]], [V["p1"]])
                dv(lambda e: e.tensor_tensor(out=V["p1"][:, :], in0=V["p1"][:, :], in1=V["gsum"][:, :], op=ALU.mult), [V["p1"], V["gsum"]], [V["p1"]])
                dv(lambda e: e.reciprocal(out=V["p1"][:, :], in_=V["p1"][:, :]), [V["p1"]], [V["p1"]])
                dv(lambda e: e.tensor_tensor(out=V["p2"][:, :], in0=V["p1"][:, :], in1=V["d"][:, :], op=ALU.mult), [V["p1"], V["d"]], [V["p2"]])
                dv(lambda e: e.tensor_scalar(out=G[:, :], in0=oh1[:, :], scalar1=V["p1"][:, 0:1], scalar2=None, op0=ALU.mult), [oh1, V["p1"]], [G])
                dv(lambda e: e.scalar_tensor_tensor(out=G[:, :], in0=oh2[:, :], scalar=V["p2"][:, 0:1], in1=G[:, :], op0=ALU.mult, op1=ALU.add), [oh2, V["p2"], G], [G])
                dv(lambda e: e.tensor_tensor(out=prod[:, :, :], in0=esl[:, :, :], in1=G[:, :].unsqueeze(1).broadcast_to([128, NE, NEX]), op=ALU.mult), [esl, G], [prod])
                dv(lambda e: e.tensor_reduce(out=GL[:, :], in_=prod[:, :, :], axis=AX.X, op=ALU.add), [prod], [GL])
                dv(lambda e: e.tensor_tensor(out=gj[:, :], in0=goh[:, :], in1=gs[:, :], op=ALU.mult), [goh, gs], [gj])
                dv(lambda e: e.tensor_reduce(out=V["own"][:, :], in_=gj[:, :], axis=AX.X, op=ALU.add), [gj], [V["own"]])
                p.op("pe", lambda e: e.transpose(out=pgt[0:NE, :], in_=GL[:, :], identity=idf[:, :]), reads=[GL, idf], writes=[pgt])
                p.op("act", lambda e: e.activation(out=glt[:, :], in_=pgt[0:NE, :], func=AF.Copy), reads=[pgt], writes=[glt])
                p.dma("sp", GTt[:, i * 128:(i + 1) * 128], glt[:, :], reads=[glt])
                p.dma("sp", own_o[i * 128:(i + 1) * 128, :], V["own"][:, :], reads=[V["own"]])

        for le in range(NE):
            gemm_gated(p, lambda cc, nb, nw, le=le: HT4[nb // 128:(nb + nw) // 128, :, le * (FF // 128) + cc, :].rearrange("t p n -> p t n"),
                       xb(u3T), w_e1[le], w_e3[le], GTt[le], D, S, FF)
        KCT = NE * FF // 128
        KH = KCT // 2
        w2cat = w_e2.rearrange("e f d -> (e f) d")
        part = dscr("mpart", [S, D], F32)
        prev = dscr("macc", [S, D], F32)
        gemm(p, part, None, w2cat[0:KH * 128, :], KH * 128, S, D, tm=True, add_tm=x2, dst_dt=F32,
             cblk=1024, wbufs=1, nblk=128, x_ap_fn=lambda nb, nw: HT4[nb // 128][:, 0:KH, :])
        gemm(p, prev, None, w2cat[KH * 128:, :], (KCT - KH) * 128, S, D, tm=True, add_tm=part, dst_dt=F32,
             cblk=1024, wbufs=1, nblk=128, x_ap_fn=lambda nb, nw: HT4[nb // 128][:, KH:KCT, :])

        rmsnorm(p, prev, g_final, S, D, dstT=None, dst_tm=out, dst_tm_dt=F32)
        p.barrier()
        STATS.update(cnt=dict(p.cnt), nsem=p.nsem)
    return nc


def _bucket(d):
    d = np.maximum(d, 0)
    df = np.maximum(d, 1).astype(np.float32)
    large = 16 + (np.log(df / np.float32(16)) / np.float32(math.log(128 / 16)) * np.float32(16)).astype(np.int32)
    large = np.minimum(large, 31)
    return np.where(d < 16, d, large)


def _bias_tiles(rel_bias):
    s = np.arange(128)[:, None]
    t = np.arange(128)[None, :]
    sb = np.full((128, 16, 2, 128), NEG, np.float32)
    db = np.full((128, 16, 2, 128), NEG, np.float32)
    for j in range(2):
        dist = t + 128 - (j * 128 + s)
        valid = (dist >= 0) & (dist < 128)
        g = rel_bias[_bucket(dist)]
        for h in range(16):
            sb[:, h, j, :] = np.where(valid, g[:, :, h], np.float32(NEG))
        dist = t - s + (128 if j == 1 else 0)
        valid = dist >= 0
        g = rel_bias[_bucket(dist)]
        for h in range(16):
            db[:, h, j, :] = np.where(valid, g[:, :, 16 + h], np.float32(NEG))
    return sb, db


def make_in_maps(inputs, cfg, n_cores):
    f = lambda a: np.ascontiguousarray(np.asarray(a, dtype=np.float32))
    NE, EPG = cfg["NE"], cfg["EPG"]
    NEX = N_GROUPS * EPG
    rel_bias = f(inputs["rel_bias"])
    sb, db = _bias_tiles(rel_bias)
    cpb = cfg["cores_per_batch"]
    gpc = N_GROUPS // cpb
    shared = dict(
        rel31=f(rel_bias[31, 16:32]), sbias=sb, dbias=db,
        g_mix=f(inputs["g_mix"][0]), w_in=f(inputs["w_in"][0]), g_cq=f(inputs["g_cq"][0]), w_uq=f(inputs["w_uq"][0]),
        w_qidx=f(inputs["w_qidx"][0]), g_ckv=f(inputs["g_ckv"][0]), w_uk=f(inputs["w_uk"][0]).reshape(KV_RANK, 2048),
        w_uv=f(inputs["w_uv"][0]).reshape(KV_RANK, 2048), g_kidx=f(inputs["g_kidx"][0]), sink_a=f(inputs["sink_a"][0]),
        w_pa=f(inputs["w_pa"][0]), w_pb=f(inputs["w_pb"][0]), w_out=f(inputs["w_out"][0]), g_xattn=f(inputs["g_xattn"][0]),
        g_mem=f(inputs["g_mem"][0]), w_qm=f(inputs["w_qm"][0]), w_km=f(inputs["w_km"][0]), w_vm=f(inputs["w_vm"][0]),
        w_om=f(inputs["w_om"][0]), g_ffn=f(inputs["g_ffn"][0]), w_grp=f(inputs["w_grp"][0]), b_grp=f(inputs["b_grp"][0]),
        w_exp=f(inputs["w_exp"][0]), b_exp=f(inputs["b_exp"][0]), g_final=f(inputs["g_final"]),
    )
    maps = []
    for c in range(n_cores):
        b, pp = c // cpb, c % cpb
        e0 = pp * gpc * EPG
        gsel = np.zeros(N_GROUPS, np.float32)
        gsel[pp * gpc:(pp + 1) * gpc] = 1.0
        esel = np.zeros((NE, NEX), np.float32)
        esel[np.arange(NE), e0 + np.arange(NE)] = 1.0
        m = dict(shared)
        m.update(x=f(inputs["x"][b]), mem=f(inputs["mem"][b]), gsel=gsel, esel=esel,
                 w_e1=f(inputs["w_e1"][0][e0:e0 + NE]), w_e3=f(inputs["w_e3"][0][e0:e0 + NE]), w_e2=f(inputs["w_e2"][0][e0:e0 + NE]))
        maps.append(m)
    return maps


def assemble(results, cfg, n_cores, B, S, D):
    cpb = cfg["cores_per_batch"]
    out = np.zeros((B, S, D), np.float32)
    for c in range(n_cores):
        b = c // cpb
        own = results[c]["own"][:, 0] > 0.5
        out[b][own] = results[c]["out"][own]
    return out


STATS = {}
CFG = dict(D=4096, S=8192, ML=256, NE=16, EPG=8, FF=768, KEEP=256, NIT=26, cores_per_batch=4)


def kernel(**inputs):
    cfg = CFG
    nc = build(cfg)
    maps = make_in_maps(inputs, cfg, 8)
    res = run_bass_kernel_spmd(nc, maps, core_ids=list(range(8)))
    return assemble(res.results, cfg, 8, 2, cfg["S"], cfg["D"])
```
